# Optimizing a Trainium2 kernel written in Bass

```python
import math
import jax, jax.numpy as jnp
from jax import lax
import numpy as np

D_MODEL = 2048
BATCH = 4
SEQ = 2048
DEPTH = 1
DEC_BATCH = 128
DEC_SEQ = 1
PAST_LEN = 16384
PAGE_SIZE = 128

CONV_W = D_MODEL // 2
CONV_K = 31
RWKV_W = D_MODEL - CONV_W
RWKV_HEAD = 64
RWKV_H = RWKV_W // RWKV_HEAD
DECAY_LORA = 64
AAA_LORA = 64
GATE_LORA = 160
SHIFT_W = 3 * RWKV_W + DECAY_LORA + AAA_LORA + GATE_LORA
IN_W = 2 * CONV_W + SHIFT_W
N_MEM = 256
X_HEADS = 4
X_HEAD = D_MODEL // X_HEADS
N_GROUPS = 4
EXP_PER_GROUP = 8
TOP_K_IN_GROUP = 2
D_EXPERT = D_MODEL // 4
RMS_EPS = 1e-6
LN_EPS = 1e-5
GN_EPS = 64e-5
DECAY_SCALE = math.exp(-0.5)

kernel_name = 'hymba_conformer_rwkv7_hmoe_step'

F32 = jnp.float32


def rmsnorm(x, g):
    xf = x.astype(F32)
    y = xf * lax.rsqrt(jnp.mean(xf * xf, axis=-1, keepdims=True) + RMS_EPS)
    return (y * g.astype(F32)).astype(x.dtype)


def conformer_conv(u, conv_buf, conv_w, conv_b, ln_g, ln_b):
    u_full = jnp.concatenate([conv_buf.astype(u.dtype), u], axis=1)
    c = lax.conv_general_dilated(
        u_full, conv_w[:, None, :].astype(u.dtype), (1,), 'VALID',
        dimension_numbers=('NWC', 'WIO', 'NWC'), feature_group_count=CONV_W)
    cf = (c + conv_b).astype(F32)
    mu = jnp.mean(cf, axis=-1, keepdims=True)
    var = jnp.mean(jnp.square(cf - mu), axis=-1, keepdims=True)
    cf = (cf - mu) * lax.rsqrt(var + LN_EPS) * ln_g.astype(F32) + ln_b.astype(F32)
    return jax.nn.silu(cf).astype(u.dtype), u_full[:, -(CONV_K - 1):]


def rwkv7_recurrence(S0, r, w, k, v, kk, a):
    def step(S, inp):
        r_t, w_t, k_t, v_t, kk_t, a_t = inp
        sa = jnp.einsum('bhvk,bhk->bhv', S, kk_t)
        S = (S * w_t[:, :, None, :]
             - sa[..., None] * (kk_t * a_t)[:, :, None, :]
             + v_t[..., None] * k_t[:, :, None, :])
        return S, jnp.einsum('bhvk,bhk->bhv', S, r_t)
    xs = tuple(jnp.moveaxis(t, 1, 0) for t in (r, w, k, v, kk, a))
    S, o = lax.scan(step, S0, xs)
    return S, jnp.moveaxis(o, 0, 1)


def rwkv7_mix(q, shift_buf, S0, p):
    B, T, _ = q.shape
    q_prev = jnp.concatenate([shift_buf[:, None, :].astype(q.dtype), q[:, :-1]], axis=1)
    qs = q + (q_prev - q) * p['shift_mu']
    splits = np.cumsum([RWKV_W, RWKV_W, RWKV_W, DECAY_LORA, AAA_LORA]).tolist()
    r, k, v, pw, pa, pg = jnp.split(qs, splits, axis=-1)
    hd = lambda t: t.reshape(B, T, RWKV_H, RWKV_HEAD)
    decay = jnp.exp(-DECAY_SCALE * jax.nn.sigmoid(
        (p['decay_bias'] + jnp.tanh(pw) @ p['w_decay_up']).astype(F32)))
    a = jax.nn.sigmoid((p['a_bias'] + pa @ p['w_a_up']).astype(F32))
    g = jax.nn.sigmoid(pg) @ p['w_g_up']
    r = r.astype(F32)
    k = k.astype(F32)
    v = v.astype(F32)
    kk = hd(k * p['k_k'].astype(F32))
    kk = kk / jnp.maximum(jnp.sqrt(jnp.sum(kk * kk, axis=-1, keepdims=True)), 1e-12)
    k = k * (1.0 + (a - 1.0) * p['k_a'].astype(F32))
    S, o = rwkv7_recurrence(S0, hd(r), hd(decay), hd(k), hd(v), kk, hd(a))
    mu = jnp.mean(o, axis=-1, keepdims=True)
    var = jnp.mean(jnp.square(o - mu), axis=-1, keepdims=True)
    o = ((o - mu) * lax.rsqrt(var + GN_EPS)).reshape(B, T, RWKV_W)
    o = o * p['lnx_g'].astype(F32) + p['lnx_b'].astype(F32)
    bonus = jnp.sum(hd(r) * hd(k) * p['r_k'].astype(F32), axis=-1, keepdims=True) * hd(v)
    o = (o + bonus.reshape(B, T, RWKV_W)) * g.astype(F32)
    return o.astype(q.dtype), S, q[:, -1]


def hier_moe(h, p):
    hf = h.astype(F32)
    lg = hf @ p['w_route_group'].astype(F32) + p['b_route_group'].astype(F32)
    pg = jax.nn.softmax(lg, axis=-1)
    g_val, g_idx = lax.top_k(pg, 1)
    le = jnp.einsum('nd,dge->nge', hf, p['w_route_expert'].astype(F32)) + p['b_route_expert'].astype(F32)
    le_sel = jnp.take_along_axis(le, g_idx[:, :, None], axis=1)[:, 0]
    e_val, e_idx = lax.top_k(le_sel, TOP_K_IN_GROUP)
    e_w = jax.nn.softmax(e_val, axis=-1) * g_val
    e_comb = jnp.sum(jax.nn.one_hot(e_idx, EXP_PER_GROUP, dtype=F32) * e_w[..., None], axis=1)
    combine = jax.nn.one_hot(g_idx[:, 0], N_GROUPS, dtype=F32)[:, :, None] * e_comb[:, None, :]
    out = jnp.zeros(h.shape, F32)
    for gi in range(N_GROUPS):
        hg = jnp.einsum('nd,edf->nef', h, p['w_gate'][gi])
        hu = jnp.einsum('nd,edf->nef', h, p['w_up'][gi])
        act = jax.nn.silu(hg) * hu * combine[:, gi, :, None].astype(h.dtype)
        out = out + jnp.einsum('nef,efd->nd', act, p['w_down'][gi]).astype(F32)
    return out.astype(h.dtype)


def layer(x, mem_k, mem_v, conv_buf, shift_buf, S0, p):
    B, T, _ = x.shape
    h = rmsnorm(x, p['norm_mix'])
    proj = h @ p['w_in']
    u = proj[..., :CONV_W] * jax.nn.sigmoid(proj[..., CONV_W:2 * CONV_W])
    c, new_conv = conformer_conv(u, conv_buf, p['conv_w'], p['conv_b'], p['conv_ln_g'], p['conv_ln_b'])
    o, new_S, new_shift = rwkv7_mix(proj[..., 2 * CONV_W:], shift_buf, S0, p)
    x = x + jnp.concatenate([c, o], axis=-1) @ p['w_out']
    q = (rmsnorm(x, p['norm_x']) @ p['w_cq']).reshape(B, T, X_HEADS, X_HEAD)
    s = jnp.einsum('bthd,bmhd->bhtm', q.astype(F32), mem_k.astype(F32)) * (X_HEAD ** -0.5)
    att = jax.nn.softmax(s, axis=-1)
    ctx = jnp.einsum('bhtm,bmhd->bthd', att, mem_v.astype(F32)).astype(x.dtype).reshape(B, T, D_MODEL)
    x = x + ctx @ p['w_co']
    h2 = rmsnorm(x, p['norm_ffn']).reshape(B * T, D_MODEL)
    x = x + hier_moe(h2, p).reshape(B, T, D_MODEL)
    return x, new_conv, new_shift, new_S


def setup_inputs(seed: int = 0) -> dict:
    key = jax.random.key(seed)
    ks = iter(jax.random.split(key, 48))
    L = DEPTH
    D = D_MODEL

    def nrm(shape, scale):
        return scale * jax.random.normal(next(ks), shape, F32)

    def gain(shape):
        return 1.0 + 0.05 * jax.random.normal(next(ks), shape, F32)

    return {
        'x_prompt': nrm((BATCH, SEQ, D), 1.0),
        'x_sample': nrm((DEC_BATCH, DEC_SEQ, D), 1.0),
        'mem_prompt': nrm((BATCH, N_MEM, D), 1.0),
        'cache_conv': nrm((L, DEC_BATCH, CONV_K - 1, CONV_W), 0.5),
        'state_shift': nrm((L, DEC_BATCH, SHIFT_W), 1.0),
        'state_rwkv': nrm((L, DEC_BATCH, RWKV_H, RWKV_HEAD, RWKV_HEAD), 0.3),
        'cache_mem_k': nrm((L, DEC_BATCH, N_MEM, X_HEADS, X_HEAD), 1.0),
        'cache_mem_v': nrm((L, DEC_BATCH, N_MEM, X_HEADS, X_HEAD), 1.0),
        'norm_mix': gain((L, D)),
        'w_in': nrm((L, D, IN_W), D ** -0.5),
        'conv_w': nrm((L, CONV_K, CONV_W), CONV_K ** -0.5),
        'conv_b': nrm((L, CONV_W), 0.02),
        'conv_ln_g': gain((L, CONV_W)),
        'conv_ln_b': nrm((L, CONV_W), 0.02),
        'shift_mu': jax.random.uniform(next(ks), (L, SHIFT_W), F32),
        'w_decay_up': nrm((L, DECAY_LORA, RWKV_W), DECAY_LORA ** -0.5),
        'decay_bias': nrm((L, RWKV_W), 1.0),
        'w_a_up': nrm((L, AAA_LORA, RWKV_W), AAA_LORA ** -0.5),
        'a_bias': nrm((L, RWKV_W), 0.5),
        'w_g_up': nrm((L, GATE_LORA, RWKV_W), GATE_LORA ** -0.5),
        'k_k': 0.85 + nrm((L, RWKV_W), 0.05),
        'k_a': gain((L, RWKV_W)),
        'r_k': nrm((L, RWKV_H, RWKV_HEAD), 0.1),
        'lnx_g': gain((L, RWKV_W)),
        'lnx_b': nrm((L, RWKV_W), 0.02),
        'w_out': nrm((L, D, D), D ** -0.5),
        'norm_x': gain((L, D)),
        'norm_mem': gain((L, D)),
        'w_cq': nrm((L, D, D), D ** -0.5),
        'w_ck': nrm((L, D, D), D ** -0.5),
        'w_cv': nrm((L, D, D), D ** -0.5),
        'w_co': nrm((L, D, D), D ** -0.5),
        'norm_ffn': gain((L, D)),
        'w_route_group': nrm((L, D, N_GROUPS), D ** -0.5),
        'b_route_group': nrm((L, N_GROUPS), 0.01),
        'w_route_expert': nrm((L, D, N_GROUPS, EXP_PER_GROUP), D ** -0.5),
        'b_route_expert': nrm((L, N_GROUPS, EXP_PER_GROUP), 0.01),
        'w_gate': nrm((L, N_GROUPS, EXP_PER_GROUP, D, D_EXPERT), D ** -0.5),
        'w_up': nrm((L, N_GROUPS, EXP_PER_GROUP, D, D_EXPERT), D ** -0.5),
        'w_down': nrm((L, N_GROUPS, EXP_PER_GROUP, D_EXPERT, D), D_EXPERT ** -0.5),
        'norm_final': gain((D,)),
    }


def reference(x_prompt, x_sample, mem_prompt, cache_conv, state_shift, state_rwkv,
              cache_mem_k, cache_mem_v, norm_mix, w_in, conv_w, conv_b, conv_ln_g, conv_ln_b,
              shift_mu, w_decay_up, decay_bias, w_a_up, a_bias, w_g_up, k_k, k_a, r_k,
              lnx_g, lnx_b, w_out, norm_x, norm_mem, w_cq, w_ck, w_cv, w_co, norm_ffn,
              w_route_group, b_route_group, w_route_expert, b_route_expert,
              w_gate, w_up, w_down, norm_final):
    xp = x_prompt
    xs = x_sample
    B = xp.shape[0]
    conv_p, shift_p, rwkv_p, memk_p, memv_p = [], [], [], [], []
    conv_s, shift_s, rwkv_s = [], [], []
    for l in range(DEPTH):
        p = dict(norm_mix=norm_mix[l], w_in=w_in[l], conv_w=conv_w[l], conv_b=conv_b[l],
                 conv_ln_g=conv_ln_g[l], conv_ln_b=conv_ln_b[l], shift_mu=shift_mu[l],
                 w_decay_up=w_decay_up[l], decay_bias=decay_bias[l], w_a_up=w_a_up[l],
                 a_bias=a_bias[l], w_g_up=w_g_up[l], k_k=k_k[l], k_a=k_a[l], r_k=r_k[l],
                 lnx_g=lnx_g[l], lnx_b=lnx_b[l], w_out=w_out[l], norm_x=norm_x[l],
                 w_cq=w_cq[l], w_co=w_co[l], norm_ffn=norm_ffn[l],
                 w_route_group=w_route_group[l], b_route_group=b_route_group[l],
                 w_route_expert=w_route_expert[l], b_route_expert=b_route_expert[l],
                 w_gate=w_gate[l], w_up=w_up[l], w_down=w_down[l])
        mn = rmsnorm(mem_prompt, norm_mem[l])
        mk = (mn @ w_ck[l]).reshape(B, N_MEM, X_HEADS, X_HEAD)
        mv = (mn @ w_cv[l]).reshape(B, N_MEM, X_HEADS, X_HEAD)
        xp, cp, sp, Sp = layer(xp, mk, mv,
                               jnp.zeros((B, CONV_K - 1, CONV_W), xp.dtype),
                               jnp.zeros((B, SHIFT_W), xp.dtype),
                               jnp.zeros((B, RWKV_H, RWKV_HEAD, RWKV_HEAD), F32), p)
        xs, cs, ss, Ss = layer(xs, cache_mem_k[l], cache_mem_v[l], cache_conv[l],
                               state_shift[l], state_rwkv[l].astype(F32), p)
        conv_p.append(cp)
        shift_p.append(sp)
        rwkv_p.append(Sp)
        memk_p.append(mk)
        memv_p.append(mv)
        conv_s.append(cs)
        shift_s.append(ss)
        rwkv_s.append(Ss)
    y_prompt = rmsnorm(xp, norm_final)
    y_sample = rmsnorm(xs, norm_final)
    return (y_prompt, y_sample, jnp.stack(conv_p), jnp.stack(shift_p), jnp.stack(rwkv_p),
            jnp.stack(memk_p), jnp.stack(memv_p), jnp.stack(conv_s), jnp.stack(shift_s),
            jnp.stack(rwkv_s))
```

```python
import contextlib
import math
import os
import numpy as np
import concourse.bass as bass
import concourse.mybir as mybir
from concourse.bass_utils import run_bass_kernel_spmd

F32 = mybir.dt.float32
BF16 = mybir.dt.bfloat16
AF = mybir.ActivationFunctionType
ALU = mybir.AluOpType
AX = mybir.AxisListType

D = 2048
NW = 16
NOWN = 8
SHIFT_W = 3360
IN_W = 5408
DS = math.exp(-0.5)
RMS_EPS = 1e-6
LN_EPS = 1e-5
GN_EPS = 64e-5
CW = 1024


class Buf:
    __slots__ = ("last_w", "readers")

    def __init__(self):
        self.last_w = None
        self.readers = []


class Op:
    __slots__ = ("eng", "fn", "deps", "idx", "eidx", "is_dma", "sig", "need", "key")


class Prog:
    EPOCH = 16000

    def __init__(self, nc, semfn):
        self.nc = nc
        self.semfn = semfn
        self.ops = []
        self.nops = 0
        self.ecount = {}
        self.engs = {"pe": nc.tensor, "act": nc.scalar, "dve": nc.vector,
                     "pool": nc.gpsimd, "sp": nc.sync}
        self.bufs = []
        self.esem = {}
        self.ecnt = {}
        self.dsem = {}
        self.dcnt = {}
        self.waited = {}
        self.last_on = {}
        self.bar_deps = []
        self.bar_id = 0
        self.eng_bar = {}
        self.nwait = 0
        self.free_dsems = []
        self.free_by = {}
        self.dcls = {}
        self.nds = 0

    def buf(self):
        b = Buf()
        self.bufs.append(b)
        return b

    def op(self, eng, fn, reads=(), writes=(), dma=False, key=None):
        o = Op()
        o.eng, o.fn, o.is_dma, o.key = eng, fn, dma, key
        o.sig = None
        o.need = dma
        o.idx = self.nops
        self.nops += 1
        o.eidx = self.ecount.get(eng, 0)
        self.ecount[eng] = o.eidx + 1
        deps = {}
        for b in reads:
            p = b.last_w
            if p is None:
                continue
            if (not p.is_dma) and p.eng == eng:
                if dma or (eng != "pe" and o.eidx - p.eidx <= 2):
                    deps[p.idx] = p
            else:
                deps[p.idx] = p
        for b in writes:
            p = b.last_w
            if p is not None and (p.is_dma or p.eng != eng or dma):
                deps[p.idx] = p
            for rd in b.readers:
                if rd.is_dma or rd.eng != eng or dma:
                    deps[rd.idx] = rd
        for b in reads:
            b.readers.append(o)
        for b in writes:
            b.last_w = o
            b.readers = []
        deps.pop(o.idx, None)
        o.deps = list(deps.values())
        for p in o.deps:
            p.need = True
        self.ops.append(o)
        self.last_on[eng] = o
        return o

    def dma(self, q, out, in_, reads, writes, key):
        if writes:
            key = ("w", id(writes[0]))
        return self.op(q, lambda e: e.dma_start(out=out, in_=in_), reads, writes, dma=True, key=key)

    def flush(self, final=False):
        self.nflush = getattr(self, "nflush", -1) + 1
        if self.nflush in [int(x) for x in os.environ.get("KB_DROP", "").split(",") if x]:
            self.ops = []
            self.last_on = {}
            for b in self.bufs:
                b.last_w = None
                b.readers = []
            return
        had_ops = len(self.ops) > 0
        for e, o in self.last_on.items():
            if o is not None and not o.is_dma:
                o.need = True
        nkey = {}
        for o in self.ops:
            if o.need and o.is_dma:
                nkey[o.key] = nkey.get(o.key, 0) + 1
        for o in self.ops:
            if not o.need:
                continue
            if o.is_dma:
                k = o.key
                cls = "sw" if o.eng == "pool" else "hw"
                if k not in self.dsem:
                    fl = self.free_by.setdefault(cls, [])
                    fl.sort(key=lambda sc: -sc[1])
                    self.dcls[k] = cls
                    if fl and fl[-1][1] + 16 * nkey[k] <= 24000:
                        self.dsem[k], self.dcnt[k] = fl.pop()
                    else:
                        self.nds += 1
                        self.dsem[k] = self.semfn("dsem%d" % self.nds)
                        self.dcnt[k] = 0
                assert self.dcls[k] == cls, (k, cls)
                self.dcnt[k] += 16
                o.sig = (self.dsem[k], self.dcnt[k], 16)
            else:
                c = self.ecnt.get(o.eng, 0)
                ep = c // self.EPOCH
                lst = self.esem.setdefault(o.eng, [])
                while len(lst) <= ep:
                    lst.append(self.semfn("e_%s_%d" % (o.eng, len(lst))))
                o.sig = (lst[ep], c % self.EPOCH + 1, 1)
                self.ecnt[o.eng] = c + 1
        for o in self.ops:
            e = self.engs[o.eng]
            w = self.waited.setdefault(o.eng, {})
            need = {}
            if self.eng_bar.get(o.eng, 0) < self.bar_id:
                self.eng_bar[o.eng] = self.bar_id
                for (s, v) in self.bar_deps:
                    need[id(s)] = (s, v)
            for p in o.deps:
                s, v, _ = p.sig
                sid = id(s)
                if sid not in need or need[sid][1] < v:
                    need[sid] = (s, v)
            for sid, (s, v) in need.items():
                if w.get(sid, 0) < v:
                    e.wait_ge(s, v)
                    w[sid] = v
                    self.nwait += 1
            ins = o.fn(e)
            if o.sig is not None:
                ins.then_inc(o.sig[0], o.sig[2])
        bd = []
        for e, o in self.last_on.items():
            if o is not None and not o.is_dma and o.sig is not None:
                bd.append((o.sig[0], o.sig[1]))
        for k, s in self.dsem.items():
            bd.append((s, self.dcnt[k]))
        if not had_ops:
            bd = bd + list(self.bar_deps)
        self.bar_deps = bd
        self.bar_id += 1
        for k in list(self.dsem.keys()):
            self.free_by.setdefault(self.dcls[k], []).append((self.dsem[k], self.dcnt[k]))
        self.dcls = {}
        self.dsem = {}
        self.dcnt = {}
        self.ops = []
        self.last_on = {}
        for b in self.bufs:
            b.last_w = None
            b.readers = []
        if final:
            for en in ("sp", "act", "dve", "pool", "pe"):
                e = self.engs[en]
                w = self.waited.setdefault(en, {})
                for (s, v) in bd:
                    if w.get(id(s), 0) < v:
                        e.wait_ge(s, v)
                        w[id(s)] = v


def build_consts():
    c = np.zeros((128, 1024), np.float32)
    c[:, 0:128] = np.eye(128)
    s = np.arange(128)[:, None]
    t = np.arange(128)[None, :]
    same = (s // 64) == (t // 64)
    c[:, 128:256] = (same & (s < t))
    c[:, 256:384] = (same & (s <= t))
    c[:, 384:512] = (same & (s > t))
    c[:, 512:640] = -DS * (same & (s <= t))
    c[:, 640:768] = -DS * (same & (s < t))
    c[:, 768:896] = -DS * (same & (s > t))
    c[63, 896] = 1.0
    c[127, 897] = 1.0
    for i in range(4):
        for sl in range(4):
            c[sl * 30:(sl + 1) * 30, 898 + i * 16 + 4 * i + sl] = 1.0
    c[:, 962] = 1.0
    return c


DECL = set()
KB_S5 = int(os.environ.get('KB_S5', 15))
KB_STAGE = int(os.environ.get('KB_STAGE', 100))


def build_program(stop_after=None):
    nc = bass.Bass("TRN2", target_bir_lowering=False)

    big = ("w_gate", "w_up", "w_down", "cmk", "cmv")
    DECL.clear()

    def din(name, shape):
        if (stop_after in ("A", "B", "C") and name in big) or (stop_after == "D" and name in big[0:3]):
            return None
        DECL.add(name)
        return nc.dram_tensor(name, list(shape), F32, kind="ExternalInput").ap()

    def dout(name, shape):
        return nc.dram_tensor(name, list(shape), F32, kind="ExternalOutput").ap()

    xw = din("xw", [NW * 128, D])
    xs = din("xs", [16, D])
    mem = din("mem", [256, D])
    cconv = din("cconv", [16, 30, CW])
    sshift = din("sshift", [16, SHIFT_W])
    srwkv = din("srwkv", [16, 16, 64, 64])
    cmk = din("cmk", [16, 256, D])
    cmv = din("cmv", [16, 256, D])
    consts = din("consts", [128, 1024])
    norm_mix = din("norm_mix", [D]); w_in = din("w_in", [D, IN_W])
    conv_w = din("conv_w", [31, CW]); conv_b = din("conv_b", [CW])
    conv_ln_g = din("conv_ln_g", [CW]); conv_ln_b = din("conv_ln_b", [CW])
    shift_mu = din("shift_mu", [SHIFT_W])
    w_decay_up = din("w_decay_up", [64, 1024]); decay_bias = din("decay_bias", [1024])
    w_a_up = din("w_a_up", [64, 1024]); a_bias = din("a_bias", [1024])
    w_g_up = din("w_g_up", [160, 1024])
    k_k = din("k_k", [1024]); k_a = din("k_a", [1024]); r_k = din("r_k", [1024])
    lnx_g = din("lnx_g", [1024]); lnx_b = din("lnx_b", [1024])
    w_out = din("w_out", [D, D]); norm_x = din("norm_x", [D]); norm_mem = din("norm_mem", [D])
    w_cq = din("w_cq", [D, D]); w_ck = din("w_ck", [D, D]); w_cv = din("w_cv", [D, D]); w_co = din("w_co", [D, D])
    norm_ffn = din("norm_ffn", [D])
    w_route = din("w_route", [D, 36]); b_route = din("b_route", [36])
    w_gate = din("w_gate", [32, D, 512]); w_up = din("w_up", [32, D, 512]); w_down = din("w_down", [32, 512, D])
    norm_final = din("norm_final", [D])

    y_o = dout("y_o", [1040, D])
    convp_o = dout("convp_o", [30, CW])
    shiftp_o = dout("shiftp_o", [1, SHIFT_W])
    rwkvp_o = dout("rwkvp_o", [16, 64, 64])
    memk_o = dout("memk_o", [256, D])
    memv_o = dout("memv_o", [256, D])
    convs_o = dout("convs_o", [16, 30, CW])
    shifts_o = dout("shifts_o", [16, SHIFT_W])
    rwkvs_o = dout("rwkvs_o", [16, 16, 64, 64])

    q_scr = nc.dram_tensor("q_scr", [1 + NW * 128 + 16, SHIFT_W], F32, kind="Internal").ap()
    xres = nc.dram_tensor("xres", [1040, D], F32, kind="Internal").ap()
    qtok_scr = nc.dram_tensor("qtok_scr", [16, D], F32, kind="Internal").ap()
    bscr = nc.dram_tensor("bscr", [NW * 128 + 16, 2, 5, 512], F32, kind="Internal").ap()

    top = contextlib.ExitStack()
    with top:
        P = Prog(nc, lambda n: top.enter_context(nc.semaphore(n)))

        def MM(out, lhsT, rhs, st, sp, R, W):
            P.op("pe", lambda e: e.matmul(out, lhsT, rhs, start=st, stop=sp), R, W)

        def TR(out, in_, idn, R, W):
            P.op("pe", lambda e: e.transpose(out, in_, idn), R, W)

        def ACT(out, in_, func, R, W, **kw):
            P.op("act", lambda e: e.activation(out, in_, func, **kw), R, W)

        def TT(eng, out, a, b, op, R, W):
            P.op(eng, lambda e: e.tensor_tensor(out, a, b, op), R, W)

        def TS(eng, out, a, s1, s2, op0, op1, R, W):
            if s2 is None:
                P.op(eng, lambda e: e.tensor_scalar(out, a, s1, None, op0), R, W)
            else:
                P.op(eng, lambda e: e.tensor_scalar(out, a, s1, s2, op0, op1), R, W)

        def STT(eng, out, a, s, b, op0, op1, R, W):
            P.op(eng, lambda e: e.scalar_tensor_tensor(out, a, s, b, op0, op1), R, W)

        def CP(eng, out, in_, R, W):
            if eng == "act":
                P.op(eng, lambda e: e.copy(out, in_), R, W)
            else:
                P.op(eng, lambda e: e.tensor_copy(out, in_), R, W)

        def RED(eng, out, in_, op, R, W):
            P.op(eng, lambda e: e.tensor_reduce(out, in_, AX.X, op), R, W)

        def MEMSET(eng, ap, val, W):
            P.op(eng, lambda e: e.memset(ap, val), [], W)

        def RECIP(out, in_, R, W):
            P.op("dve", lambda e: e.reciprocal(out, in_), R, W)

        def RSQRT(ap, R, W):
            ACT(ap, ap, AF.Sqrt, R, W)
            RECIP(ap, ap, R, W)

        def sbt(es, name, shape, dt):
            return es.enter_context(nc.sbuf_tensor(name, list(shape), dt))

        def pst(es, name, shape, dt):
            return es.enter_context(nc.psum_tensor(name, list(shape), dt))

        cst = sbt(top, "cst", [128, 1024], F32); b_cst = P.buf()
        cTb = sbt(top, "cTb", [128, 16, 1040], BF16)
        identb = sbt(top, "identb", [128, 128], BF16)
        ident = cst[:, 0:128]
        mask_ur = cst[:, 128:384]
        mask_sl = cst[:, 384:512]
        tri_le = cst[:, 512:640]; tri_lt = cst[:, 640:768]; tri_gt = cst[:, 768:896]
        esel = cst[:, 896:898]
        ones_col = cst[:, 962:963]
        P.dma("sp", cst[:], consts, [], [b_cst], "cst")
        if os.environ.get("KB_SIMINIT"):
            P.op("dve", lambda e: e.memset(cTb[:], 0.0), [], [b_cst])
        CP("dve", identb[:], ident, [b_cst], [b_cst])
        P.flush()

        rr = [0]

        def rmsnorm_tile(es_bufs, xt, np_, gb, hb, ss, rstd, tmp, R, W, b_tmp):
            ACT(tmp[0:np_, :], xt[0:np_, :], AF.Square, R, [b_tmp], accum_out=ss[0:np_, :])
            TS("dve", rstd[0:np_, :], ss[0:np_, :], 1.0 / D, RMS_EPS, ALU.mult, ALU.add, [b_tmp], [b_tmp])
            RSQRT(rstd[0:np_, :], [b_tmp], [b_tmp])
            STT("dve", hb[0:np_, :], xt[0:np_, :], rstd[0:np_, 0:1], gb[0:np_, :], ALU.mult, ALU.mult,
                R + [b_tmp], W)

        NSLOT = 2
        wring = [None] * NSLOT
        b_ring = [None] * NSLOT
        ring_ctr = [0]
        ring_gen = [0]

        def alloc_ring(es_):
            ring_gen[0] += 1
            for i_ in range(NSLOT):
                wring[i_] = sbt(es_, "wring%d_%d" % (i_, ring_gen[0]), [128, 16, 512], BF16)
                b_ring[i_] = None

        def stream_w(src_ap, kc, ncols):
            i = ring_ctr[0] % NSLOT
            ring_ctr[0] += 1
            if b_ring[i] is None or b_ring[i] not in P.bufs:
                b_ring[i] = P.buf()
            P.dma("pool", wring[i][:, 0:kc, 0:ncols], src_ap.rearrange("(c p) n -> p c n", p=128),
                  [], [b_ring[i]], "ring%d" % i)
            return wring[i], b_ring[i]

        with contextlib.ExitStack() as es:
            uT = sbt(es, "uT", [128, 8, 1072], F32); b_uT = [P.buf() for _ in range(8)]
            esA = contextlib.ExitStack()
            alloc_ring(esA)
            NT = NW * 128 + 16
            hT = sbt(esA, "hT", [128, 16, NT], BF16); b_hT = P.buf()
            gb = sbt(esA, "gb", [128, D], F32); b_gb = P.buf()
            P.dma("sp", gb[:], norm_mix.partition_broadcast(128), [], [b_gb], "gb")
            xt2 = [sbt(esA, "xt0", [128, D], F32)] * 2
            b_xt = [P.buf()] * 2
            hb = sbt(esA, "hb", [128, D], BF16); b_hb = P.buf()
            ss = sbt(esA, "ss", [128, 1], F32); rstd = sbt(esA, "rstd", [128, 1], F32); b_tmp = P.buf()
            pT = [pst(esA, "pT%d" % i, [128, 8, 128], BF16) for i in range(2)]
            b_pT = [P.buf() for _ in range(2)]
            zrow = sbt(esA, "zrow", [105, 32], F32); b_z = P.buf()
            MEMSET("dve", zrow[:], 0.0, [b_z])
            P.dma("sp", q_scr[0, :].rearrange("(p n) -> p n", n=32), zrow[:], [b_z], [], "zrow")
            for i in range(NW + 1):
                np_ = 128 if i < NW else 16
                xt = xt2[i % 2]; bx = b_xt[i % 2]
                src = xw[i * 128:(i + 1) * 128, :] if i < NW else xs
                P.dma("sp" if i % 2 == 0 else "act", xt[0:np_, :], src, [], [bx], "xl0")
                rmsnorm_tile(es, xt, np_, gb, hb, ss, rstd, hb, [bx, b_gb], [b_hb], b_hb)
                for hf in range(2):
                    for j in range(8):
                        dc = hf * 8 + j
                        TR(pT[hf][0:128, j, 0:np_], hb[0:np_, dc * 128:(dc + 1) * 128], identb[0:np_, 0:np_],
                           [b_hb, b_cst], [b_pT[hf]])
                    CP("act" if hf == 0 else "dve", hT[:, hf * 8:(hf + 1) * 8, i * 128:i * 128 + np_],
                       pT[hf][:, :, 0:np_], [b_pT[hf]], [b_hT])
            pq = [pst(esA, "pq%d" % i, [128, 512], F32) for i in range(3)]
            b_pq = [P.buf() for _ in range(3)]
            qst = [sbt(esA, "qst%d" % i, [128, 512], F32) for i in range(3)]
            b_qst = [P.buf() for _ in range(3)]
            cnt = 0
            for cb in range(7):
                c0 = 2048 + cb * 512
                ncol = min(512, IN_W - c0)
                slot, bs = stream_w(w_in[:, c0:c0 + ncol], 16, ncol)
                for i in range(NW + 1):
                    np_ = 128 if i < NW else 16
                    k = cnt % 3
                    cnt += 1
                    for dc in range(16):
                        MM(pq[k][0:np_, 0:ncol], hT[:, dc, i * 128:i * 128 + np_], slot[:, dc, 0:ncol],
                           dc == 0, dc == 15, [b_hT, bs], [b_pq[k]])
                    CP("act" if cnt % 2 == 0 else "dve", qst[k][0:np_, 0:ncol], pq[k][0:np_, 0:ncol],
                       [b_pq[k]], [b_qst[k]])
                    P.dma("sp", q_scr[1 + i * 128:1 + i * 128 + np_, c0 - 2048:c0 - 2048 + ncol],
                          qst[k][0:np_, 0:ncol], [b_qst[k]], [], "qst%d" % k)
            chunks = [(992, 512), (1504, 512), (2016, 48)]
            sg = [sbt(esA, "sg%d" % i, [128, 512], F32) for i in range(2)]
            b_sg = [P.buf() for _ in range(2)]
            for cb in range(4):
                slot, bs = stream_w(w_in[:, cb * 512:(cb + 1) * 512], 16, 512)
                for fb in range(4):
                    fblk = cb * 4 + fb
                    for (n0, nn) in chunks:
                        k = cnt % 3
                        cnt += 1
                        for dc in range(16):
                            MM(pq[k][:, 0:nn], slot[:, dc, fb * 128:(fb + 1) * 128], hT[:, dc, n0:n0 + nn],
                               dc == 0, dc == 15, [b_hT, bs], [b_pq[k]])
                        if fblk < 8:
                            CP("dve", uT[:, fblk, n0 - 992:n0 - 992 + nn], pq[k][:, 0:nn], [b_pq[k]], [b_uT[fblk]])
                        else:
                            ACT(sg[k % 2][:, 0:nn], pq[k][:, 0:nn], AF.Sigmoid, [b_pq[k]], [b_sg[k % 2]])
                            TT("dve", uT[:, fblk - 8, n0 - 992:n0 - 992 + nn], uT[:, fblk - 8, n0 - 992:n0 - 992 + nn],
                               sg[k % 2][:, 0:nn], ALU.mult, [b_sg[k % 2], b_uT[fblk - 8]], [b_uT[fblk - 8]])
            P.flush()
            esA.close()
            P.dma("sp", shiftp_o, q_scr[NW * 128:NW * 128 + 1, :], [], [], "sho")
            P.dma("sp", shifts_o, q_scr[1 + NW * 128:1 + NW * 128 + 16, :], [], [], "sho")

            with contextlib.ExitStack() as es2:
                cw_tm = sbt(es2, "cw_tm", [31, CW], F32); b_cw = P.buf()
                P.dma("sp", cw_tm[:], conv_w, [], [b_cw], "cw")
                pv = sbt(es2, "pv", [24, 128], F32); b_pv = P.buf()
                P.dma("sp", pv[0:8, :], conv_b.rearrange("(b p) -> b p", p=128), [], [b_pv], "cw")
                P.dma("sp", pv[8:16, :], conv_ln_g.rearrange("(b p) -> b p", p=128), [], [b_pv], "cw")
                P.dma("sp", pv[16:24, :], conv_ln_b.rearrange("(b p) -> b p", p=128), [], [b_pv], "cw")
                cwT = sbt(es2, "cwT", [128, 8, 31], F32); b_cwT = P.buf()
                pvT = sbt(es2, "pvT", [128, 24], F32)
                pc = pst(es2, "pc", [128, 8, 32], F32); b_pc = P.buf()
                pc2 = pst(es2, "pc2", [128, 32], F32); b_pc2 = P.buf()
                for blk in range(8):
                    TR(pc[:, blk, 0:31], cw_tm[:, blk * 128:(blk + 1) * 128], ident[0:31, 0:31], [b_cw, b_cst], [b_pc])
                CP("dve", cwT[:], pc[:, :, 0:31], [b_pc], [b_cwT])
                TR(pc2[:, 0:24], pv[:], ident[0:24, 0:24], [b_pv, b_cst], [b_pc2])
                CP("dve", pvT[:], pc2[:, 0:24], [b_pc2], [b_cwT])
                cT = sbt(es2, "cT", [128, 8, 1024], F32); b_cT = [P.buf() for _ in range(8)]
                for blk in range(8):
                    eng = "dve"
                    TS(eng, cT[:, blk, :], uT[:, blk, 2:1026], cwT[:, blk, 0:1], pvT[:, blk:blk + 1], ALU.mult, ALU.add,
                       [b_uT[blk], b_cwT], [b_cT[blk]])
                    for j in range(1, 31):
                        STT(eng, cT[:, blk, :], uT[:, blk, 2 + j:1026 + j], cwT[:, blk, j:j + 1], cT[:, blk, :],
                            ALU.mult, ALU.add, [b_uT[blk], b_cwT, b_cT[blk]], [b_cT[blk]])
                urow = sbt(es2, "urow", [30, CW], F32); b_urow = P.buf()
                us = sbt(es2, "us", [16, CW], F32); b_us = P.buf()
                pu = pst(es2, "pu", [32, 1024], F32); b_pu = P.buf()
                for blk in range(8):
                    TR(pu[0:30, blk * 128:(blk + 1) * 128], uT[:, blk, 1026:1056], ident, [b_uT[blk], b_cst], [b_pu])
                CP("act", urow[:], pu[0:30, :], [b_pu], [b_urow])
                P.dma("sp", convp_o, urow[:], [b_urow], [], "cpo")
                for blk in range(8):
                    TR(pu[0:16, blk * 128:(blk + 1) * 128], uT[:, blk, 1056:1072], ident, [b_uT[blk], b_cst], [b_pu])
                CP("act", us[:], pu[0:16, :], [b_pu], [b_us])
                P.dma("sp", convs_o[:, 29, :], us[:], [b_us], [], "cso")
                P.dma("act", convs_o[:, 0:29, :], cconv[:, 1:30, :], [], [], "cso2")
                onesm = sbt(es2, "onesm", [128, 128], F32); b_ones = P.buf()
                MEMSET("dve", onesm[:], 1.0 / CW, [b_ones])
                ps1 = pst(es2, "ps1", [128, 512], F32); b_ps1 = P.buf()
                ps2 = pst(es2, "ps2", [128, 512], F32); b_ps2 = P.buf()
                sqc = [sbt(es2, "sqc%d" % i, [128, 512], F32) for i in range(2)]
                b_sqc = [P.buf() for _ in range(2)]
                mean = sbt(es2, "mean", [128, 512], F32); rs = sbt(es2, "rs", [128, 512], F32); b_st = P.buf()
                tn = [sbt(es2, "tn%d" % i, [128, 512], F32) for i in range(2)]
                b_tn = [P.buf() for _ in range(2)]
                b_cTb = P.buf()
                for ncx in range(2):
                    n0 = ncx * 512
                    for blk in range(8):
                        MM(ps1[:], onesm[:], cT[:, blk, n0:n0 + 512], blk == 0, blk == 7, [b_ones, b_cT[blk]], [b_ps1])
                        ACT(sqc[blk % 2][:], cT[:, blk, n0:n0 + 512], AF.Square, [b_cT[blk]], [b_sqc[blk % 2]])
                        MM(ps2[:], onesm[:], sqc[blk % 2][:], blk == 0, blk == 7, [b_ones, b_sqc[blk % 2]], [b_ps2])
                    CP("dve", mean[:], ps1[:], [b_ps1], [b_st])
                    TT("dve", rs[:], mean[:], mean[:], ALU.mult, [b_st], [b_st])
                    TT("dve", rs[:], ps2[:], rs[:], ALU.subtract, [b_ps2, b_st], [b_st])
                    TS("dve", rs[:], rs[:], LN_EPS, None, ALU.add, None, [b_st], [b_st])
                    RSQRT(rs[:], [b_st], [b_st])
                    for blk in range(8):
                        t = tn[blk % 2]; bt = b_tn[blk % 2]
                        TT("dve", t[:], cT[:, blk, n0:n0 + 512], mean[:], ALU.subtract, [b_cT[blk], b_st], [bt])
                        TT("dve", t[:], t[:], rs[:], ALU.mult, [bt, b_st], [bt])
                        ACT(cTb[:, blk, n0:n0 + 512], t[:], AF.Silu, [bt, b_cwT], [b_cTb],
                            bias=pvT[:, 16 + blk:17 + blk], scale=pvT[:, 8 + blk:9 + blk])
                wrep = sbt(es2, "wrep", [120, CW], F32); b_wrep = P.buf()
                for r4 in range(4):
                    P.dma("sp", wrep[r4 * 30:(r4 + 1) * 30, :], conv_w[0:30, :], [], [b_wrep], "wrep")
                bc16 = sbt(es2, "bc16", [16, 4, CW], F32); b_bc16 = P.buf()
                P.dma("sp", bc16[:, 0, :], conv_w[30, :].partition_broadcast(16), [], [b_bc16], "bc16")
                P.dma("sp", bc16[:, 1, :], conv_b.partition_broadcast(16), [], [b_bc16], "bc16")
                P.dma("sp", bc16[:, 2, :], conv_ln_g.partition_broadcast(16), [], [b_bc16], "bc16")
                P.dma("sp", bc16[:, 3, :], conv_ln_b.partition_broadcast(16), [], [b_bc16], "bc16")
                cch = [sbt(es2, "cch%d" % i, [120, CW], F32) for i in range(2)]
                b_cch = [P.buf() for _ in range(2)]
                pcs = pst(es2, "pcs", [16, 1024], F32); b_pcs = P.buf()
                for i4 in range(4):
                    t = cch[i4 % 2]; bt = b_cch[i4 % 2]
                    P.dma("sp", t[:], cconv[i4 * 4:(i4 + 1) * 4, :, :].rearrange("s j c -> (s j) c"), [], [bt], "cch%d" % (i4 % 2))
                    TT("dve", t[:], t[:], wrep[:], ALU.mult, [bt, b_wrep], [bt])
                    for hf in range(2):
                        MM(pcs[:, hf * 512:(hf + 1) * 512], cst[0:120, 898 + i4 * 16:898 + (i4 + 1) * 16],
                           t[:, hf * 512:(hf + 1) * 512], i4 == 0, i4 == 3, [bt, b_cst], [b_pcs])
                cs = sbt(es2, "cs", [16, CW], F32); b_cs = P.buf()
                cs2 = sbt(es2, "cs2", [16, CW], F32)
                st16 = sbt(es2, "st16", [16, 4], F32)
                TT("dve", cs[:], us[:], bc16[:, 0, :], ALU.mult, [b_us, b_bc16], [b_cs])
                TT("dve", cs[:], cs[:], pcs[:], ALU.add, [b_cs, b_pcs], [b_cs])
                TT("dve", cs[:], cs[:], bc16[:, 1, :], ALU.add, [b_cs, b_bc16], [b_cs])
                ACT(cs2[:], cs[:], AF.Copy, [b_cs], [b_cs], accum_out=st16[:, 0:1])
                TS("dve", st16[:, 0:1], st16[:, 0:1], 1.0 / CW, None, ALU.mult, None, [b_cs], [b_cs])
                TS("dve", cs[:], cs[:], st16[:, 0:1], None, ALU.subtract, None, [b_cs], [b_cs])
                ACT(cs2[:], cs[:], AF.Square, [b_cs], [b_cs], accum_out=st16[:, 1:2])
                TS("dve", st16[:, 1:2], st16[:, 1:2], 1.0 / CW, LN_EPS, ALU.mult, ALU.add, [b_cs], [b_cs])
                RSQRT(st16[:, 1:2], [b_cs], [b_cs])
                STT("dve", cs[:], cs[:], st16[:, 1:2], bc16[:, 2, :], ALU.mult, ALU.mult, [b_cs, b_bc16], [b_cs])
                TT("dve", cs[:], cs[:], bc16[:, 3, :], ALU.add, [b_cs, b_bc16], [b_cs])
                ACT(cs2[:], cs[:], AF.Silu, [b_cs], [b_cs])
                for blk in range(8):
                    TR(pc[:, blk, 0:16], cs2[:, blk * 128:(blk + 1) * 128], ident[0:16, 0:16], [b_cs, b_cst], [b_pc])
                CP("dve", cTb[:, 0:8, 1024:1040], pc[:, :, 0:16], [b_pc], [b_cTb])
                P.flush()
        if stop_after == "A":
            P.flush(final=True)
            return nc

        def load_bc(es_, name, src, n, np_=128):
            t = sbt(es_, name, [np_, n], F32)
            b = P.buf()
            P.dma("sp", t[:], src.partition_broadcast(np_), [], [b], "bc")
            return t, b

        with contextlib.ExitStack() as es:
            mu_b, b_mu = load_bc(es, "mu_b", shift_mu, SHIFT_W)
            kk_b, b_par = load_bc(es, "kk_b", k_k, 1024)
            ka_b, _b = load_bc(es, "ka_b", k_a, 1024); rk_b, _b2 = load_bc(es, "rk_b", r_k, 1024)
            lg_b, _b3 = load_bc(es, "lg_b", lnx_g, 1024); lb_b, _b4 = load_bc(es, "lb_b", lnx_b, 1024)
            b_pars = [b_par, _b, _b2, _b3, _b4]
            wdu = sbt(es, "wdu", [65, 1024], F32); wau = sbt(es, "wau", [65, 1024], F32)
            wgu = sbt(es, "wgu", [128, 2, 1024], F32); b_lw = P.buf()
            P.dma("sp", wdu[0:64, :], w_decay_up, [], [b_lw], "lw")
            P.dma("sp", wdu[64:65, :], decay_bias.rearrange("(o n) -> o n", o=1), [], [b_lw], "lw")
            P.dma("sp", wau[0:64, :], w_a_up, [], [b_lw], "lw")
            P.dma("sp", wau[64:65, :], a_bias.rearrange("(o n) -> o n", o=1), [], [b_lw], "lw")
            P.dma("sp", wgu[:, 0, :], w_g_up[0:128, :], [], [b_lw], "lw")
            P.dma("sp", wgu[0:32, 1, :], w_g_up[128:160, :], [], [b_lw], "lw")
            q = sbt(es, "q", [128, SHIFT_W], F32); b_q = P.buf()
            qp = sbt(es, "qp", [128, SHIFT_W], F32); b_qp = P.buf()
            Wt_full = [sbt(es, "W%d" % i, [128, 1024], F32) for i in range(3, 7)]
            Wt = [qp[:, 0:1024], qp[:, 1024:2048], qp[:, 2048:3072]] + [w_[:] for w_ in Wt_full]
            b_W = [b_qp, b_qp, b_qp] + [P.buf() for _ in range(4)]
            loT = sbt(es, "loT", [128, 4, 128], F32); b_loT = P.buf()
            MEMSET("dve", loT[:], 1.0, [b_loT])
            lo_in = sbt(es, "lo_in", [128, 288], F32); b_loin = P.buf()
            sm = sbt(es, "sm", [128, 64], F32); b_sm = P.buf()
            Ot = Wt[3]; b_Ot = b_W[3]
            S = sbt(es, "S", [128, 8, 64], F32); b_S = P.buf()
            MEMSET("dve", S[:], 0.0, [b_S])
            tA = sbt(es, "tA", [128, 8, 64], F32); b_tA = P.buf()
            tB = sbt(es, "tB", [128, 8, 64], F32); b_tB = P.buf()
            tC = sbt(es, "tC", [128, 8, 64], F32); b_tC = P.buf()
            tD = [sbt(es, "tD%d" % i, [128, 8, 64], F32) for i in range(2)]; b_tD = [P.buf() for _ in range(2)]
            sa = sbt(es, "sa", [128, 8], F32); b_sa = P.buf()
            vT = sbt(es, "vT", [128, 8, 128], F32); b_vT = P.buf()
            oT = sbt(es, "oT", [128, 8, 128], F32); b_oT = P.buf()
            NB = 3
            bc = [sbt(es, "bc%d" % i, [128, 2560], F32) for i in range(NB)]; b_bc = [P.buf() for _ in range(NB)]
            b_scr = P.buf()
            bk = [pst(es, "bk%d" % i, [128, 512], F32) for i in range(8)]
            b_bk = [P.buf() for _ in range(8)]
            pL = [bk[0], bk[1]]; b_pL = [b_bk[0], b_bk[1]]
            pA = [bk[3], bk[4]]; b_pA = [b_bk[3], b_bk[4]]
            pB = [bk[2][:].rearrange("p (a b) -> p a b", b=128), bk[5][:].rearrange("p (a b) -> p a b", b=128)]
            b_pB = [b_bk[2], b_bk[5]]
            vps = [bk[6][:].rearrange("p (a b) -> p a b", b=128), bk[7][:].rearrange("p (a b) -> p a b", b=128)]

            def vec_part(np_, own):
                r_ = slice(0, np_)
                TT("dve", qp[r_, :], qp[r_, :], q[r_, :], ALU.subtract, [b_qp, b_q], [b_qp])
                TT("pool", qp[r_, :], qp[r_, :], mu_b[r_, :], ALU.mult, [b_qp, b_mu], [b_qp])
                TT("dve", q[r_, :], q[r_, :], qp[r_, :], ALU.add, [b_qp, b_q], [b_q])
                ACT(lo_in[r_, 0:64], q[r_, 3072:3136], AF.Tanh, [b_q], [b_loin])
                CP("act", lo_in[r_, 64:128], q[r_, 3136:3200], [b_q], [b_loin])
                ACT(lo_in[r_, 128:288], q[r_, 3200:3360], AF.Sigmoid, [b_q], [b_loin])
                k = 0
                TR(pB[k][0:64, 0, 0:np_], lo_in[r_, 0:64], ident[r_, r_], [b_loin, b_cst], [b_pB[k]])
                TR(pB[k][0:64, 1, 0:np_], lo_in[r_, 64:128], ident[r_, r_], [b_loin, b_cst], [b_pB[k]])
                TR(pB[k][0:128, 2, 0:np_], lo_in[r_, 128:256], ident[r_, r_], [b_loin, b_cst], [b_pB[k]])
                TR(pB[k][0:32, 3, 0:np_], lo_in[r_, 256:288], ident[r_, r_], [b_loin, b_cst], [b_pB[k]])
                CP("dve", loT[0:64, 0:2, 0:np_], pB[k][0:64, 0:2, 0:np_], [b_pB[k]], [b_loT])
                CP("dve", loT[:, 2, 0:np_], pB[k][:, 2, 0:np_], [b_pB[k]], [b_loT])
                CP("dve", loT[0:32, 3, 0:np_], pB[k][0:32, 3, 0:np_], [b_pB[k]], [b_loT])
                for hf in range(2):
                    c = slice(hf * 512, (hf + 1) * 512)
                    MM(pL[0][r_, :], loT[0:65, 0, r_], wdu[0:65, c], True, True, [b_loT, b_lw], [b_pL[0]])
                    ACT(Wt[0][r_, c], pL[0][r_, :], AF.Sigmoid, [b_pL[0]], [b_W[0]])
                    MM(pL[1][r_, :], loT[0:65, 1, r_], wau[0:65, c], True, True, [b_loT, b_lw], [b_pL[1]])
                    ACT(Wt[1][r_, c], pL[1][r_, :], AF.Sigmoid, [b_pL[1]], [b_W[1]])
                    if own:
                        MM(pA[hf][r_, :], loT[0:128, 2, r_], wgu[:, 0, c], True, False, [b_loT, b_lw], [b_pA[hf]])
                        MM(pA[hf][r_, :], loT[0:32, 3, r_], wgu[0:32, 1, c], False, True, [b_loT, b_lw], [b_pA[hf]])
                        CP("act", Wt[2][r_, c], pA[hf][r_, :], [b_pA[hf]], [b_W[2]])
                kv = q[r_, 1024:2048]; rv = q[r_, 0:1024]
                a = Wt[1][r_, :]
                TT("dve", Wt[3][r_, :], kv, kk_b[r_, :], ALU.mult, [b_q] + b_pars, [b_W[3]])
                TT("pool", Wt[5][r_, :], Wt[3][r_, :], Wt[3][r_, :], ALU.mult, [b_W[3]], [b_W[5]])
                RED("dve", sm[r_, 16:32], Wt[5][r_, :].rearrange("p (h k) -> p h k", k=64), ALU.add, [b_W[5]], [b_sm])
                ACT(sm[r_, 16:32], sm[r_, 16:32], AF.Sqrt, [b_sm], [b_sm])
                TS("dve", sm[r_, 16:32], sm[r_, 16:32], 1e-12, None, ALU.max, None, [b_sm], [b_sm])
                RECIP(sm[r_, 16:32], sm[r_, 16:32], [b_sm], [b_sm])
                TT("dve", Wt[3][r_, :].rearrange("p (h k) -> p h k", k=64), Wt[3][r_, :].rearrange("p (h k) -> p h k", k=64),
                   sm[r_, 16:32].unsqueeze(2).broadcast_to([np_, 16, 64]), ALU.mult, [b_W[3], b_sm], [b_W[3]])
                TT("pool", Wt[4][r_, :], Wt[3][r_, :], a, ALU.mult, [b_W[3], b_W[1]], [b_W[4]])
                STT("dve", Wt[5][r_, :], a, -1.0, ka_b[r_, :], ALU.add, ALU.mult, [b_W[1]] + b_pars, [b_W[5]])
                STT("dve", Wt[5][r_, :], Wt[5][r_, :], 1.0, kv, ALU.add, ALU.mult, [b_W[5], b_q], [b_W[5]])
                if own:
                    TT("pool", Wt[6][r_, :], rv, rk_b[r_, :], ALU.mult, [b_q] + b_pars, [b_W[6]])
                    TT("pool", Wt[6][r_, :], Wt[6][r_, :], Wt[5][r_, :], ALU.mult, [b_W[6], b_W[5]], [b_W[6]])
                    RED("dve", sm[r_, 0:16], Wt[6][r_, :].rearrange("p (h k) -> p h k", k=64), ALU.add, [b_W[6]], [b_sm])

            def out_part(np_, col0):
                r_ = slice(0, np_)
                O3 = Ot[r_, :].rearrange("p (h k) -> p h k", k=64)
                RED("dve", sm[r_, 32:48], O3, ALU.add, [b_Ot], [b_sm])
                TS("dve", sm[r_, 32:48], sm[r_, 32:48], 1.0 / 64, None, ALU.mult, None, [b_sm], [b_sm])
                TT("dve", O3, O3, sm[r_, 32:48].unsqueeze(2).broadcast_to([np_, 16, 64]), ALU.subtract, [b_Ot, b_sm], [b_Ot])
                TT("pool", Wt[6][r_, :], Ot[r_, :], Ot[r_, :], ALU.mult, [b_Ot], [b_W[6]])
                RED("dve", sm[r_, 48:64], Wt[6][r_, :].rearrange("p (h k) -> p h k", k=64), ALU.add, [b_W[6]], [b_sm])
                TS("dve", sm[r_, 48:64], sm[r_, 48:64], 1.0 / 64, GN_EPS, ALU.mult, ALU.add, [b_sm], [b_sm])
                RSQRT(sm[r_, 48:64], [b_sm], [b_sm])
                TT("dve", O3, O3, sm[r_, 48:64].unsqueeze(2).broadcast_to([np_, 16, 64]), ALU.mult, [b_Ot, b_sm], [b_Ot])
                TT("pool", Ot[r_, :], Ot[r_, :], lg_b[r_, :], ALU.mult, [b_Ot] + b_pars, [b_Ot])
                TT("dve", Ot[r_, :], Ot[r_, :], lb_b[r_, :], ALU.add, [b_Ot] + b_pars, [b_Ot])
                V3 = q[r_, 2048:3072].rearrange("p (h k) -> p h k", k=64)
                W63 = Wt[6][r_, :].rearrange("p (h k) -> p h k", k=64)
                TT("dve", W63, V3, sm[r_, 0:16].unsqueeze(2).broadcast_to([np_, 16, 64]), ALU.mult, [b_q, b_sm], [b_W[6]])
                TT("dve", Ot[r_, :], Ot[r_, :], Wt[6][r_, :], ALU.add, [b_Ot, b_W[6]], [b_Ot])
                TT("dve", Ot[r_, :], Ot[r_, :], Wt[2][r_, :], ALU.mult, [b_Ot, b_W[2]], [b_Ot])
                for hf in range(2):
                    for j in range(4):
                        blk = hf * 4 + j
                        TR(pB[hf][:, j, 0:np_], Ot[r_, blk * 128:(blk + 1) * 128], ident[r_, r_], [b_Ot, b_cst], [b_pB[hf]])
                    CP("act", cTb[:, 8 + hf * 4:12 + hf * 4, col0:col0 + np_], pB[hf][:, :, 0:np_], [b_pB[hf]], [b_cTbB])

            b_cTbB = P.buf()
            NTL = int(os.environ.get('KB_TILES', NW))
            for i in list(range(NTL)) + [NW]:
                samp = (i == NW)
                np_ = 16 if samp else 128
                own = samp or i >= NW - NOWN
                r_ = slice(0, np_)
                if samp:
                    P.dma("sp", q[r_, :], q_scr[1 + NW * 128:1 + NW * 128 + 16, :], [], [b_q], "ql")
                    P.dma("act", qp[r_, :], sshift, [], [b_qp], "qpl")
                else:
                    P.dma("sp", q[:], q_scr[1 + i * 128:1 + (i + 1) * 128, :], [], [b_q], "ql")
                    P.dma("act", qp[:], q_scr[i * 128:(i + 1) * 128, :], [], [b_qp], "qpl")
                vec_part(np_, own)
                ACT(Wt[6][r_, :], Wt[0][r_, :], AF.Exp, [b_W[0], b_sm], [b_W[6]], scale=-DS)
                srcs = [(Wt[6], b_W[6]), (Wt[3], b_W[3]), (Wt[4], b_W[4]), (Wt[5], b_W[5]), (q[:, 0:1024], b_q)]
                for f, (src, bs) in enumerate(srcs):
                    for hp in range(2):
                        P.dma("sp" if hp == 0 else "act",
                              bscr[i * 128:i * 128 + np_, hp, f, :].rearrange("t (j k) -> t j k", k=64),
                              src[r_, :].rearrange("p (j hp k) -> p hp j k", hp=2, k=64)[:, hp],
                              [bs], [b_scr], "scr")
                for j in range(8):
                    TR(vps[j // 4][:, j % 4, 0:np_], q[r_, 2048 + j * 128:2048 + (j + 1) * 128], ident[r_, r_],
                       [b_q, b_cst], [b_bk[6 + j // 4]])
                CP("act", vT[:, 0:4, 0:np_], vps[0][:, :, 0:np_], [b_bk[6]], [b_vT])
                CP("dve", vT[:, 4:8, 0:np_], vps[1][:, :, 0:np_], [b_bk[7]], [b_vT])
                for t in range(np_):
                    g = i * 128 + t
                    kb = g % NB
                    B_ = bc[kb]; bB = b_bc[kb]
                    for hp in range(2):
                        P.dma("sp", B_[hp * 64:(hp + 1) * 64, :],
                              bscr[g, hp].rearrange("f n -> (f n)").partition_broadcast(64),
                              [b_scr], [bB], "bc%d" % kb)
                    if samp:
                        for hp in range(2):
                            P.dma("act", S[hp * 64:(hp + 1) * 64, :, :],
                                  srwkv[t].rearrange("(j hp) v k -> hp v j k", hp=2)[hp], [], [b_S], "Sl")
                    fv = lambda f: B_[:, f * 512:(f + 1) * 512].rearrange("p (j k) -> p j k", k=64)
                    td = tD[g % 2]; btd = b_tD[g % 2]
                    TT("pool", td[:], fv(3), vT[:, :, t].unsqueeze(2).broadcast_to([128, 8, 64]), ALU.mult, [bB, b_vT], [btd])
                    TT("dve", tA[:], S[:], fv(1), ALU.mult, [b_S, bB], [b_tA])
                    RED("dve", sa[:], tA[:], ALU.add, [b_tA], [b_sa])
                    TT("dve", S[:], S[:], fv(0), ALU.mult, [b_S, bB], [b_S])
                    TT("dve", tB[:], fv(2), sa[:].unsqueeze(2).broadcast_to([128, 8, 64]), ALU.mult, [bB, b_sa], [b_tB])
                    TT("dve", S[:], S[:], tB[:], ALU.subtract, [b_S, b_tB], [b_S])
                    TT("dve", S[:], S[:], td[:], ALU.add, [b_S, btd], [b_S])
                    if own:
                        TT("dve", tC[:], S[:], fv(4), ALU.mult, [b_S, bB], [b_tC])
                        RED("dve", oT[:, :, t], tC[:], ALU.add, [b_tC], [b_oT])
                    if samp:
                        for hp in range(2):
                            P.dma("sp", rwkvs_o[t].rearrange("(j hp) v k -> hp v j k", hp=2)[hp],
                                  S[hp * 64:(hp + 1) * 64, :, :], [b_S], [], "So")
                if i == NW - 1:
                    for hp in range(2):
                        P.dma("sp", rwkvp_o.rearrange("(j hp) v k -> hp v j k", hp=2)[hp],
                              S[hp * 64:(hp + 1) * 64, :, :], [b_S], [], "So")
                if own:
                    for j in range(8):
                        TR(vps[j // 4][0:np_, j % 4, :], oT[:, j, 0:np_], ident, [b_oT, b_cst], [b_bk[6 + j // 4]])
                    CP("act", Ot[r_, 0:512], bk[6][r_, :], [b_bk[6]], [b_Ot])
                    CP("dve", Ot[r_, 512:1024], bk[7][r_, :], [b_bk[7]], [b_Ot])
                    out_part(np_, 1024 if samp else (i - (NW - NOWN)) * 128)
                P.flush()
        if stop_after == "B":
            P.flush(final=True)
            return nc

        uid = [0]

        def un(n):
            uid[0] += 1
            return "%s_%d" % (n, uid[0])

        TILES = [(i, 128, i * 128) for i in range(8)] + [(8, 16, 1024)]
        ATT_SCALE = 512.0 ** -0.5

        def x_src(ti):
            return xw[1024 + ti * 128:1024 + (ti + 1) * 128, :] if ti < 8 else xs

        def xres_src(ti):
            (_, np_, c0) = TILES[ti]
            return xres[c0:c0 + np_, :]

        def proj_res(w, src_fn):
            with contextlib.ExitStack() as es_:
                alloc_ring(es_)
                pq = [pst(es_, un("pq"), [128, 512], F32) for _ in range(3)]; b_pq = [P.buf() for _ in range(3)]
                xin = [sbt(es_, un("xin"), [128, 512], F32) for _ in range(3)]; b_xin = [P.buf() for _ in range(3)]
                xo = [sbt(es_, un("xo"), [128, 512], F32) for _ in range(3)]; b_xo = [P.buf() for _ in range(3)]
                cnt = 0
                for cb in range(4):
                    slot, bs = stream_w(w[:, cb * 512:(cb + 1) * 512], 16, 512)
                    for (ti, np_, c0) in TILES:
                        k = cnt % 3
                        cnt += 1
                        P.dma("sp", xin[k][0:np_, :], src_fn(ti)[:, cb * 512:(cb + 1) * 512], [], [b_xin[k]], "xin")
                        for dc in range(16):
                            MM(pq[k][0:np_, :], cTb[:, dc, c0:c0 + np_], slot[:, dc, :], dc == 0, dc == 15, [bs], [b_pq[k]])
                        TT("dve", xo[k][0:np_, :], pq[k][0:np_, :], xin[k][0:np_, :], ALU.add, [b_pq[k], b_xin[k]], [b_xo[k]])
                        P.dma("act", xres[c0:c0 + np_, cb * 512:(cb + 1) * 512], xo[k][0:np_, :], [b_xo[k]], [], "xo%d" % k)
                P.flush()

        def norm_T(es_, src_list, g_dram, dst, route=None):
            gb = sbt(es_, un("gb"), [128, D], F32); b_gb = P.buf()
            P.dma("sp", gb[:], g_dram.partition_broadcast(128), [], [b_gb], "gb")
            xt2 = [sbt(es_, un("xt"), [128, D], F32) for _ in range(2)]; b_xt = [P.buf(), P.buf()]
            hb = sbt(es_, un("hb"), [128, D], BF16); b_hb = P.buf()
            ss = sbt(es_, un("ss"), [128, 1], F32); rstd = sbt(es_, un("rstd"), [128, 1], F32)
            pT = [pst(es_, un("pT"), [128, 8, 128], BF16) for _ in range(2)]; b_pT = [P.buf(), P.buf()]
            b_dst = P.buf()
            for n_, (src, np_, c0) in enumerate(src_list):
                xt = xt2[n_ % 2]; bx = b_xt[n_ % 2]
                P.dma("sp" if n_ % 2 == 0 else "act", xt[0:np_, :], src, [], [bx], "xl")
                rmsnorm_tile(None, xt, np_, gb, hb, ss, rstd, hb, [bx, b_gb], [b_hb], b_hb)
                for hf in range(2):
                    for j in range(8):
                        dc = hf * 8 + j
                        TR(pT[hf][0:128, j, 0:np_], hb[0:np_, dc * 128:(dc + 1) * 128], identb[0:np_, 0:np_],
                           [b_hb, b_cst], [b_pT[hf]])
                    CP("act" if hf == 0 else "dve", dst[:, hf * 8:(hf + 1) * 8, c0:c0 + np_], pT[hf][:, :, 0:np_],
                       [b_pT[hf]], [b_dst])
                if route is not None:
                    route(n_, np_, xt, bx, rstd, gb, b_gb, b_hb)

        proj_res(w_out, x_src)
        if stop_after == "C":
            P.flush(final=True)
            return nc

        with contextlib.ExitStack() as esD:
            qT = sbt(esD, "qT", [128, 16, 1040], BF16); b_qT = P.buf()
            kT = sbt(esD, "kT", [128, 16, 256], BF16); b_kT = P.buf()
            Vb = sbt(esD, "Vb", [128, 2, 2048], BF16); b_Vb = P.buf()
            with contextlib.ExitStack() as esa:
                norm_T(esa, [(xres_src(ti), np_, c0) for (ti, np_, c0) in TILES], norm_x, cTb)
                P.flush()
            with contextlib.ExitStack() as es1:
                mnT = sbt(es1, "mnT", [128, 16, 256], BF16)
                with contextlib.ExitStack() as esb:
                    norm_T(esb, [(mem[mt * 128:(mt + 1) * 128, :], 128, mt * 128) for mt in range(2)], norm_mem, mnT)
                    P.flush()
                alloc_ring(es1)
                pq = [pst(es1, un("pq"), [128, 512], F32) for _ in range(3)]; b_pq = [P.buf() for _ in range(3)]
                qtok = sbt(es1, "qtok", [16, D], F32); b_qtok = P.buf()
                stg = [sbt(es1, un("stg"), [128, 512], F32) for _ in range(2)]; b_stg = [P.buf(), P.buf()]
                cnt = 0
                P23 = os.environ.get("KB_P23", "qtkv")
                for cb in range(4 if "q" in P23 else 0):
                    slot, bs = stream_w(w_cq[:, cb * 512:(cb + 1) * 512], 16, 512)
                    for fb in range(4):
                        for (n0, nn) in [(0, 512), (512, 512), (1024, 16)]:
                            k = cnt % 3
                            cnt += 1
                            for dc in range(16):
                                MM(pq[k][:, 0:nn], slot[:, dc, fb * 128:(fb + 1) * 128], cTb[:, dc, n0:n0 + nn],
                                   dc == 0, dc == 15, [bs], [b_pq[k]])
                            CP("act" if cnt % 2 == 0 else "dve", qT[:, cb * 4 + fb, n0:n0 + nn], pq[k][:, 0:nn],
                               [b_pq[k]], [b_qT])
                    if "t" not in P23:
                        continue
                    k = cnt % 3
                    cnt += 1
                    for dc in range(16):
                        MM(pq[k][0:16, :], cTb[:, dc, 1024:1040], slot[:, dc, :], dc == 0, dc == 15, [bs], [b_pq[k]])
                    CP("act", qtok[:, cb * 512:(cb + 1) * 512], pq[k][0:16, :], [b_pq[k]], [b_qtok])
                if "t" in P23:
                    P.dma("sp", qtok_scr, qtok[:], [b_qtok], [], "qtk")
                scnt = 0
                for (w, is_k, o_ap) in ((w_ck, True, memk_o), (w_cv, False, memv_o)):
                    if ("k" if is_k else "v") not in P23:
                        continue
                    for cb in range(4):
                        slot, bs = stream_w(w[:, cb * 512:(cb + 1) * 512], 16, 512)
                        for mt in range(2):
                            k = cnt % 3
                            cnt += 1
                            for dc in range(16):
                                MM(pq[k][:, :], mnT[:, dc, mt * 128:(mt + 1) * 128], slot[:, dc, :], dc == 0, dc == 15,
                                   [bs], [b_pq[k]])
                            s2 = scnt % 2
                            scnt += 1
                            CP("act", stg[s2][:], pq[k][:, :], [b_pq[k]], [b_stg[s2]])
                            P.dma("sp", o_ap[mt * 128:(mt + 1) * 128, cb * 512:(cb + 1) * 512], stg[s2][:], [b_stg[s2]], [],
                                  "mo%d" % s2)
                            if not is_k:
                                CP("dve", Vb[:, mt, cb * 512:(cb + 1) * 512], stg[s2][:], [b_stg[s2]], [b_Vb])
                        if is_k:
                            for fb in range(4):
                                k = cnt % 3
                                cnt += 1
                                for dc in range(16):
                                    MM(pq[k][:, 0:256], slot[:, dc, fb * 128:(fb + 1) * 128], mnT[:, dc, 0:256],
                                       dc == 0, dc == 15, [bs], [b_pq[k]])
                                CP("dve", kT[:, cb * 4 + fb, :], pq[k][:, 0:256], [b_pq[k]], [b_kT])
                P.flush()
            with contextlib.ExitStack() as es4:
                onesb = sbt(es4, "onesb", [128, 128], BF16); b_on = P.buf()
                onesf = sbt(es4, "onesf", [128, 128], F32)
                MEMSET("dve", onesb[:], 1.0, [b_on])
                MEMSET("dve", onesf[:], 1.0, [b_on])
                psc = [pst(es4, un("psc"), [128, 512], F32) for _ in range(2)]; b_psc = [P.buf(), P.buf()]
                pdn = pst(es4, "pdn", [128, 512], F32); b_pdn = P.buf()
                pcx = [pst(es4, un("pcx"), [128, 512], F32) for _ in range(2)]; b_pcx = [P.buf(), P.buf()]
                eT = [sbt(es4, un("eT"), [128, 2, 512], BF16) for _ in range(2)]; b_eT = [P.buf(), P.buf()]
                rden = [sbt(es4, un("rden"), [128, 512], F32) for _ in range(2)]; b_rden = [P.buf(), P.buf()]
                b_ctx = P.buf()
                it = 0
                ccnt = 0
                for tb in range(0 if 'D4' in os.environ.get('KB_SKIP', '') else 2):
                    n0 = tb * 512
                    for h in range(4):
                        k2 = it % 2
                        it += 1
                        for mt in range(2):
                            for dc in range(4):
                                MM(psc[mt][:, :], kT[:, h * 4 + dc, mt * 128:(mt + 1) * 128], qT[:, h * 4 + dc, n0:n0 + 512],
                                   dc == 0, dc == 3, [], [b_psc[mt]])
                            ACT(eT[k2][:, mt, :], psc[mt][:, :], AF.Exp, [b_psc[mt]], [b_eT[k2]], scale=ATT_SCALE)
                        for mt in range(2):
                            MM(pdn[:, :], onesb[:], eT[k2][:, mt, :], mt == 0, mt == 1, [b_on, b_eT[k2]], [b_pdn])
                        RECIP(rden[k2][:], pdn[:, :], [b_pdn], [b_rden[k2]])
                        for dc in range(4):
                            c2 = ccnt % 2
                            ccnt += 1
                            for mt in range(2):
                                MM(pcx[c2][:, :], Vb[:, mt, h * 512 + dc * 128:h * 512 + (dc + 1) * 128], eT[k2][:, mt, :],
                                   mt == 0, mt == 1, [b_eT[k2]], [b_pcx[c2]])
                            TT("dve", cTb[:, h * 4 + dc, n0:n0 + 512], pcx[c2][:, :], rden[k2][:], ALU.mult,
                               [b_pcx[c2], b_rden[k2]], [b_ctx])
                P.flush()
                Ks = [sbt(es4, un("Ks"), [128, 2, D], F32) for _ in range(2)]; b_Ks = [P.buf(), P.buf()]
                Vs = [sbt(es4, un("Vs"), [128, 2, D], F32) for _ in range(2)]; b_Vs = [P.buf(), P.buf()]
                qb = [sbt(es4, un("qb"), [128, D], F32) for _ in range(2)]; b_qb = [P.buf(), P.buf()]
                sc = [sbt(es4, un("sc"), [128, 8], F32) for _ in range(2)]; b_sc = [P.buf(), P.buf()]
                rd4 = [sbt(es4, un("rd4"), [128, 4], F32) for _ in range(2)]; b_rd4 = [P.buf(), P.buf()]
                pct = [psc[0][:, 0:16], psc[1][:, 0:16]]; b_pct = b_psc
                pd4 = [pcx[0][:, 0:4], pcx[1][:, 0:4]]; b_pd4 = b_pcx
                for s_ in range(0 if 'D5' in os.environ.get('KB_SKIP', '') else 16):
                    k2 = s_ % 2
                    P.dma("sp", Ks[k2][:], cmk[s_].rearrange("(mt p) d -> p mt d", p=128), [], [b_Ks[k2]], "ks")
                    P.dma("act", Vs[k2][:], cmv[s_].rearrange("(mt p) d -> p mt d", p=128), [], [b_Vs[k2]], "vs")
                    P.dma("sp", qb[k2][:], qtok_scr[s_, :].partition_broadcast(128), [], [b_qb[k2]], "qb")
                    for mt in range(2):
                        TT("dve" if mt == 0 else "pool", Ks[k2][:, mt, :], Ks[k2][:, mt, :], qb[k2][:], ALU.mult,
                           [b_Ks[k2], b_qb[k2]], [b_Ks[k2]])
                    RED("dve", sc[k2][:], Ks[k2][:].rearrange("p mt (h d) -> p (mt h) d", d=512), ALU.add, [b_Ks[k2]], [b_sc[k2]])
                    ACT(sc[k2][:], sc[k2][:], AF.Exp, [b_sc[k2]], [b_sc[k2]], scale=ATT_SCALE)
                    for mt in range(2):
                        MM(pd4[k2], onesf[:], sc[k2][:, mt * 4:(mt + 1) * 4], mt == 0, mt == 1, [b_on, b_sc[k2]], [b_pd4[k2]])
                    RECIP(rd4[k2][:], pd4[k2], [b_pd4[k2]], [b_rd4[k2]])
                    for kc in range(16):
                        h = kc // 4
                        for mt in range(2):
                            MM(pct[k2][:, kc:kc + 1], Vs[k2][:, mt, kc * 128:(kc + 1) * 128], sc[k2][:, mt * 4 + h:mt * 4 + h + 1],
                               mt == 0, mt == 1, [b_Vs[k2], b_sc[k2]], [b_pct[k2]])
                    TT("dve", cTb[:, :, 1024 + s_].rearrange("p (h c) -> p h c", c=4),
                       pct[k2].rearrange("p (h c) -> p h c", c=4),
                       rd4[k2][:].unsqueeze(2).broadcast_to([128, 4, 4]), ALU.mult, [b_pct[k2], b_rd4[k2]], [b_ctx])
                P.flush()
        proj_res(w_co, xres_src)
        if stop_after == "D":
            P.flush(final=True)
            return nc

        with contextlib.ExitStack() as esE:
            comb = sbt(esE, "comb", [128, 9, 32], F32); b_comb = P.buf()
            with contextlib.ExitStack() as es1:
                wr_sb = sbt(es1, "wr_sb", [128, 16, 36], F32); b_wr = P.buf()
                P.dma("sp", wr_sb[:], w_route.rearrange("(c p) n -> p c n", p=128), [], [b_wr], "wr")
                br_b = sbt(es1, "br_b", [128, 36], F32)
                P.dma("sp", br_b[:], b_route.partition_broadcast(128), [], [b_wr], "wr")
                h2f = sbt(es1, "h2f", [128, D], F32); b_h2f = P.buf()
                h2Tf = sbt(es1, "h2Tf", [128, 16, 128], F32); b_h2Tf = P.buf()
                pTf = [pst(es1, un("pTf"), [128, 4, 128], F32) for _ in range(2)]; b_pTf = [P.buf(), P.buf()]
                plg = pst(es1, "plg", [128, 64], F32); b_plg = P.buf()
                lg = sbt(es1, "lg", [128, 36], F32); b_r = P.buf()
                rt = sbt(es1, "rt", [128, 96], F32)
                gmax = rt[:, 0:1]; ngmax = rt[:, 1:2]; sume = rt[:, 2:3]; gval = rt[:, 3:4]
                m1 = rt[:, 4:5]; m2 = rt[:, 5:6]; dd = rt[:, 6:7]; e1 = rt[:, 7:8]; w1 = rt[:, 8:9]; w2 = rt[:, 9:10]
                ohg = rt[:, 12:16]; junk = rt[:, 16:20]; les = rt[:, 24:32]; oh1 = rt[:, 32:40]; msk = rt[:, 40:48]
                oh2 = rt[:, 48:56]; ec = rt[:, 56:64]; t32 = rt[:, 64:96]

                def route(n_, np_, xt, bx, rstd, gb, b_gb, b_hb):
                    r_ = slice(0, np_)
                    R_ = [b_r]
                    STT("dve", h2f[r_, :], xt[r_, :], rstd[r_, 0:1], gb[r_, :], ALU.mult, ALU.mult, [bx, b_gb, b_hb], [b_h2f])
                    for grp in range(4):
                        pt = pTf[grp % 2]; bpt = b_pTf[grp % 2]
                        for j in range(4):
                            dc = grp * 4 + j
                            TR(pt[:, j, 0:np_], h2f[r_, dc * 128:(dc + 1) * 128], ident[r_, r_], [b_h2f, b_cst], [bpt])
                        CP("act" if grp % 2 == 0 else "dve", h2Tf[:, grp * 4:(grp + 1) * 4, 0:np_], pt[:, :, 0:np_], [bpt], [b_h2Tf])
                    for dc in range(16):
                        MM(plg[r_, 0:36], h2Tf[:, dc, 0:np_], wr_sb[:, dc, :], dc == 0, dc == 15, [b_h2Tf, b_wr], [b_plg])
                    TT("dve", lg[r_, :], plg[r_, 0:36], br_b[r_, :], ALU.add, [b_plg, b_wr], R_)
                    RED("dve", gmax[r_, :], lg[r_, 0:4], ALU.max, R_, R_)
                    TS("dve", ohg[r_, :], lg[r_, 0:4], gmax[r_, :], None, ALU.is_ge, None, R_, R_)
                    TS("dve", ngmax[r_, :], gmax[r_, :], -1.0, None, ALU.mult, None, R_, R_)
                    ACT(junk[r_, :], lg[r_, 0:4], AF.Exp, R_, R_, bias=ngmax[r_, :], scale=1.0, accum_out=sume[r_, :])
                    RECIP(gval[r_, :], sume[r_, :], R_, R_)
                    TT("dve", t32[r_, :].rearrange("p (g e) -> p g e", e=8), lg[r_, 4:36].rearrange("p (g e) -> p g e", e=8),
                       ohg[r_, :].unsqueeze(2).broadcast_to([np_, 4, 8]), ALU.mult, R_, R_)
                    RED("dve", les[r_, :], t32[r_, :].rearrange("p (g e) -> p e g", e=8), ALU.add, R_, R_)
                    RED("dve", m1[r_, :], les[r_, :], ALU.max, R_, R_)
                    TS("dve", oh1[r_, :], les[r_, :], m1[r_, :], None, ALU.is_ge, None, R_, R_)
                    STT("dve", msk[r_, :], oh1[r_, :], -1e30, les[r_, :], ALU.mult, ALU.add, R_, R_)
                    RED("dve", m2[r_, :], msk[r_, :], ALU.max, R_, R_)
                    TS("dve", oh2[r_, :], msk[r_, :], m2[r_, :], None, ALU.is_ge, None, R_, R_)
                    TT("dve", dd[r_, :], m2[r_, :], m1[r_, :], ALU.subtract, R_, R_)
                    ACT(e1[r_, :], dd[r_, :], AF.Exp, R_, R_)
                    TS("dve", w1[r_, :], e1[r_, :], 1.0, None, ALU.add, None, R_, R_)
                    RECIP(w1[r_, :], w1[r_, :], R_, R_)
                    TT("dve", w2[r_, :], e1[r_, :], w1[r_, :], ALU.mult, R_, R_)
                    TT("dve", w1[r_, :], w1[r_, :], gval[r_, :], ALU.mult, R_, R_)
                    TT("dve", w2[r_, :], w2[r_, :], gval[r_, :], ALU.mult, R_, R_)
                    TS("dve", ec[r_, :], oh1[r_, :], w1[r_, :], None, ALU.mult, None, R_, R_)
                    STT("dve", ec[r_, :], oh2[r_, :], w2[r_, :], ec[r_, :], ALU.mult, ALU.add, R_, R_)
                    TT("dve", comb[r_, n_, :].rearrange("p (g e) -> p g e", e=8),
                       ohg[r_, :].unsqueeze(2).broadcast_to([np_, 4, 8]),
                       ec[r_, :].unsqueeze(1).broadcast_to([np_, 4, 8]), ALU.mult, R_, [b_comb])

                norm_T(es1, [(xres_src(ti), np_, c0) for (ti, np_, c0) in TILES], norm_ffn, cTb, route=route)
                P.flush()
            acc = sbt(esE, "acc", [128, 9, D], F32); b_acc = [P.buf() for _ in range(9)]
            for (ti, np_, c0) in TILES:
                P.dma("sp" if ti % 2 == 0 else "act", acc[0:np_, ti, :], xres_src(ti), [], [b_acc[ti]], "acc")
            esM = contextlib.ExitStack()
            alloc_ring(esM)
            wd = sbt(esM, "wd", [128, 4, D], BF16); b_wd = P.buf()
            pg = [pst(esM, un("pg"), [128, 512], F32) for _ in range(2)]; b_pg = [P.buf(), P.buf()]
            pu = [pst(esM, un("pu"), [128, 512], F32) for _ in range(2)]; b_pu = [P.buf(), P.buf()]
            po = [pst(esM, un("po"), [128, 512], F32) for _ in range(3)]; b_po = [P.buf() for _ in range(3)]
            sg = [sbt(esM, un("sg"), [128, 512], F32) for _ in range(2)]; b_sg = [P.buf(), P.buf()]
            aT = [sbt(esM, un("aT"), [128, 4, 512], BF16) for _ in range(2)]; b_aT = [P.buf(), P.buf()]
            gcnt = 0
            ocnt = 0
            bcnt = 0
            NEXP = int(os.environ.get('KB_NEXP', 32))
            for e_ in range(NEXP):
                wg_, b_wg = stream_w(w_gate[e_], 16, 512)
                wu_, b_wu = stream_w(w_up[e_], 16, 512)
                P.dma("pool", wd[:], w_down[e_].rearrange("(c p) n -> p c n", p=128), [], [b_wd], "wd")
                for (n0, nn, tls) in [(0, 512, [0, 1, 2, 3]), (512, 512, [4, 5, 6, 7]), (1024, 16, [8])]:
                    a_ = aT[bcnt % 2]; ba = b_aT[bcnt % 2]
                    bcnt += 1
                    for fc in range(4):
                        k = gcnt % 2
                        gcnt += 1
                        for dc in range(16):
                            MM(pg[k][:, 0:nn], wg_[:, dc, fc * 128:(fc + 1) * 128], cTb[:, dc, n0:n0 + nn], dc == 0, dc == 15,
                               [b_wg], [b_pg[k]])
                        ACT(sg[k][:, 0:nn], pg[k][:, 0:nn], AF.Silu, [b_pg[k]], [b_sg[k]])
                        for dc in range(16):
                            MM(pu[k][:, 0:nn], wu_[:, dc, fc * 128:(fc + 1) * 128], cTb[:, dc, n0:n0 + nn], dc == 0, dc == 15,
                               [b_wu], [b_pu[k]])
                        TT("dve", a_[:, fc, 0:nn], pu[k][:, 0:nn], sg[k][:, 0:nn], ALU.mult, [b_pu[k], b_sg[k]], [ba])
                    for ti in tls:
                        (_, np_, c0) = TILES[ti]
                        for cb in range(4):
                            k = ocnt % 3
                            ocnt += 1
                            for fc in range(4):
                                MM(po[k][0:np_, :], a_[:, fc, c0 - n0:c0 - n0 + np_], wd[:, fc, cb * 512:(cb + 1) * 512],
                                   fc == 0, fc == 3, [ba, b_wd], [b_po[k]])
                            STT("dve", acc[0:np_, ti, cb * 512:(cb + 1) * 512], po[k][0:np_, :], comb[0:np_, ti, e_:e_ + 1],
                                acc[0:np_, ti, cb * 512:(cb + 1) * 512], ALU.mult, ALU.add, [b_po[k], b_acc[ti], b_comb], [b_acc[ti]])
            P.flush()
            esM.close()
            gbf = sbt(esE, "gbf", [128, D], F32); b_gbf = P.buf()
            P.dma("sp", gbf[:], norm_final.partition_broadcast(128), [], [b_gbf], "gbf")
            yt = [sbt(esE, un("yt"), [128, D], F32) for _ in range(2)]; b_yt = [P.buf(), P.buf()]
            ssf = sbt(esE, "ssf", [128, 1], F32); rsf = sbt(esE, "rsf", [128, 1], F32)
            for (ti, np_, c0) in TILES:
                y_ = yt[ti % 2]; by = b_yt[ti % 2]
                rmsnorm_tile(None, acc[:, ti, :], np_, gbf, y_, ssf, rsf, y_, [b_gbf, b_acc[ti]], [by], by)
                P.dma("sp" if ti % 2 == 0 else "act", y_o[c0:c0 + np_, :], y_[0:np_, :], [by], [], "yo%d" % (ti % 2))
            P.flush()
        P.flush(final=True)
    return nc


_CONSTS = None


def make_in_maps(inputs):
    global _CONSTS
    if _CONSTS is None:
        _CONSTS = build_consts()
    g = lambda k: np.ascontiguousarray(np.asarray(inputs[k], dtype=np.float32))
    xp = g("x_prompt"); xs = g("x_sample"); memp = g("mem_prompt")
    shared = {
        "consts": _CONSTS,
        "norm_mix": g("norm_mix")[0], "w_in": g("w_in")[0], "conv_w": g("conv_w")[0], "conv_b": g("conv_b")[0],
        "conv_ln_g": g("conv_ln_g")[0], "conv_ln_b": g("conv_ln_b")[0], "shift_mu": g("shift_mu")[0],
        "w_decay_up": g("w_decay_up")[0], "decay_bias": g("decay_bias")[0], "w_a_up": g("w_a_up")[0],
        "a_bias": g("a_bias")[0], "w_g_up": g("w_g_up")[0], "k_k": g("k_k")[0], "k_a": g("k_a")[0],
        "r_k": g("r_k")[0].reshape(1024), "lnx_g": g("lnx_g")[0], "lnx_b": g("lnx_b")[0],
        "w_out": g("w_out")[0], "norm_x": g("norm_x")[0], "norm_mem": g("norm_mem")[0],
        "w_cq": g("w_cq")[0], "w_ck": g("w_ck")[0], "w_cv": g("w_cv")[0], "w_co": g("w_co")[0],
        "norm_ffn": g("norm_ffn")[0],
        "w_route": np.ascontiguousarray(np.concatenate([g("w_route_group")[0], g("w_route_expert")[0].reshape(D, 32)], axis=1)),
        "b_route": np.ascontiguousarray(np.concatenate([g("b_route_group")[0], g("b_route_expert")[0].reshape(32)])),
        "w_gate": g("w_gate")[0].reshape(32, D, 512), "w_up": g("w_up")[0].reshape(32, D, 512),
        "w_down": g("w_down")[0].reshape(32, 512, D), "norm_final": g("norm_final"),
    }
    cc = g("cache_conv")[0]; ssh = g("state_shift")[0]; srw = g("state_rwkv")[0]
    cmk = g("cache_mem_k")[0].reshape(128, 256, D); cmv = g("cache_mem_v")[0].reshape(128, 256, D)
    maps = []
    for c in range(8):
        b, half = c // 2, c % 2
        if half == 0:
            xw = np.concatenate([np.zeros((1024, D), np.float32), xp[b, 0:1024]], axis=0)
        else:
            xw = xp[b]
        m = dict(shared)
        m.update({"xw": np.ascontiguousarray(xw), "xs": np.ascontiguousarray(xs[16 * c:16 * c + 16, 0]),
                  "mem": memp[b], "cconv": cc[16 * c:16 * c + 16], "sshift": ssh[16 * c:16 * c + 16],
                  "srwkv": srw[16 * c:16 * c + 16], "cmk": cmk[16 * c:16 * c + 16], "cmv": cmv[16 * c:16 * c + 16]})
        maps.append({k: v for k, v in m.items() if k in DECL})
    return maps


def gather(res):
    R = res.results
    y_p = np.zeros((4, 2048, D), np.float32); y_s = np.zeros((128, 1, D), np.float32)
    conv_p = np.zeros((1, 4, 30, CW), np.float32); shift_p = np.zeros((1, 4, SHIFT_W), np.float32)
    rwkv_p = np.zeros((1, 4, 16, 64, 64), np.float32)
    memk = np.zeros((1, 4, 256, 4, 512), np.float32); memv = np.zeros((1, 4, 256, 4, 512), np.float32)
    conv_s = np.zeros((1, 128, 30, CW), np.float32); shift_s = np.zeros((1, 128, SHIFT_W), np.float32)
    rwkv_s = np.zeros((1, 128, 16, 64, 64), np.float32)
    for c in range(8):
        b, half = c // 2, c % 2
        r = R[c]
        y_p[b, half * 1024:(half + 1) * 1024] = r["y_o"][0:1024]
        y_s[16 * c:16 * c + 16, 0] = r["y_o"][1024:1040]
        conv_s[0, 16 * c:16 * c + 16] = r["convs_o"]; shift_s[0, 16 * c:16 * c + 16] = r["shifts_o"]
        rwkv_s[0, 16 * c:16 * c + 16] = r["rwkvs_o"]
        if half == 1:
            conv_p[0, b] = r["convp_o"]; shift_p[0, b] = r["shiftp_o"][0]; rwkv_p[0, b] = r["rwkvp_o"]
            memk[0, b] = r["memk_o"].reshape(256, 4, 512); memv[0, b] = r["memv_o"].reshape(256, 4, 512)
    return (y_p, y_s, conv_p, shift_p, rwkv_p, memk, memv, conv_s, shift_s, rwkv_s)


_NC = None


def kernel(**inputs):
    global _NC
    if _NC is None:
        _NC = build_program()
    maps = make_in_maps(inputs)
    res = run_bass_kernel_spmd(_NC, maps, core_ids=list(range(8)))
    return gather(res)
```

```python
import contextlib
import math
import os
import numpy as np
import concourse.bass as bass
import concourse.mybir as mybir
from concourse.bass_utils import run_bass_kernel_spmd

F32 = mybir.dt.float32
BF16 = mybir.dt.bfloat16
AF = mybir.ActivationFunctionType
ALU = mybir.AluOpType
AX = mybir.AxisListType

D = 2048
NW = 16
NOWN = 8
SHIFT_W = 3360
IN_W = 5408
DS = math.exp(-0.5)
RMS_EPS = 1e-6
LN_EPS = 1e-5
GN_EPS = 64e-5
CW = 1024


class Buf:
    __slots__ = ("last_w", "readers")

    def __init__(self):
        self.last_w = None
        self.readers = []


class Op:
    __slots__ = ("eng", "fn", "deps", "idx", "eidx", "is_dma", "sig", "need", "key")


class Prog:
    EPOCH = 16000

    def __init__(self, nc, semfn):
        self.nc = nc
        self.semfn = semfn
        self.ops = []
        self.nops = 0
        self.ecount = {}
        self.engs = {"pe": nc.tensor, "act": nc.scalar, "dve": nc.vector,
                     "pool": nc.gpsimd, "sp": nc.sync}
        self.bufs = []
        self.esem = {}
        self.ecnt = {}
        self.dsem = {}
        self.dcnt = {}
        self.waited = {}
        self.last_on = {}
        self.bar_deps = []
        self.bar_id = 0
        self.eng_bar = {}
        self.nwait = 0
        self.free_dsems = []
        self.free_by = {}
        self.dcls = {}
        self.nds = 0

    def buf(self):
        b = Buf()
        self.bufs.append(b)
        return b

    def op(self, eng, fn, reads=(), writes=(), dma=False, key=None):
        o = Op()
        o.eng, o.fn, o.is_dma, o.key = eng, fn, dma, key
        o.sig = None
        o.need = dma
        o.idx = self.nops
        self.nops += 1
        o.eidx = self.ecount.get(eng, 0)
        self.ecount[eng] = o.eidx + 1
        deps = {}
        for b in reads:
            p = b.last_w
            if p is None:
                continue
            if (not p.is_dma) and p.eng == eng:
                if dma or (eng != "pe" and o.eidx - p.eidx <= 2):
                    deps[p.idx] = p
            else:
                deps[p.idx] = p
        for b in writes:
            p = b.last_w
            if p is not None and (p.is_dma or p.eng != eng or dma):
                deps[p.idx] = p
            for rd in b.readers:
                if rd.is_dma or rd.eng != eng or dma:
                    deps[rd.idx] = rd
        for b in reads:
            b.readers.append(o)
        for b in writes:
            b.last_w = o
            b.readers = []
        deps.pop(o.idx, None)
        o.deps = list(deps.values())
        for p in o.deps:
            p.need = True
        self.ops.append(o)
        self.last_on[eng] = o
        return o

    def dma(self, q, out, in_, reads, writes, key):
        if writes:
            key = ("w", id(writes[0]))
        return self.op(q, lambda e: e.dma_start(out=out, in_=in_), reads, writes, dma=True, key=key)

    def flush(self, final=False):
        self.nflush = getattr(self, "nflush", -1) + 1
        if self.nflush in [int(x) for x in os.environ.get("KB_DROP", "").split(",") if x]:
            self.ops = []
            self.last_on = {}
            for b in self.bufs:
                b.last_w = None
                b.readers = []
            return
        had_ops = len(self.ops) > 0
        for e, o in self.last_on.items():
            if o is not None and not o.is_dma:
                o.need = True
        nkey = {}
        for o in self.ops:
            if o.need and o.is_dma:
                nkey[o.key] = nkey.get(o.key, 0) + 1
        for o in self.ops:
            if not o.need:
                continue
            if o.is_dma:
                k = o.key
                cls = "sw" if o.eng == "pool" else "hw"
                if k not in self.dsem:
                    fl = self.free_by.setdefault(cls, [])
                    fl.sort(key=lambda sc: -sc[1])
                    self.dcls[k] = cls
                    if fl and fl[-1][1] + 16 * nkey[k] <= 24000:
                        self.dsem[k], self.dcnt[k] = fl.pop()
                    else:
                        self.nds += 1
                        self.dsem[k] = self.semfn("dsem%d" % self.nds)
                        self.dcnt[k] = 0
                assert self.dcls[k] == cls, (k, cls)
                self.dcnt[k] += 16
                o.sig = (self.dsem[k], self.dcnt[k], 16)
            else:
                c = self.ecnt.get(o.eng, 0)
                ep = c // self.EPOCH
                lst = self.esem.setdefault(o.eng, [])
                while len(lst) <= ep:
                    lst.append(self.semfn("e_%s_%d" % (o.eng, len(lst))))
                o.sig = (lst[ep], c % self.EPOCH + 1, 1)
                self.ecnt[o.eng] = c + 1
        for o in self.ops:
            e = self.engs[o.eng]
            w = self.waited.setdefault(o.eng, {})
            need = {}
            if self.eng_bar.get(o.eng, 0) < self.bar_id:
                self.eng_bar[o.eng] = self.bar_id
                for (s, v) in self.bar_deps:
                    need[id(s)] = (s, v)
            for p in o.deps:
                s, v, _ = p.sig
                sid = id(s)
                if sid not in need or need[sid][1] < v:
                    need[sid] = (s, v)
            for sid, (s, v) in need.items():
                if w.get(sid, 0) < v:
                    e.wait_ge(s, v)
                    w[sid] = v
                    self.nwait += 1
            ins = o.fn(e)
            if o.sig is not None:
                ins.then_inc(o.sig[0], o.sig[2])
        bd = []
        for e, o in self.last_on.items():
            if o is not None and not o.is_dma and o.sig is not None:
                bd.append((o.sig[0], o.sig[1]))
        for k, s in self.dsem.items():
            bd.append((s, self.dcnt[k]))
        if not had_ops:
            bd = bd + list(self.bar_deps)
        self.bar_deps = bd
        self.bar_id += 1
        for k in list(self.dsem.keys()):
            self.free_by.setdefault(self.dcls[k], []).append((self.dsem[k], self.dcnt[k]))
        self.dcls = {}
        self.dsem = {}
        self.dcnt = {}
        self.ops = []
        self.last_on = {}
        for b in self.bufs:
            b.last_w = None
            b.readers = []
        if final:
            for en in ("sp", "act", "dve", "pool", "pe"):
                e = self.engs[en]
                w = self.waited.setdefault(en, {})
                for (s, v) in bd:
                    if w.get(id(s), 0) < v:
                        e.wait_ge(s, v)
                        w[id(s)] = v


def build_consts():
    c = np.zeros((128, 1024), np.float32)
    c[:, 0:128] = np.eye(128)
    s = np.arange(128)[:, None]
    t = np.arange(128)[None, :]
    same = (s // 64) == (t // 64)
    c[:, 128:256] = (same & (s < t))
    c[:, 256:384] = (same & (s <= t))
    c[:, 384:512] = (same & (s > t))
    c[:, 512:640] = -DS * (same & (s <= t))
    c[:, 640:768] = -DS * (same & (s < t))
    c[:, 768:896] = -DS * (same & (s > t))
    c[63, 896] = 1.0
    c[127, 897] = 1.0
    for i in range(4):
        for sl in range(4):
            c[sl * 30:(sl + 1) * 30, 898 + i * 16 + 4 * i + sl] = 1.0
    c[:, 962] = 1.0
    return c


DECL = set()
KB_S5 = int(os.environ.get('KB_S5', 15))
KB_STAGE = int(os.environ.get('KB_STAGE', 100))


def build_program(stop_after=None):
    nc = bass.Bass("TRN2", target_bir_lowering=False)

    big = ("w_gate", "w_up", "w_down", "cmk", "cmv")
    DECL.clear()

    def din(name, shape):
        if (stop_after in ("A", "B", "C") and name in big) or (stop_after == "D" and name in big[0:3]):
            return None
        DECL.add(name)
        return nc.dram_tensor(name, list(shape), F32, kind="ExternalInput").ap()

    def dout(name, shape):
        return nc.dram_tensor(name, list(shape), F32, kind="ExternalOutput").ap()

    xw = din("xw", [NW * 128, D])
    xs = din("xs", [16, D])
    mem = din("mem", [256, D])
    cconv = din("cconv", [16, 30, CW])
    sshift = din("sshift", [16, SHIFT_W])
    srwkv = din("srwkv", [16, 16, 64, 64])
    cmk = din("cmk", [16, 256, D])
    cmv = din("cmv", [16, 256, D])
    consts = din("consts", [128, 1024])
    norm_mix = din("norm_mix", [D]); w_in = din("w_in", [D, IN_W])
    conv_w = din("conv_w", [31, CW]); conv_b = din("conv_b", [CW])
    conv_ln_g = din("conv_ln_g", [CW]); conv_ln_b = din("conv_ln_b", [CW])
    shift_mu = din("shift_mu", [SHIFT_W])
    w_decay_up = din("w_decay_up", [64, 1024]); decay_bias = din("decay_bias", [1024])
    w_a_up = din("w_a_up", [64, 1024]); a_bias = din("a_bias", [1024])
    w_g_up = din("w_g_up", [160, 1024])
    k_k = din("k_k", [1024]); k_a = din("k_a", [1024]); r_k = din("r_k", [1024])
    lnx_g = din("lnx_g", [1024]); lnx_b = din("lnx_b", [1024])
    w_out = din("w_out", [D, D]); norm_x = din("norm_x", [D]); norm_mem = din("norm_mem", [D])
    w_cq = din("w_cq", [D, D]); w_ck = din("w_ck", [D, D]); w_cv = din("w_cv", [D, D]); w_co = din("w_co", [D, D])
    norm_ffn = din("norm_ffn", [D])
    w_route = din("w_route", [D, 36]); b_route = din("b_route", [36])
    w_gate = din("w_gate", [32, D, 512]); w_up = din("w_up", [32, D, 512]); w_down = din("w_down", [32, 512, D])
    norm_final = din("norm_final", [D])

    y_o = dout("y_o", [1040, D])
    convp_o = dout("convp_o", [30, CW])
    shiftp_o = dout("shiftp_o", [1, SHIFT_W])
    rwkvp_o = dout("rwkvp_o", [16, 64, 64])
    memk_o = dout("memk_o", [256, D])
    memv_o = dout("memv_o", [256, D])
    convs_o = dout("convs_o", [16, 30, CW])
    shifts_o = dout("shifts_o", [16, SHIFT_W])
    rwkvs_o = dout("rwkvs_o", [16, 16, 64, 64])

    q_scr = nc.dram_tensor("q_scr", [1 + NW * 128 + 16, SHIFT_W], F32, kind="Internal").ap()
    xres = nc.dram_tensor("xres", [1040, D], F32, kind="Internal").ap()
    qtok_scr = nc.dram_tensor("qtok_scr", [16, D], F32, kind="Internal").ap()
    bscr = nc.dram_tensor("bscr", [NW * 128 + 16, 2, 5, 512], F32, kind="Internal").ap()

    top = contextlib.ExitStack()
    with top:
        P = Prog(nc, lambda n: top.enter_context(nc.semaphore(n)))

        def MM(out, lhsT, rhs, st, sp, R, W):
            P.op("pe", lambda e: e.matmul(out, lhsT, rhs, start=st, stop=sp), R, W)

        def TR(out, in_, idn, R, W):
            P.op("pe", lambda e: e.transpose(out, in_, idn), R, W)

        def ACT(out, in_, func, R, W, **kw):
            P.op("act", lambda e: e.activation(out, in_, func, **kw), R, W)

        def TT(eng, out, a, b, op, R, W):
            P.op(eng, lambda e: e.tensor_tensor(out, a, b, op), R, W)

        def TS(eng, out, a, s1, s2, op0, op1, R, W):
            if s2 is None:
                P.op(eng, lambda e: e.tensor_scalar(out, a, s1, None, op0), R, W)
            else:
                P.op(eng, lambda e: e.tensor_scalar(out, a, s1, s2, op0, op1), R, W)

        def STT(eng, out, a, s, b, op0, op1, R, W):
            P.op(eng, lambda e: e.scalar_tensor_tensor(out, a, s, b, op0, op1), R, W)

        def CP(eng, out, in_, R, W):
            if eng == "act":
                P.op(eng, lambda e: e.copy(out, in_), R, W)
            else:
                P.op(eng, lambda e: e.tensor_copy(out, in_), R, W)

        def RED(eng, out, in_, op, R, W):
            P.op(eng, lambda e: e.tensor_reduce(out, in_, AX.X, op), R, W)

        def MEMSET(eng, ap, val, W):
            P.op(eng, lambda e: e.memset(ap, val), [], W)

        def RECIP(out, in_, R, W):
            P.op("dve", lambda e: e.reciprocal(out, in_), R, W)

        def RSQRT(ap, R, W):
            ACT(ap, ap, AF.Sqrt, R, W)
            RECIP(ap, ap, R, W)

        def sbt(es, name, shape, dt):
            return es.enter_context(nc.sbuf_tensor(name, list(shape), dt))

        def pst(es, name, shape, dt):
            return es.enter_context(nc.psum_tensor(name, list(shape), dt))

        cst = sbt(top, "cst", [128, 1024], F32); b_cst = P.buf()
        cTb = sbt(top, "cTb", [128, 16, 1040], BF16)
        identb = sbt(top, "identb", [128, 128], BF16)
        ident = cst[:, 0:128]
        mask_ur = cst[:, 128:384]
        mask_sl = cst[:, 384:512]
        tri_le = cst[:, 512:640]; tri_lt = cst[:, 640:768]; tri_gt = cst[:, 768:896]
        esel = cst[:, 896:898]
        ones_col = cst[:, 962:963]
        P.dma("sp", cst[:], consts, [], [b_cst], "cst")
        if os.environ.get("KB_SIMINIT"):
            P.op("dve", lambda e: e.memset(cTb[:], 0.0), [], [b_cst])
        CP("dve", identb[:], ident, [b_cst], [b_cst])
        P.flush()

        rr = [0]

        def rmsnorm_tile(es_bufs, xt, np_, gb, hb, ss, rstd, tmp, R, W, b_tmp):
            ACT(tmp[0:np_, :], xt[0:np_, :], AF.Square, R, [b_tmp], accum_out=ss[0:np_, :])
            TS("dve", rstd[0:np_, :], ss[0:np_, :], 1.0 / D, RMS_EPS, ALU.mult, ALU.add, [b_tmp], [b_tmp])
            RSQRT(rstd[0:np_, :], [b_tmp], [b_tmp])
            STT("dve", hb[0:np_, :], xt[0:np_, :], rstd[0:np_, 0:1], gb[0:np_, :], ALU.mult, ALU.mult,
                R + [b_tmp], W)

        NSLOT = 2
        wring = [None] * NSLOT
        b_ring = [None] * NSLOT
        ring_ctr = [0]
        ring_gen = [0]

        def alloc_ring(es_):
            ring_gen[0] += 1
            for i_ in range(NSLOT):
                wring[i_] = sbt(es_, "wring%d_%d" % (i_, ring_gen[0]), [128, 16, 512], BF16)
                b_ring[i_] = None

        def stream_w(src_ap, kc, ncols):
            i = ring_ctr[0] % NSLOT
            ring_ctr[0] += 1
            if b_ring[i] is None or b_ring[i] not in P.bufs:
                b_ring[i] = P.buf()
            P.dma("pool", wring[i][:, 0:kc, 0:ncols], src_ap.rearrange("(c p) n -> p c n", p=128),
                  [], [b_ring[i]], "ring%d" % i)
            return wring[i], b_ring[i]

        with contextlib.ExitStack() as es:
            uT = sbt(es, "uT", [128, 8, 1072], F32); b_uT = [P.buf() for _ in range(8)]
            esA = contextlib.ExitStack()
            alloc_ring(esA)
            NT = NW * 128 + 16
            hT = sbt(esA, "hT", [128, 16, NT], BF16); b_hT = P.buf()
            gb = sbt(esA, "gb", [128, D], F32); b_gb = P.buf()
            P.dma("sp", gb[:], norm_mix.partition_broadcast(128), [], [b_gb], "gb")
            xt2 = [sbt(esA, "xt0", [128, D], F32)] * 2
            b_xt = [P.buf()] * 2
            hb = sbt(esA, "hb", [128, D], BF16); b_hb = P.buf()
            ss = sbt(esA, "ss", [128, 1], F32); rstd = sbt(esA, "rstd", [128, 1], F32); b_tmp = P.buf()
            pT = [pst(esA, "pT%d" % i, [128, 8, 128], BF16) for i in range(2)]
            b_pT = [P.buf() for _ in range(2)]
            zrow = sbt(esA, "zrow", [105, 32], F32); b_z = P.buf()
            MEMSET("dve", zrow[:], 0.0, [b_z])
            P.dma("sp", q_scr[0, :].rearrange("(p n) -> p n", n=32), zrow[:], [b_z], [], "zrow")
            for i in range(NW + 1):
                np_ = 128 if i < NW else 16
                xt = xt2[i % 2]; bx = b_xt[i % 2]
                src = xw[i * 128:(i + 1) * 128, :] if i < NW else xs
                P.dma("sp" if i % 2 == 0 else "act", xt[0:np_, :], src, [], [bx], "xl0")
                rmsnorm_tile(es, xt, np_, gb, hb, ss, rstd, hb, [bx, b_gb], [b_hb], b_hb)
                for hf in range(2):
                    for j in range(8):
                        dc = hf * 8 + j
                        TR(pT[hf][0:128, j, 0:np_], hb[0:np_, dc * 128:(dc + 1) * 128], identb[0:np_, 0:np_],
                           [b_hb, b_cst], [b_pT[hf]])
                    CP("act" if hf == 0 else "dve", hT[:, hf * 8:(hf + 1) * 8, i * 128:i * 128 + np_],
                       pT[hf][:, :, 0:np_], [b_pT[hf]], [b_hT])
            pq = [pst(esA, "pq%d" % i, [128, 512], F32) for i in range(3)]
            b_pq = [P.buf() for _ in range(3)]
            qst = [sbt(esA, "qst%d" % i, [128, 512], F32) for i in range(3)]
            b_qst = [P.buf() for _ in range(3)]
            cnt = 0
            for cb in range(7):
                c0 = 2048 + cb * 512
                ncol = min(512, IN_W - c0)
                slot, bs = stream_w(w_in[:, c0:c0 + ncol], 16, ncol)
                for i in range(NW + 1):
                    np_ = 128 if i < NW else 16
                    k = cnt % 3
                    cnt += 1
                    for dc in range(16):
                        MM(pq[k][0:np_, 0:ncol], hT[:, dc, i * 128:i * 128 + np_], slot[:, dc, 0:ncol],
                           dc == 0, dc == 15, [b_hT, bs], [b_pq[k]])
                    CP("act" if cnt % 2 == 0 else "dve", qst[k][0:np_, 0:ncol], pq[k][0:np_, 0:ncol],
                       [b_pq[k]], [b_qst[k]])
                    P.dma("sp", q_scr[1 + i * 128:1 + i * 128 + np_, c0 - 2048:c0 - 2048 + ncol],
                          qst[k][0:np_, 0:ncol], [b_qst[k]], [], "qst%d" % k)
            chunks = [(992, 512), (1504, 512), (2016, 48)]
            sg = [sbt(esA, "sg%d" % i, [128, 512], F32) for i in range(2)]
            b_sg = [P.buf() for _ in range(2)]
            for cb in range(4):
                slot, bs = stream_w(w_in[:, cb * 512:(cb + 1) * 512], 16, 512)
                for fb in range(4):
                    fblk = cb * 4 + fb
                    for (n0, nn) in chunks:
                        k = cnt % 3
                        cnt += 1
                        for dc in range(16):
                            MM(pq[k][:, 0:nn], slot[:, dc, fb * 128:(fb + 1) * 128], hT[:, dc, n0:n0 + nn],
                               dc == 0, dc == 15, [b_hT, bs], [b_pq[k]])
                        if fblk < 8:
                            CP("dve", uT[:, fblk, n0 - 992:n0 - 992 + nn], pq[k][:, 0:nn], [b_pq[k]], [b_uT[fblk]])
                        else:
                            ACT(sg[k % 2][:, 0:nn], pq[k][:, 0:nn], AF.Sigmoid, [b_pq[k]], [b_sg[k % 2]])
                            TT("dve", uT[:, fblk - 8, n0 - 992:n0 - 992 + nn], uT[:, fblk - 8, n0 - 992:n0 - 992 + nn],
                               sg[k % 2][:, 0:nn], ALU.mult, [b_sg[k % 2], b_uT[fblk - 8]], [b_uT[fblk - 8]])
            P.flush()
            esA.close()
            P.dma("sp", shiftp_o, q_scr[NW * 128:NW * 128 + 1, :], [], [], "sho")
            P.dma("sp", shifts_o, q_scr[1 + NW * 128:1 + NW * 128 + 16, :], [], [], "sho")

            with contextlib.ExitStack() as es2:
                cw_tm = sbt(es2, "cw_tm", [31, CW], F32); b_cw = P.buf()
                P.dma("sp", cw_tm[:], conv_w, [], [b_cw], "cw")
                pv = sbt(es2, "pv", [24, 128], F32); b_pv = P.buf()
                P.dma("sp", pv[0:8, :], conv_b.rearrange("(b p) -> b p", p=128), [], [b_pv], "cw")
                P.dma("sp", pv[8:16, :], conv_ln_g.rearrange("(b p) -> b p", p=128), [], [b_pv], "cw")
                P.dma("sp", pv[16:24, :], conv_ln_b.rearrange("(b p) -> b p", p=128), [], [b_pv], "cw")
                cwT = sbt(es2, "cwT", [128, 8, 31], F32); b_cwT = P.buf()
                pvT = sbt(es2, "pvT", [128, 24], F32)
                pc = pst(es2, "pc", [128, 8, 32], F32); b_pc = P.buf()
                pc2 = pst(es2, "pc2", [128, 32], F32); b_pc2 = P.buf()
                for blk in range(8):
                    TR(pc[:, blk, 0:31], cw_tm[:, blk * 128:(blk + 1) * 128], ident[0:31, 0:31], [b_cw, b_cst], [b_pc])
                CP("dve", cwT[:], pc[:, :, 0:31], [b_pc], [b_cwT])
                TR(pc2[:, 0:24], pv[:], ident[0:24, 0:24], [b_pv, b_cst], [b_pc2])
                CP("dve", pvT[:], pc2[:, 0:24], [b_pc2], [b_cwT])
                cT = sbt(es2, "cT", [128, 8, 1024], F32); b_cT = [P.buf() for _ in range(8)]
                for blk in range(8):
                    eng = "dve"
                    TS(eng, cT[:, blk, :], uT[:, blk, 2:1026], cwT[:, blk, 0:1], pvT[:, blk:blk + 1], ALU.mult, ALU.add,
                       [b_uT[blk], b_cwT], [b_cT[blk]])
                    for j in range(1, 31):
                        STT(eng, cT[:, blk, :], uT[:, blk, 2 + j:1026 + j], cwT[:, blk, j:j + 1], cT[:, blk, :],
                            ALU.mult, ALU.add, [b_uT[blk], b_cwT, b_cT[blk]], [b_cT[blk]])
                urow = sbt(es2, "urow", [30, CW], F32); b_urow = P.buf()
                us = sbt(es2, "us", [16, CW], F32); b_us = P.buf()
                pu = pst(es2, "pu", [32, 1024], F32); b_pu = P.buf()
                for blk in range(8):
                    TR(pu[0:30, blk * 128:(blk + 1) * 128], uT[:, blk, 1026:1056], ident, [b_uT[blk], b_cst], [b_pu])
                CP("act", urow[:], pu[0:30, :], [b_pu], [b_urow])
                P.dma("sp", convp_o, urow[:], [b_urow], [], "cpo")
                for blk in range(8):
                    TR(pu[0:16, blk * 128:(blk + 1) * 128], uT[:, blk, 1056:1072], ident, [b_uT[blk], b_cst], [b_pu])
                CP("act", us[:], pu[0:16, :], [b_pu], [b_us])
                P.dma("sp", convs_o[:, 29, :], us[:], [b_us], [], "cso")
                P.dma("act", convs_o[:, 0:29, :], cconv[:, 1:30, :], [], [], "cso2")
                onesm = sbt(es2, "onesm", [128, 128], F32); b_ones = P.buf()
                MEMSET("dve", onesm[:], 1.0 / CW, [b_ones])
                ps1 = pst(es2, "ps1", [128, 512], F32); b_ps1 = P.buf()
                ps2 = pst(es2, "ps2", [128, 512], F32); b_ps2 = P.buf()
                sqc = [sbt(es2, "sqc%d" % i, [128, 512], F32) for i in range(2)]
                b_sqc = [P.buf() for _ in range(2)]
                mean = sbt(es2, "mean", [128, 512], F32); rs = sbt(es2, "rs", [128, 512], F32); b_st = P.buf()
                tn = [sbt(es2, "tn%d" % i, [128, 512], F32) for i in range(2)]
                b_tn = [P.buf() for _ in range(2)]
                b_cTb = P.buf()
                for ncx in range(2):
                    n0 = ncx * 512
                    for blk in range(8):
                        MM(ps1[:], onesm[:], cT[:, blk, n0:n0 + 512], blk == 0, blk == 7, [b_ones, b_cT[blk]], [b_ps1])
                        ACT(sqc[blk % 2][:], cT[:, blk, n0:n0 + 512], AF.Square, [b_cT[blk]], [b_sqc[blk % 2]])
                        MM(ps2[:], onesm[:], sqc[blk % 2][:], blk == 0, blk == 7, [b_ones, b_sqc[blk % 2]], [b_ps2])
                    CP("dve", mean[:], ps1[:], [b_ps1], [b_st])
                    TT("dve", rs[:], mean[:], mean[:], ALU.mult, [b_st], [b_st])
                    TT("dve", rs[:], ps2[:], rs[:], ALU.subtract, [b_ps2, b_st], [b_st])
                    TS("dve", rs[:], rs[:], LN_EPS, None, ALU.add, None, [b_st], [b_st])
                    RSQRT(rs[:], [b_st], [b_st])
                    for blk in range(8):
                        t = tn[blk % 2]; bt = b_tn[blk % 2]
                        TT("dve", t[:], cT[:, blk, n0:n0 + 512], mean[:], ALU.subtract, [b_cT[blk], b_st], [bt])
                        TT("dve", t[:], t[:], rs[:], ALU.mult, [bt, b_st], [bt])
                        ACT(cTb[:, blk, n0:n0 + 512], t[:], AF.Silu, [bt, b_cwT], [b_cTb],
                            bias=pvT[:, 16 + blk:17 + blk], scale=pvT[:, 8 + blk:9 + blk])
                wrep = sbt(es2, "wrep", [120, CW], F32); b_wrep = P.buf()
                for r4 in range(4):
                    P.dma("sp", wrep[r4 * 30:(r4 + 1) * 30, :], conv_w[0:30, :], [], [b_wrep], "wrep")
                bc16 = sbt(es2, "bc16", [16, 4, CW], F32); b_bc16 = P.buf()
                P.dma("sp", bc16[:, 0, :], conv_w[30, :].partition_broadcast(16), [], [b_bc16], "bc16")
                P.dma("sp", bc16[:, 1, :], conv_b.partition_broadcast(16), [], [b_bc16], "bc16")
                P.dma("sp", bc16[:, 2, :], conv_ln_g.partition_broadcast(16), [], [b_bc16], "bc16")
                P.dma("sp", bc16[:, 3, :], conv_ln_b.partition_broadcast(16), [], [b_bc16], "bc16")
                cch = [sbt(es2, "cch%d" % i, [120, CW], F32) for i in range(2)]
                b_cch = [P.buf() for _ in range(2)]
                pcs = pst(es2, "pcs", [16, 1024], F32); b_pcs = P.buf()
                for i4 in range(4):
                    t = cch[i4 % 2]; bt = b_cch[i4 % 2]
                    P.dma("sp", t[:], cconv[i4 * 4:(i4 + 1) * 4, :, :].rearrange("s j c -> (s j) c"), [], [bt], "cch%d" % (i4 % 2))
                    TT("dve", t[:], t[:], wrep[:], ALU.mult, [bt, b_wrep], [bt])
                    for hf in range(2):
                        MM(pcs[:, hf * 512:(hf + 1) * 512], cst[0:120, 898 + i4 * 16:898 + (i4 + 1) * 16],
                           t[:, hf * 512:(hf + 1) * 512], i4 == 0, i4 == 3, [bt, b_cst], [b_pcs])
                cs = sbt(es2, "cs", [16, CW], F32); b_cs = P.buf()
                cs2 = sbt(es2, "cs2", [16, CW], F32)
                st16 = sbt(es2, "st16", [16, 4], F32)
                TT("dve", cs[:], us[:], bc16[:, 0, :], ALU.mult, [b_us, b_bc16], [b_cs])
                TT("dve", cs[:], cs[:], pcs[:], ALU.add, [b_cs, b_pcs], [b_cs])
                TT("dve", cs[:], cs[:], bc16[:, 1, :], ALU.add, [b_cs, b_bc16], [b_cs])
                ACT(cs2[:], cs[:], AF.Copy, [b_cs], [b_cs], accum_out=st16[:, 0:1])
                TS("dve", st16[:, 0:1], st16[:, 0:1], 1.0 / CW, None, ALU.mult, None, [b_cs], [b_cs])
                TS("dve", cs[:], cs[:], st16[:, 0:1], None, ALU.subtract, None, [b_cs], [b_cs])
                ACT(cs2[:], cs[:], AF.Square, [b_cs], [b_cs], accum_out=st16[:, 1:2])
                TS("dve", st16[:, 1:2], st16[:, 1:2], 1.0 / CW, LN_EPS, ALU.mult, ALU.add, [b_cs], [b_cs])
                RSQRT(st16[:, 1:2], [b_cs], [b_cs])
                STT("dve", cs[:], cs[:], st16[:, 1:2], bc16[:, 2, :], ALU.mult, ALU.mult, [b_cs, b_bc16], [b_cs])
                TT("dve", cs[:], cs[:], bc16[:, 3, :], ALU.add, [b_cs, b_bc16], [b_cs])
                ACT(cs2[:], cs[:], AF.Silu, [b_cs], [b_cs])
                for blk in range(8):
                    TR(pc[:, blk, 0:16], cs2[:, blk * 128:(blk + 1) * 128], ident[0:16, 0:16], [b_cs, b_cst], [b_pc])
                CP("dve", cTb[:, 0:8, 1024:1040], pc[:, :, 0:16], [b_pc], [b_cTb])
                P.flush()
        if stop_after == "A":
            P.flush(final=True)
            return nc

        def load_bc(es_, name, src, n, np_=128):
            t = sbt(es_, name, [np_, n], F32)
            b = P.buf()
            P.dma("sp", t[:], src.partition_broadcast(np_), [], [b], "bc")
            return t, b

        with contextlib.ExitStack() as es:
            mu_b, b_mu = load_bc(es, "mu_b", shift_mu, SHIFT_W)
            kk_b, b_par = load_bc(es, "kk_b", k_k, 1024)
            ka_b, _b = load_bc(es, "ka_b", k_a, 1024); rk_b, _b2 = load_bc(es, "rk_b", r_k, 1024)
            lg_b, _b3 = load_bc(es, "lg_b", lnx_g, 1024); lb_b, _b4 = load_bc(es, "lb_b", lnx_b, 1024)
            b_pars = [b_par, _b, _b2, _b3, _b4]
            wdu = sbt(es, "wdu", [65, 1024], F32); wau = sbt(es, "wau", [65, 1024], F32)
            wgu = sbt(es, "wgu", [128, 2, 1024], F32); b_lw = P.buf()
            P.dma("sp", wdu[0:64, :], w_decay_up, [], [b_lw], "lw")
            P.dma("sp", wdu[64:65, :], decay_bias.rearrange("(o n) -> o n", o=1), [], [b_lw], "lw")
            P.dma("sp", wau[0:64, :], w_a_up, [], [b_lw], "lw")
            P.dma("sp", wau[64:65, :], a_bias.rearrange("(o n) -> o n", o=1), [], [b_lw], "lw")
            P.dma("sp", wgu[:, 0, :], w_g_up[0:128, :], [], [b_lw], "lw")
            P.dma("sp", wgu[0:32, 1, :], w_g_up[128:160, :], [], [b_lw], "lw")
            q = sbt(es, "q", [128, SHIFT_W], F32); b_q = P.buf()
            qp = sbt(es, "qp", [128, SHIFT_W], F32); b_qp = P.buf()
            Wt_full = [sbt(es, "W%d" % i, [128, 1024], F32) for i in range(3, 7)]
            Wt = [qp[:, 0:1024], qp[:, 1024:2048], qp[:, 2048:3072]] + [w_[:] for w_ in Wt_full]
            b_W = [b_qp, b_qp, b_qp] + [P.buf() for _ in range(4)]
            loT = sbt(es, "loT", [128, 4, 128], F32); b_loT = P.buf()
            MEMSET("dve", loT[:], 1.0, [b_loT])
            lo_in = sbt(es, "lo_in", [128, 288], F32); b_loin = P.buf()
            sm = sbt(es, "sm", [128, 64], F32); b_sm = P.buf()
            Ot = Wt[3]; b_Ot = b_W[3]
            S = sbt(es, "S", [128, 8, 64], F32); b_S = P.buf()
            b_S2 = [P.buf(), P.buf()]; b_tA2 = [P.buf(), P.buf()]; b_Sw2 = [P.buf(), P.buf()]; b_sa2 = [P.buf(), P.buf()]
            b_tB2 = [P.buf(), P.buf()]; b_tC2 = [P.buf(), P.buf()]; b_tD2 = [[P.buf(), P.buf()], [P.buf(), P.buf()]]
            MEMSET("dve", S[:], 0.0, b_S2)
            tA = sbt(es, "tA", [128, 8, 64], F32); b_tA = P.buf()
            Sw = sbt(es, "Sw", [128, 8, 64], F32); b_Sw = P.buf()
            tB = sbt(es, "tB", [128, 8, 64], F32); b_tB = P.buf()
            tC = sbt(es, "tC", [128, 8, 64], F32); b_tC = P.buf()
            tD = [sbt(es, "tD%d" % i, [128, 8, 64], F32) for i in range(2)]; b_tD = [P.buf() for _ in range(2)]
            sa = sbt(es, "sa", [128, 8], F32); b_sa = P.buf()
            vT = sbt(es, "vT", [128, 8, 128], F32); b_vT = P.buf()
            oT = sbt(es, "oT", [128, 8, 128], F32); b_oT = P.buf()
            NB = 4
            bc = [sbt(es, "bc%d" % i, [128, 2560], F32) for i in range(NB)]; b_bc = [P.buf() for _ in range(NB)]
            b_scr = P.buf()
            bBp = [P.buf() for _ in range(NB)]
            bk = [pst(es, "bk%d" % i, [128, 512], F32) for i in range(8)]
            b_bk = [P.buf() for _ in range(8)]
            pL = [bk[0], bk[1]]; b_pL = [b_bk[0], b_bk[1]]
            pA = [bk[3], bk[4]]; b_pA = [b_bk[3], b_bk[4]]
            pB = [bk[2][:].rearrange("p (a b) -> p a b", b=128), bk[5][:].rearrange("p (a b) -> p a b", b=128)]
            b_pB = [b_bk[2], b_bk[5]]
            vps = [bk[6][:].rearrange("p (a b) -> p a b", b=128), bk[7][:].rearrange("p (a b) -> p a b", b=128)]

            def vec_part(np_, own):
                r_ = slice(0, np_)
                TT("dve", qp[r_, :], qp[r_, :], q[r_, :], ALU.subtract, [b_qp, b_q], [b_qp])
                TT("pool", qp[r_, :], qp[r_, :], mu_b[r_, :], ALU.mult, [b_qp, b_mu], [b_qp])
                TT("dve", q[r_, :], q[r_, :], qp[r_, :], ALU.add, [b_qp, b_q], [b_q])
                ACT(lo_in[r_, 0:64], q[r_, 3072:3136], AF.Tanh, [b_q], [b_loin])
                CP("act", lo_in[r_, 64:128], q[r_, 3136:3200], [b_q], [b_loin])
                ACT(lo_in[r_, 128:288], q[r_, 3200:3360], AF.Sigmoid, [b_q], [b_loin])
                k = 0
                TR(pB[k][0:64, 0, 0:np_], lo_in[r_, 0:64], ident[r_, r_], [b_loin, b_cst], [b_pB[k]])
                TR(pB[k][0:64, 1, 0:np_], lo_in[r_, 64:128], ident[r_, r_], [b_loin, b_cst], [b_pB[k]])
                TR(pB[k][0:128, 2, 0:np_], lo_in[r_, 128:256], ident[r_, r_], [b_loin, b_cst], [b_pB[k]])
                TR(pB[k][0:32, 3, 0:np_], lo_in[r_, 256:288], ident[r_, r_], [b_loin, b_cst], [b_pB[k]])
                CP("dve", loT[0:64, 0:2, 0:np_], pB[k][0:64, 0:2, 0:np_], [b_pB[k]], [b_loT])
                CP("dve", loT[:, 2, 0:np_], pB[k][:, 2, 0:np_], [b_pB[k]], [b_loT])
                CP("dve", loT[0:32, 3, 0:np_], pB[k][0:32, 3, 0:np_], [b_pB[k]], [b_loT])
                for hf in range(2):
                    c = slice(hf * 512, (hf + 1) * 512)
                    MM(pL[0][r_, :], loT[0:65, 0, r_], wdu[0:65, c], True, True, [b_loT, b_lw], [b_pL[0]])
                    ACT(Wt[0][r_, c], pL[0][r_, :], AF.Sigmoid, [b_pL[0]], [b_W[0]])
                    MM(pL[1][r_, :], loT[0:65, 1, r_], wau[0:65, c], True, True, [b_loT, b_lw], [b_pL[1]])
                    ACT(Wt[1][r_, c], pL[1][r_, :], AF.Sigmoid, [b_pL[1]], [b_W[1]])
                    if own:
                        MM(pA[hf][r_, :], loT[0:128, 2, r_], wgu[:, 0, c], True, False, [b_loT, b_lw], [b_pA[hf]])
                        MM(pA[hf][r_, :], loT[0:32, 3, r_], wgu[0:32, 1, c], False, True, [b_loT, b_lw], [b_pA[hf]])
                        CP("act", Wt[2][r_, c], pA[hf][r_, :], [b_pA[hf]], [b_W[2]])
                kv = q[r_, 1024:2048]; rv = q[r_, 0:1024]
                a = Wt[1][r_, :]
                TT("dve", Wt[3][r_, :], kv, kk_b[r_, :], ALU.mult, [b_q] + b_pars, [b_W[3]])
                TT("pool", Wt[5][r_, :], Wt[3][r_, :], Wt[3][r_, :], ALU.mult, [b_W[3]], [b_W[5]])
                RED("dve", sm[r_, 16:32], Wt[5][r_, :].rearrange("p (h k) -> p h k", k=64), ALU.add, [b_W[5]], [b_sm])
                ACT(sm[r_, 16:32], sm[r_, 16:32], AF.Sqrt, [b_sm], [b_sm])
                TS("dve", sm[r_, 16:32], sm[r_, 16:32], 1e-12, None, ALU.max, None, [b_sm], [b_sm])
                RECIP(sm[r_, 16:32], sm[r_, 16:32], [b_sm], [b_sm])
                TT("dve", Wt[3][r_, :].rearrange("p (h k) -> p h k", k=64), Wt[3][r_, :].rearrange("p (h k) -> p h k", k=64),
                   sm[r_, 16:32].unsqueeze(2).broadcast_to([np_, 16, 64]), ALU.mult, [b_W[3], b_sm], [b_W[3]])
                TT("pool", Wt[4][r_, :], Wt[3][r_, :], a, ALU.mult, [b_W[3], b_W[1]], [b_W[4]])
                STT("dve", Wt[5][r_, :], a, -1.0, ka_b[r_, :], ALU.add, ALU.mult, [b_W[1]] + b_pars, [b_W[5]])
                STT("dve", Wt[5][r_, :], Wt[5][r_, :], 1.0, kv, ALU.add, ALU.mult, [b_W[5], b_q], [b_W[5]])
                if own:
                    TT("pool", Wt[6][r_, :], rv, rk_b[r_, :], ALU.mult, [b_q] + b_pars, [b_W[6]])
                    TT("pool", Wt[6][r_, :], Wt[6][r_, :], Wt[5][r_, :], ALU.mult, [b_W[6], b_W[5]], [b_W[6]])
                    RED("dve", sm[r_, 0:16], Wt[6][r_, :].rearrange("p (h k) -> p h k", k=64), ALU.add, [b_W[6]], [b_sm])

            def out_part(np_, col0):
                r_ = slice(0, np_)
                O3 = Ot[r_, :].rearrange("p (h k) -> p h k", k=64)
                RED("dve", sm[r_, 32:48], O3, ALU.add, [b_Ot], [b_sm])
                TS("dve", sm[r_, 32:48], sm[r_, 32:48], 1.0 / 64, None, ALU.mult, None, [b_sm], [b_sm])
                TT("dve", O3, O3, sm[r_, 32:48].unsqueeze(2).broadcast_to([np_, 16, 64]), ALU.subtract, [b_Ot, b_sm], [b_Ot])
                TT("pool", Wt[6][r_, :], Ot[r_, :], Ot[r_, :], ALU.mult, [b_Ot], [b_W[6]])
                RED("dve", sm[r_, 48:64], Wt[6][r_, :].rearrange("p (h k) -> p h k", k=64), ALU.add, [b_W[6]], [b_sm])
                TS("dve", sm[r_, 48:64], sm[r_, 48:64], 1.0 / 64, GN_EPS, ALU.mult, ALU.add, [b_sm], [b_sm])
                RSQRT(sm[r_, 48:64], [b_sm], [b_sm])
                TT("dve", O3, O3, sm[r_, 48:64].unsqueeze(2).broadcast_to([np_, 16, 64]), ALU.mult, [b_Ot, b_sm], [b_Ot])
                TT("pool", Ot[r_, :], Ot[r_, :], lg_b[r_, :], ALU.mult, [b_Ot] + b_pars, [b_Ot])
                TT("dve", Ot[r_, :], Ot[r_, :], lb_b[r_, :], ALU.add, [b_Ot] + b_pars, [b_Ot])
                V3 = q[r_, 2048:3072].rearrange("p (h k) -> p h k", k=64)
                W63 = Wt[6][r_, :].rearrange("p (h k) -> p h k", k=64)
                TT("dve", W63, V3, sm[r_, 0:16].unsqueeze(2).broadcast_to([np_, 16, 64]), ALU.mult, [b_q, b_sm], [b_W[6]])
                TT("dve", Ot[r_, :], Ot[r_, :], Wt[6][r_, :], ALU.add, [b_Ot, b_W[6]], [b_Ot])
                TT("dve", Ot[r_, :], Ot[r_, :], Wt[2][r_, :], ALU.mult, [b_Ot, b_W[2]], [b_Ot])
                for hf in range(2):
                    for j in range(4):
                        blk = hf * 4 + j
                        TR(pB[hf][:, j, 0:np_], Ot[r_, blk * 128:(blk + 1) * 128], ident[r_, r_], [b_Ot, b_cst], [b_pB[hf]])
                    CP("act", cTb[:, 8 + hf * 4:12 + hf * 4, col0:col0 + np_], pB[hf][:, :, 0:np_], [b_pB[hf]], [b_cTbB])

            b_cTbB = P.buf()
            NTL = int(os.environ.get('KB_TILES', NW))
            for i in list(range(NTL)) + [NW]:
                samp = (i == NW)
                np_ = 16 if samp else 128
                own = samp or i >= NW - NOWN
                r_ = slice(0, np_)
                if samp:
                    P.dma("sp", q[r_, :], q_scr[1 + NW * 128:1 + NW * 128 + 16, :], [], [b_q], "ql")
                    P.dma("act", qp[r_, :], sshift, [], [b_qp], "qpl")
                else:
                    P.dma("sp", q[:], q_scr[1 + i * 128:1 + (i + 1) * 128, :], [], [b_q], "ql")
                    P.dma("act", qp[:], q_scr[i * 128:(i + 1) * 128, :], [], [b_qp], "qpl")
                vec_part(np_, own)
                ACT(Wt[6][r_, :], Wt[0][r_, :], AF.Exp, [b_W[0], b_sm], [b_W[6]], scale=-DS)
                srcs = [(Wt[6], b_W[6]), (Wt[3], b_W[3]), (Wt[4], b_W[4]), (Wt[5], b_W[5]), (q[:, 0:1024], b_q)]
                for f, (src, bs) in enumerate(srcs):
                    for hp in range(2):
                        P.dma("sp" if hp == 0 else "act",
                              bscr[i * 128:i * 128 + np_, hp, f, :].rearrange("t (j k) -> t j k", k=64),
                              src[r_, :].rearrange("p (j hp k) -> p hp j k", hp=2, k=64)[:, hp],
                              [bs], [b_scr], "scr")
                for j in range(8):
                    TR(vps[j // 4][:, j % 4, 0:np_], q[r_, 2048 + j * 128:2048 + (j + 1) * 128], ident[r_, r_],
                       [b_q, b_cst], [b_bk[6 + j // 4]])
                CP("act", vT[:, 0:4, 0:np_], vps[0][:, :, 0:np_], [b_bk[6]], [b_vT])
                CP("dve", vT[:, 4:8, 0:np_], vps[1][:, :, 0:np_], [b_bk[7]], [b_vT])
                for t in range(np_):
                    g = i * 128 + t
                    kb = g % NB
                    B_ = bc[kb]; bB = b_bc[kb]
                    for hp in range(2):
                        nf = 5 if own else 4
                        if os.environ.get("KB_Q4", "0") == "1":
                            P.dma("act" if hp == 1 else "sp", B_[hp * 64:(hp + 1) * 64, 0:1024],
                                  bscr[g, hp, 0:2].rearrange("f n -> (f n)").partition_broadcast(64),
                                  [b_scr], [bB], "bc%d" % kb)
                            P.dma("pool", B_[hp * 64:(hp + 1) * 64, 1024:nf * 512],
                                  bscr[g, hp, 2:nf].rearrange("f n -> (f n)").partition_broadcast(64),
                                  [b_scr], [bBp[kb]], "bcp%d" % kb)
                        else:
                            P.dma("act" if hp == 1 else "sp", B_[hp * 64:(hp + 1) * 64, 0:nf * 512],
                                  bscr[g, hp, 0:nf].rearrange("f n -> (f n)").partition_broadcast(64),
                                  [b_scr], [bB], "bc%d" % kb)
                    if samp:
                        for hp in range(2):
                            P.dma("act", S[hp * 64:(hp + 1) * 64, :, :],
                                  srwkv[t].rearrange("(j hp) v k -> hp v j k", hp=2)[hp], [], b_S2, "Sl")
                    fv = lambda f, js: B_[:, f * 512:(f + 1) * 512].rearrange("p (j k) -> p j k", k=64)[:, js, :]
                    td = tD[g % 2]
                    JS = [slice(0, 4), slice(4, 8)]
                    G2 = (0, 1)
                    for g2 in G2:
                        js = JS[g2]
                        TT("pool", td[:, js, :], fv(3, js), vT[:, js, t].unsqueeze(2).broadcast_to([128, 4, 64]), ALU.mult,
                           [bB, bBp[kb], b_vT], [b_tD2[g % 2][g2]])
                    for g2 in G2:
                        js = JS[g2]
                        TT("dve", tA[:, js, :], S[:, js, :], fv(1, js), ALU.mult, [b_S2[g2], bB, bBp[kb]], [b_tA2[g2]])
                    for g2 in G2:
                        js = JS[g2]
                        TT("pool", Sw[:, js, :], S[:, js, :], fv(0, js), ALU.mult, [b_S2[g2], bB, bBp[kb]], [b_Sw2[g2]])
                    for g2 in G2:
                        js = JS[g2]
                        RED("dve", sa[:, js], tA[:, js, :], ALU.add, [b_tA2[g2]], [b_sa2[g2]])
                    for g2 in G2:
                        js = JS[g2]
                        TT("dve", tB[:, js, :], fv(2, js), sa[:, js].unsqueeze(2).broadcast_to([128, 4, 64]), ALU.mult,
                           [bB, bBp[kb], b_sa2[g2]], [b_tB2[g2]])
                    for g2 in G2:
                        js = JS[g2]
                        TT("dve", S[:, js, :], Sw[:, js, :], tB[:, js, :], ALU.subtract, [b_Sw2[g2], b_tB2[g2]], [b_S2[g2]])
                    for g2 in G2:
                        js = JS[g2]
                        TT("dve", S[:, js, :], S[:, js, :], td[:, js, :], ALU.add, [b_S2[g2], b_tD2[g % 2][g2]], [b_S2[g2]])
                    if own:
                        for g2 in G2:
                            js = JS[g2]
                            TT("dve", tC[:, js, :], S[:, js, :], fv(4, js), ALU.mult, [b_S2[g2], bB, bBp[kb]], [b_tC2[g2]])
                        for g2 in G2:
                            js = JS[g2]
                            RED("dve", oT[:, js, t], tC[:, js, :], ALU.add, [b_tC2[g2]], [b_oT])
                    if samp:
                        for hp in range(2):
                            P.dma("sp", rwkvs_o[t].rearrange("(j hp) v k -> hp v j k", hp=2)[hp],
                                  S[hp * 64:(hp + 1) * 64, :, :], b_S2, [], "So")
                if i == NW - 1:
                    for hp in range(2):
                        P.dma("sp", rwkvp_o.rearrange("(j hp) v k -> hp v j k", hp=2)[hp],
                              S[hp * 64:(hp + 1) * 64, :, :], b_S2, [], "So")
                if own:
                    for j in range(8):
                        TR(vps[j // 4][0:np_, j % 4, :], oT[:, j, 0:np_], ident, [b_oT, b_cst], [b_bk[6 + j // 4]])
                    CP("act", Ot[r_, 0:512], bk[6][r_, :], [b_bk[6]], [b_Ot])
                    CP("dve", Ot[r_, 512:1024], bk[7][r_, :], [b_bk[7]], [b_Ot])
                    out_part(np_, 1024 if samp else (i - (NW - NOWN)) * 128)
                P.flush()
        if stop_after == "B":
            P.flush(final=True)
            return nc

        uid = [0]

        def un(n):
            uid[0] += 1
            return "%s_%d" % (n, uid[0])

        TILES = [(i, 128, i * 128) for i in range(8)] + [(8, 16, 1024)]
        ATT_SCALE = 512.0 ** -0.5

        def x_src(ti):
            return xw[1024 + ti * 128:1024 + (ti + 1) * 128, :] if ti < 8 else xs

        def xres_src(ti):
            (_, np_, c0) = TILES[ti]
            return xres[c0:c0 + np_, :]

        def proj_res(w, src_fn):
            with contextlib.ExitStack() as es_:
                alloc_ring(es_)
                pq = [pst(es_, un("pq"), [128, 512], F32) for _ in range(3)]; b_pq = [P.buf() for _ in range(3)]
                xin = [sbt(es_, un("xin"), [128, 512], F32) for _ in range(3)]; b_xin = [P.buf() for _ in range(3)]
                xo = [sbt(es_, un("xo"), [128, 512], F32) for _ in range(3)]; b_xo = [P.buf() for _ in range(3)]
                cnt = 0
                for cb in range(4):
                    slot, bs = stream_w(w[:, cb * 512:(cb + 1) * 512], 16, 512)
                    for (ti, np_, c0) in TILES:
                        k = cnt % 3
                        cnt += 1
                        P.dma("sp", xin[k][0:np_, :], src_fn(ti)[:, cb * 512:(cb + 1) * 512], [], [b_xin[k]], "xin")
                        for dc in range(16):
                            MM(pq[k][0:np_, :], cTb[:, dc, c0:c0 + np_], slot[:, dc, :], dc == 0, dc == 15, [bs], [b_pq[k]])
                        TT("dve", xo[k][0:np_, :], pq[k][0:np_, :], xin[k][0:np_, :], ALU.add, [b_pq[k], b_xin[k]], [b_xo[k]])
                        P.dma("act", xres[c0:c0 + np_, cb * 512:(cb + 1) * 512], xo[k][0:np_, :], [b_xo[k]], [], "xo%d" % k)
                P.flush()

        def norm_T(es_, src_list, g_dram, dst, route=None):
            gb = sbt(es_, un("gb"), [128, D], F32); b_gb = P.buf()
            P.dma("sp", gb[:], g_dram.partition_broadcast(128), [], [b_gb], "gb")
            xt2 = [sbt(es_, un("xt"), [128, D], F32) for _ in range(2)]; b_xt = [P.buf(), P.buf()]
            hb = sbt(es_, un("hb"), [128, D], BF16); b_hb = P.buf()
            ss = sbt(es_, un("ss"), [128, 1], F32); rstd = sbt(es_, un("rstd"), [128, 1], F32)
            pT = [pst(es_, un("pT"), [128, 8, 128], BF16) for _ in range(2)]; b_pT = [P.buf(), P.buf()]
            b_dst = P.buf()
            for n_, (src, np_, c0) in enumerate(src_list):
                xt = xt2[n_ % 2]; bx = b_xt[n_ % 2]
                P.dma("sp" if n_ % 2 == 0 else "act", xt[0:np_, :], src, [], [bx], "xl")
                rmsnorm_tile(None, xt, np_, gb, hb, ss, rstd, hb, [bx, b_gb], [b_hb], b_hb)
                for hf in range(2):
                    for j in range(8):
                        dc = hf * 8 + j
                        TR(pT[hf][0:128, j, 0:np_], hb[0:np_, dc * 128:(dc + 1) * 128], identb[0:np_, 0:np_],
                           [b_hb, b_cst], [b_pT[hf]])
                    CP("act" if hf == 0 else "dve", dst[:, hf * 8:(hf + 1) * 8, c0:c0 + np_], pT[hf][:, :, 0:np_],
                       [b_pT[hf]], [b_dst])
                if route is not None:
                    route(n_, np_, xt, bx, rstd, gb, b_gb, b_hb)

        proj_res(w_out, x_src)
        if stop_after == "C":
            P.flush(final=True)
            return nc

        with contextlib.ExitStack() as esD:
            qT = sbt(esD, "qT", [128, 16, 1040], BF16); b_qT = P.buf()
            kT = sbt(esD, "kT", [128, 16, 256], BF16); b_kT = P.buf()
            Vb = sbt(esD, "Vb", [128, 2, 2048], BF16); b_Vb = P.buf()
            with contextlib.ExitStack() as esa:
                norm_T(esa, [(xres_src(ti), np_, c0) for (ti, np_, c0) in TILES], norm_x, cTb)
                P.flush()
            with contextlib.ExitStack() as es1:
                mnT = sbt(es1, "mnT", [128, 16, 256], BF16)
                with contextlib.ExitStack() as esb:
                    norm_T(esb, [(mem[mt * 128:(mt + 1) * 128, :], 128, mt * 128) for mt in range(2)], norm_mem, mnT)
                    P.flush()
                alloc_ring(es1)
                pq = [pst(es1, un("pq"), [128, 512], F32) for _ in range(3)]; b_pq = [P.buf() for _ in range(3)]
                qtok = sbt(es1, "qtok", [16, D], F32); b_qtok = P.buf()
                stg = [sbt(es1, un("stg"), [128, 512], F32) for _ in range(2)]; b_stg = [P.buf(), P.buf()]
                cnt = 0
                P23 = os.environ.get("KB_P23", "qtkv")
                for cb in range(4 if "q" in P23 else 0):
                    slot, bs = stream_w(w_cq[:, cb * 512:(cb + 1) * 512], 16, 512)
                    for fb in range(4):
                        for (n0, nn) in [(0, 512), (512, 512), (1024, 16)]:
                            k = cnt % 3
                            cnt += 1
                            for dc in range(16):
                                MM(pq[k][:, 0:nn], slot[:, dc, fb * 128:(fb + 1) * 128], cTb[:, dc, n0:n0 + nn],
                                   dc == 0, dc == 15, [bs], [b_pq[k]])
                            CP("act" if cnt % 2 == 0 else "dve", qT[:, cb * 4 + fb, n0:n0 + nn], pq[k][:, 0:nn],
                               [b_pq[k]], [b_qT])
                    if "t" not in P23:
                        continue
                    k = cnt % 3
                    cnt += 1
                    for dc in range(16):
                        MM(pq[k][0:16, :], cTb[:, dc, 1024:1040], slot[:, dc, :], dc == 0, dc == 15, [bs], [b_pq[k]])
                    CP("act", qtok[:, cb * 512:(cb + 1) * 512], pq[k][0:16, :], [b_pq[k]], [b_qtok])
                if "t" in P23:
                    P.dma("sp", qtok_scr, qtok[:], [b_qtok], [], "qtk")
                scnt = 0
                for (w, is_k, o_ap) in ((w_ck, True, memk_o), (w_cv, False, memv_o)):
                    if ("k" if is_k else "v") not in P23:
                        continue
                    for cb in range(4):
                        slot, bs = stream_w(w[:, cb * 512:(cb + 1) * 512], 16, 512)
                        for mt in range(2):
                            k = cnt % 3
                            cnt += 1
                            for dc in range(16):
                                MM(pq[k][:, :], mnT[:, dc, mt * 128:(mt + 1) * 128], slot[:, dc, :], dc == 0, dc == 15,
                                   [bs], [b_pq[k]])
                            s2 = scnt % 2
                            scnt += 1
                            CP("act", stg[s2][:], pq[k][:, :], [b_pq[k]], [b_stg[s2]])
                            P.dma("sp", o_ap[mt * 128:(mt + 1) * 128, cb * 512:(cb + 1) * 512], stg[s2][:], [b_stg[s2]], [],
                                  "mo%d" % s2)
                            if not is_k:
                                CP("dve", Vb[:, mt, cb * 512:(cb + 1) * 512], stg[s2][:], [b_stg[s2]], [b_Vb])
                        if is_k:
                            for fb in range(4):
                                k = cnt % 3
                                cnt += 1
                                for dc in range(16):
                                    MM(pq[k][:, 0:256], slot[:, dc, fb * 128:(fb + 1) * 128], mnT[:, dc, 0:256],
                                       dc == 0, dc == 15, [bs], [b_pq[k]])
                                CP("dve", kT[:, cb * 4 + fb, :], pq[k][:, 0:256], [b_pq[k]], [b_kT])
                P.flush()
            with contextlib.ExitStack() as es4:
                onesb = sbt(es4, "onesb", [128, 128], BF16); b_on = P.buf()
                onesf = sbt(es4, "onesf", [128, 128], F32)
                MEMSET("dve", onesb[:], 1.0, [b_on])
                MEMSET("dve", onesf[:], 1.0, [b_on])
                psc = [pst(es4, un("psc"), [128, 512], F32) for _ in range(2)]; b_psc = [P.buf(), P.buf()]
                pdn = pst(es4, "pdn", [128, 512], F32); b_pdn = P.buf()
                pcx = [pst(es4, un("pcx"), [128, 512], F32) for _ in range(2)]; b_pcx = [P.buf(), P.buf()]
                eT = [sbt(es4, un("eT"), [128, 2, 512], BF16) for _ in range(2)]; b_eT = [P.buf(), P.buf()]
                rden = [sbt(es4, un("rden"), [128, 512], F32) for _ in range(2)]; b_rden = [P.buf(), P.buf()]
                b_ctx = P.buf()
                it = 0
                ccnt = 0
                for tb in range(0 if 'D4' in os.environ.get('KB_SKIP', '') else 2):
                    n0 = tb * 512
                    for h in range(4):
                        k2 = it % 2
                        it += 1
                        for mt in range(2):
                            for dc in range(4):
                                MM(psc[mt][:, :], kT[:, h * 4 + dc, mt * 128:(mt + 1) * 128], qT[:, h * 4 + dc, n0:n0 + 512],
                                   dc == 0, dc == 3, [], [b_psc[mt]])
                            ACT(eT[k2][:, mt, :], psc[mt][:, :], AF.Exp, [b_psc[mt]], [b_eT[k2]], scale=ATT_SCALE)
                        for mt in range(2):
                            MM(pdn[:, :], onesb[:], eT[k2][:, mt, :], mt == 0, mt == 1, [b_on, b_eT[k2]], [b_pdn])
                        RECIP(rden[k2][:], pdn[:, :], [b_pdn], [b_rden[k2]])
                        for dc in range(4):
                            c2 = ccnt % 2
                            ccnt += 1
                            for mt in range(2):
                                MM(pcx[c2][:, :], Vb[:, mt, h * 512 + dc * 128:h * 512 + (dc + 1) * 128], eT[k2][:, mt, :],
                                   mt == 0, mt == 1, [b_eT[k2]], [b_pcx[c2]])
                            TT("dve", cTb[:, h * 4 + dc, n0:n0 + 512], pcx[c2][:, :], rden[k2][:], ALU.mult,
                               [b_pcx[c2], b_rden[k2]], [b_ctx])
                P.flush()
                Ks = [sbt(es4, un("Ks"), [128, 2, D], F32) for _ in range(2)]; b_Ks = [P.buf(), P.buf()]
                Vs = [sbt(es4, un("Vs"), [128, 2, D], F32) for _ in range(2)]; b_Vs = [P.buf(), P.buf()]
                qb = [sbt(es4, un("qb"), [128, D], F32) for _ in range(2)]; b_qb = [P.buf(), P.buf()]
                sc = [sbt(es4, un("sc"), [128, 8], F32) for _ in range(2)]; b_sc = [P.buf(), P.buf()]
                rd4 = [sbt(es4, un("rd4"), [128, 4], F32) for _ in range(2)]; b_rd4 = [P.buf(), P.buf()]
                pct = [psc[0][:, 0:16], psc[1][:, 0:16]]; b_pct = b_psc
                pd4 = [pcx[0][:, 0:4], pcx[1][:, 0:4]]; b_pd4 = b_pcx
                for s_ in range(0 if 'D5' in os.environ.get('KB_SKIP', '') else 16):
                    k2 = s_ % 2
                    P.dma("sp", Ks[k2][:], cmk[s_].rearrange("(mt p) d -> p mt d", p=128), [], [b_Ks[k2]], "ks")
                    P.dma("act", Vs[k2][:], cmv[s_].rearrange("(mt p) d -> p mt d", p=128), [], [b_Vs[k2]], "vs")
                    P.dma("sp", qb[k2][:], qtok_scr[s_, :].partition_broadcast(128), [], [b_qb[k2]], "qb")
                    for mt in range(2):
                        TT("dve" if mt == 0 else "pool", Ks[k2][:, mt, :], Ks[k2][:, mt, :], qb[k2][:], ALU.mult,
                           [b_Ks[k2], b_qb[k2]], [b_Ks[k2]])
                    RED("dve", sc[k2][:], Ks[k2][:].rearrange("p mt (h d) -> p (mt h) d", d=512), ALU.add, [b_Ks[k2]], [b_sc[k2]])
                    ACT(sc[k2][:], sc[k2][:], AF.Exp, [b_sc[k2]], [b_sc[k2]], scale=ATT_SCALE)
                    for mt in range(2):
                        MM(pd4[k2], onesf[:], sc[k2][:, mt * 4:(mt + 1) * 4], mt == 0, mt == 1, [b_on, b_sc[k2]], [b_pd4[k2]])
                    RECIP(rd4[k2][:], pd4[k2], [b_pd4[k2]], [b_rd4[k2]])
                    for kc in range(16):
                        h = kc // 4
                        for mt in range(2):
                            MM(pct[k2][:, kc:kc + 1], Vs[k2][:, mt, kc * 128:(kc + 1) * 128], sc[k2][:, mt * 4 + h:mt * 4 + h + 1],
                               mt == 0, mt == 1, [b_Vs[k2], b_sc[k2]], [b_pct[k2]])
                    TT("dve", cTb[:, :, 1024 + s_].rearrange("p (h c) -> p h c", c=4),
                       pct[k2].rearrange("p (h c) -> p h c", c=4),
                       rd4[k2][:].unsqueeze(2).broadcast_to([128, 4, 4]), ALU.mult, [b_pct[k2], b_rd4[k2]], [b_ctx])
                P.flush()
        proj_res(w_co, xres_src)
        if stop_after == "D":
            P.flush(final=True)
            return nc

        with contextlib.ExitStack() as esE:
            comb = sbt(esE, "comb", [128, 9, 32], F32); b_comb = P.buf()
            with contextlib.ExitStack() as es1:
                wr_sb = sbt(es1, "wr_sb", [128, 16, 36], F32); b_wr = P.buf()
                P.dma("sp", wr_sb[:], w_route.rearrange("(c p) n -> p c n", p=128), [], [b_wr], "wr")
                br_b = sbt(es1, "br_b", [128, 36], F32)
                P.dma("sp", br_b[:], b_route.partition_broadcast(128), [], [b_wr], "wr")
                h2f = sbt(es1, "h2f", [128, D], F32); b_h2f = P.buf()
                h2Tf = sbt(es1, "h2Tf", [128, 16, 128], F32); b_h2Tf = P.buf()
                pTf = [pst(es1, un("pTf"), [128, 4, 128], F32) for _ in range(2)]; b_pTf = [P.buf(), P.buf()]
                plg = pst(es1, "plg", [128, 64], F32); b_plg = P.buf()
                lg = sbt(es1, "lg", [128, 36], F32); b_r = P.buf()
                rt = sbt(es1, "rt", [128, 96], F32)
                gmax = rt[:, 0:1]; ngmax = rt[:, 1:2]; sume = rt[:, 2:3]; gval = rt[:, 3:4]
                m1 = rt[:, 4:5]; m2 = rt[:, 5:6]; dd = rt[:, 6:7]; e1 = rt[:, 7:8]; w1 = rt[:, 8:9]; w2 = rt[:, 9:10]
                ohg = rt[:, 12:16]; junk = rt[:, 16:20]; les = rt[:, 24:32]; oh1 = rt[:, 32:40]; msk = rt[:, 40:48]
                oh2 = rt[:, 48:56]; ec = rt[:, 56:64]; t32 = rt[:, 64:96]

                def route(n_, np_, xt, bx, rstd, gb, b_gb, b_hb):
                    r_ = slice(0, np_)
                    R_ = [b_r]
                    STT("dve", h2f[r_, :], xt[r_, :], rstd[r_, 0:1], gb[r_, :], ALU.mult, ALU.mult, [bx, b_gb, b_hb], [b_h2f])
                    for grp in range(4):
                        pt = pTf[grp % 2]; bpt = b_pTf[grp % 2]
                        for j in range(4):
                            dc = grp * 4 + j
                            TR(pt[:, j, 0:np_], h2f[r_, dc * 128:(dc + 1) * 128], ident[r_, r_], [b_h2f, b_cst], [bpt])
                        CP("act" if grp % 2 == 0 else "dve", h2Tf[:, grp * 4:(grp + 1) * 4, 0:np_], pt[:, :, 0:np_], [bpt], [b_h2Tf])
                    for dc in range(16):
                        MM(plg[r_, 0:36], h2Tf[:, dc, 0:np_], wr_sb[:, dc, :], dc == 0, dc == 15, [b_h2Tf, b_wr], [b_plg])
                    TT("dve", lg[r_, :], plg[r_, 0:36], br_b[r_, :], ALU.add, [b_plg, b_wr], R_)
                    RED("dve", gmax[r_, :], lg[r_, 0:4], ALU.max, R_, R_)
                    TS("dve", ohg[r_, :], lg[r_, 0:4], gmax[r_, :], None, ALU.is_ge, None, R_, R_)
                    TS("dve", ngmax[r_, :], gmax[r_, :], -1.0, None, ALU.mult, None, R_, R_)
                    ACT(junk[r_, :], lg[r_, 0:4], AF.Exp, R_, R_, bias=ngmax[r_, :], scale=1.0, accum_out=sume[r_, :])
                    RECIP(gval[r_, :], sume[r_, :], R_, R_)
                    TT("dve", t32[r_, :].rearrange("p (g e) -> p g e", e=8), lg[r_, 4:36].rearrange("p (g e) -> p g e", e=8),
                       ohg[r_, :].unsqueeze(2).broadcast_to([np_, 4, 8]), ALU.mult, R_, R_)
                    RED("dve", les[r_, :], t32[r_, :].rearrange("p (g e) -> p e g", e=8), ALU.add, R_, R_)
                    RED("dve", m1[r_, :], les[r_, :], ALU.max, R_, R_)
                    TS("dve", oh1[r_, :], les[r_, :], m1[r_, :], None, ALU.is_ge, None, R_, R_)
                    STT("dve", msk[r_, :], oh1[r_, :], -1e30, les[r_, :], ALU.mult, ALU.add, R_, R_)
                    RED("dve", m2[r_, :], msk[r_, :], ALU.max, R_, R_)
                    TS("dve", oh2[r_, :], msk[r_, :], m2[r_, :], None, ALU.is_ge, None, R_, R_)
                    TT("dve", dd[r_, :], m2[r_, :], m1[r_, :], ALU.subtract, R_, R_)
                    ACT(e1[r_, :], dd[r_, :], AF.Exp, R_, R_)
                    TS("dve", w1[r_, :], e1[r_, :], 1.0, None, ALU.add, None, R_, R_)
                    RECIP(w1[r_, :], w1[r_, :], R_, R_)
                    TT("dve", w2[r_, :], e1[r_, :], w1[r_, :], ALU.mult, R_, R_)
                    TT("dve", w1[r_, :], w1[r_, :], gval[r_, :], ALU.mult, R_, R_)
                    TT("dve", w2[r_, :], w2[r_, :], gval[r_, :], ALU.mult, R_, R_)
                    TS("dve", ec[r_, :], oh1[r_, :], w1[r_, :], None, ALU.mult, None, R_, R_)
                    STT("dve", ec[r_, :], oh2[r_, :], w2[r_, :], ec[r_, :], ALU.mult, ALU.add, R_, R_)
                    TT("dve", comb[r_, n_, :].rearrange("p (g e) -> p g e", e=8),
                       ohg[r_, :].unsqueeze(2).broadcast_to([np_, 4, 8]),
                       ec[r_, :].unsqueeze(1).broadcast_to([np_, 4, 8]), ALU.mult, R_, [b_comb])

                norm_T(es1, [(xres_src(ti), np_, c0) for (ti, np_, c0) in TILES], norm_ffn, cTb, route=route)
                P.flush()
            acc = sbt(esE, "acc", [128, 9, D], F32); b_acc = [P.buf() for _ in range(9)]
            for (ti, np_, c0) in TILES:
                P.dma("sp" if ti % 2 == 0 else "act", acc[0:np_, ti, :], xres_src(ti), [], [b_acc[ti]], "acc")
            esM = contextlib.ExitStack()
            alloc_ring(esM)
            wd = sbt(esM, "wd", [128, 4, D], BF16); b_wd = P.buf()
            pg = [pst(esM, un("pg"), [128, 512], F32) for _ in range(2)]; b_pg = [P.buf(), P.buf()]
            pu = [pst(esM, un("pu"), [128, 512], F32) for _ in range(2)]; b_pu = [P.buf(), P.buf()]
            po = [pst(esM, un("po"), [128, 512], F32) for _ in range(3)]; b_po = [P.buf() for _ in range(3)]
            sg = [sbt(esM, un("sg"), [128, 512], F32) for _ in range(2)]; b_sg = [P.buf(), P.buf()]
            aT = [sbt(esM, un("aT"), [128, 4, 512], BF16) for _ in range(2)]; b_aT = [P.buf(), P.buf()]
            gcnt = 0
            ocnt = 0
            bcnt = 0
            NEXP = int(os.environ.get('KB_NEXP', 32))
            for e_ in range(NEXP):
                wg_, b_wg = stream_w(w_gate[e_], 16, 512)
                wu_, b_wu = stream_w(w_up[e_], 16, 512)
                P.dma("pool", wd[:], w_down[e_].rearrange("(c p) n -> p c n", p=128), [], [b_wd], "wd")
                for (n0, nn, tls) in [(0, 512, [0, 1, 2, 3]), (512, 512, [4, 5, 6, 7]), (1024, 16, [8])]:
                    a_ = aT[bcnt % 2]; ba = b_aT[bcnt % 2]
                    bcnt += 1
                    for fc in range(4):
                        k = gcnt % 2
                        gcnt += 1
                        for dc in range(16):
                            MM(pg[k][:, 0:nn], wg_[:, dc, fc * 128:(fc + 1) * 128], cTb[:, dc, n0:n0 + nn], dc == 0, dc == 15,
                               [b_wg], [b_pg[k]])
                        ACT(sg[k][:, 0:nn], pg[k][:, 0:nn], AF.Silu, [b_pg[k]], [b_sg[k]])
                        for dc in range(16):
                            MM(pu[k][:, 0:nn], wu_[:, dc, fc * 128:(fc + 1) * 128], cTb[:, dc, n0:n0 + nn], dc == 0, dc == 15,
                               [b_wu], [b_pu[k]])
                        TT("dve", a_[:, fc, 0:nn], pu[k][:, 0:nn], sg[k][:, 0:nn], ALU.mult, [b_pu[k], b_sg[k]], [ba])
                    for ti in tls:
                        (_, np_, c0) = TILES[ti]
                        for cb in range(4):
                            k = ocnt % 3
                            ocnt += 1
                            for fc in range(4):
                                MM(po[k][0:np_, :], a_[:, fc, c0 - n0:c0 - n0 + np_], wd[:, fc, cb * 512:(cb + 1) * 512],
                                   fc == 0, fc == 3, [ba, b_wd], [b_po[k]])
                            STT("dve", acc[0:np_, ti, cb * 512:(cb + 1) * 512], po[k][0:np_, :], comb[0:np_, ti, e_:e_ + 1],
                                acc[0:np_, ti, cb * 512:(cb + 1) * 512], ALU.mult, ALU.add, [b_po[k], b_acc[ti], b_comb], [b_acc[ti]])
            P.flush()
            esM.close()
            gbf = sbt(esE, "gbf", [128, D], F32); b_gbf = P.buf()
            P.dma("sp", gbf[:], norm_final.partition_broadcast(128), [], [b_gbf], "gbf")
            yt = [sbt(esE, un("yt"), [128, D], F32) for _ in range(2)]; b_yt = [P.buf(), P.buf()]
            ssf = sbt(esE, "ssf", [128, 1], F32); rsf = sbt(esE, "rsf", [128, 1], F32)
            for (ti, np_, c0) in TILES:
                y_ = yt[ti % 2]; by = b_yt[ti % 2]
                rmsnorm_tile(None, acc[:, ti, :], np_, gbf, y_, ssf, rsf, y_, [b_gbf, b_acc[ti]], [by], by)
                P.dma("sp" if ti % 2 == 0 else "act", y_o[c0:c0 + np_, :], y_[0:np_, :], [by], [], "yo%d" % (ti % 2))
            P.flush()
        P.flush(final=True)
    return nc


_CONSTS = None


def make_in_maps(inputs):
    global _CONSTS
    if _CONSTS is None:
        _CONSTS = build_consts()
    g = lambda k: np.ascontiguousarray(np.asarray(inputs[k], dtype=np.float32))
    xp = g("x_prompt"); xs = g("x_sample"); memp = g("mem_prompt")
    shared = {
        "consts": _CONSTS,
        "norm_mix": g("norm_mix")[0], "w_in": g("w_in")[0], "conv_w": g("conv_w")[0], "conv_b": g("conv_b")[0],
        "conv_ln_g": g("conv_ln_g")[0], "conv_ln_b": g("conv_ln_b")[0], "shift_mu": g("shift_mu")[0],
        "w_decay_up": g("w_decay_up")[0], "decay_bias": g("decay_bias")[0], "w_a_up": g("w_a_up")[0],
        "a_bias": g("a_bias")[0], "w_g_up": g("w_g_up")[0], "k_k": g("k_k")[0], "k_a": g("k_a")[0],
        "r_k": g("r_k")[0].reshape(1024), "lnx_g": g("lnx_g")[0], "lnx_b": g("lnx_b")[0],
        "w_out": g("w_out")[0], "norm_x": g("norm_x")[0], "norm_mem": g("norm_mem")[0],
        "w_cq": g("w_cq")[0], "w_ck": g("w_ck")[0], "w_cv": g("w_cv")[0], "w_co": g("w_co")[0],
        "norm_ffn": g("norm_ffn")[0],
        "w_route": np.ascontiguousarray(np.concatenate([g("w_route_group")[0], g("w_route_expert")[0].reshape(D, 32)], axis=1)),
        "b_route": np.ascontiguousarray(np.concatenate([g("b_route_group")[0], g("b_route_expert")[0].reshape(32)])),
        "w_gate": g("w_gate")[0].reshape(32, D, 512), "w_up": g("w_up")[0].reshape(32, D, 512),
        "w_down": g("w_down")[0].reshape(32, 512, D), "norm_final": g("norm_final"),
    }
    cc = g("cache_conv")[0]; ssh = g("state_shift")[0]; srw = g("state_rwkv")[0]
    cmk = g("cache_mem_k")[0].reshape(128, 256, D); cmv = g("cache_mem_v")[0].reshape(128, 256, D)
    maps = []
    for c in range(8):
        b, half = c // 2, c % 2
        if half == 0:
            xw = np.concatenate([np.zeros((1024, D), np.float32), xp[b, 0:1024]], axis=0)
        else:
            xw = xp[b]
        m = dict(shared)
        m.update({"xw": np.ascontiguousarray(xw), "xs": np.ascontiguousarray(xs[16 * c:16 * c + 16, 0]),
                  "mem": memp[b], "cconv": cc[16 * c:16 * c + 16], "sshift": ssh[16 * c:16 * c + 16],
                  "srwkv": srw[16 * c:16 * c + 16], "cmk": cmk[16 * c:16 * c + 16], "cmv": cmv[16 * c:16 * c + 16]})
        maps.append({k: v for k, v in m.items() if k in DECL})
    return maps


def gather(res):
    R = res.results
    y_p = np.zeros((4, 2048, D), np.float32); y_s = np.zeros((128, 1, D), np.float32)
    conv_p = np.zeros((1, 4, 30, CW), np.float32); shift_p = np.zeros((1, 4, SHIFT_W), np.float32)
    rwkv_p = np.zeros((1, 4, 16, 64, 64), np.float32)
    memk = np.zeros((1, 4, 256, 4, 512), np.float32); memv = np.zeros((1, 4, 256, 4, 512), np.float32)
    conv_s = np.zeros((1, 128, 30, CW), np.float32); shift_s = np.zeros((1, 128, SHIFT_W), np.float32)
    rwkv_s = np.zeros((1, 128, 16, 64, 64), np.float32)
    for c in range(8):
        b, half = c // 2, c % 2
        r = R[c]
        y_p[b, half * 1024:(half + 1) * 1024] = r["y_o"][0:1024]
        y_s[16 * c:16 * c + 16, 0] = r["y_o"][1024:1040]
        conv_s[0, 16 * c:16 * c + 16] = r["convs_o"]; shift_s[0, 16 * c:16 * c + 16] = r["shifts_o"]
        rwkv_s[0, 16 * c:16 * c + 16] = r["rwkvs_o"]
        if half == 1:
            conv_p[0, b] = r["convp_o"]; shift_p[0, b] = r["shiftp_o"][0]; rwkv_p[0, b] = r["rwkvp_o"]
            memk[0, b] = r["memk_o"].reshape(256, 4, 512); memv[0, b] = r["memv_o"].reshape(256, 4, 512)
    return (y_p, y_s, conv_p, shift_p, rwkv_p, memk, memv, conv_s, shift_s, rwkv_s)


_NC = None


def kernel(**inputs):
    global _NC
    if _NC is None:
        _NC = build_program()
    maps = make_in_maps(inputs)
    res = run_bass_kernel_spmd(_NC, maps, core_ids=list(range(8)))
    return gather(res)
```

```python
import contextlib
import math
import os
import numpy as np
import concourse.bass as bass
import concourse.mybir as mybir
from concourse.bass_utils import run_bass_kernel_spmd

F32 = mybir.dt.float32
BF16 = mybir.dt.bfloat16
AF = mybir.ActivationFunctionType
ALU = mybir.AluOpType
AX = mybir.AxisListType

D = 2048
NW = 16
NOWN = 8
SHIFT_W = 3360
IN_W = 5408
DS = math.exp(-0.5)
RMS_EPS = 1e-6
LN_EPS = 1e-5
GN_EPS = 64e-5
CW = 1024


class Buf:
    __slots__ = ("last_w", "readers")

    def __init__(self):
        self.last_w = None
        self.readers = []


class Op:
    __slots__ = ("eng", "fn", "deps", "idx", "eidx", "is_dma", "sig", "need", "key")


class Prog:
    EPOCH = 16000

    def __init__(self, nc, semfn):
        self.nc = nc
        self.semfn = semfn
        self.ops = []
        self.nops = 0
        self.ecount = {}
        self.engs = {"pe": nc.tensor, "act": nc.scalar, "dve": nc.vector,
                     "pool": nc.gpsimd, "sp": nc.sync}
        self.bufs = []
        self.esem = {}
        self.ecnt = {}
        self.dsem = {}
        self.dcnt = {}
        self.waited = {}
        self.last_on = {}
        self.bar_deps = []
        self.bar_id = 0
        self.eng_bar = {}
        self.nwait = 0
        self.free_dsems = []
        self.free_by = {}
        self.dcls = {}
        self.nds = 0

    def buf(self):
        b = Buf()
        self.bufs.append(b)
        return b

    def op(self, eng, fn, reads=(), writes=(), dma=False, key=None):
        o = Op()
        o.eng, o.fn, o.is_dma, o.key = eng, fn, dma, key
        o.sig = None
        o.need = dma
        o.idx = self.nops
        self.nops += 1
        o.eidx = self.ecount.get(eng, 0)
        self.ecount[eng] = o.eidx + 1
        deps = {}
        for b in reads:
            p = b.last_w
            if p is None:
                continue
            if (not p.is_dma) and p.eng == eng:
                if dma or (eng != "pe" and o.eidx - p.eidx <= 2):
                    deps[p.idx] = p
            else:
                deps[p.idx] = p
        for b in writes:
            p = b.last_w
            if p is not None and (p.is_dma or p.eng != eng or dma):
                deps[p.idx] = p
            for rd in b.readers:
                if rd.is_dma or rd.eng != eng or dma:
                    deps[rd.idx] = rd
        for b in reads:
            b.readers.append(o)
        for b in writes:
            b.last_w = o
            b.readers = []
        deps.pop(o.idx, None)
        o.deps = list(deps.values())
        for p in o.deps:
            p.need = True
        self.ops.append(o)
        self.last_on[eng] = o
        return o

    def dma(self, q, out, in_, reads, writes, key):
        if writes:
            key = ("w", id(writes[0]))
        return self.op(q, lambda e: e.dma_start(out=out, in_=in_), reads, writes, dma=True, key=key)

    def flush(self, final=False):
        self.nflush = getattr(self, "nflush", -1) + 1
        if self.nflush in [int(x) for x in os.environ.get("KB_DROP", "").split(",") if x]:
            self.ops = []
            self.last_on = {}
            for b in self.bufs:
                b.last_w = None
                b.readers = []
            return
        had_ops = len(self.ops) > 0
        for e, o in self.last_on.items():
            if o is not None and not o.is_dma:
                o.need = True
        nkey = {}
        for o in self.ops:
            if o.need and o.is_dma:
                nkey[o.key] = nkey.get(o.key, 0) + 1
        for o in self.ops:
            if not o.need:
                continue
            if o.is_dma:
                k = o.key
                cls = "sw" if o.eng == "pool" else "hw"
                if k not in self.dsem:
                    fl = self.free_by.setdefault(cls, [])
                    fl.sort(key=lambda sc: -sc[1])
                    self.dcls[k] = cls
                    if fl and fl[-1][1] + 16 * nkey[k] <= 24000:
                        self.dsem[k], self.dcnt[k] = fl.pop()
                    else:
                        self.nds += 1
                        self.dsem[k] = self.semfn("dsem%d" % self.nds)
                        self.dcnt[k] = 0
                assert self.dcls[k] == cls, (k, cls)
                self.dcnt[k] += 16
                o.sig = (self.dsem[k], self.dcnt[k], 16)
            else:
                c = self.ecnt.get(o.eng, 0)
                ep = c // self.EPOCH
                lst = self.esem.setdefault(o.eng, [])
                while len(lst) <= ep:
                    lst.append(self.semfn("e_%s_%d" % (o.eng, len(lst))))
                o.sig = (lst[ep], c % self.EPOCH + 1, 1)
                self.ecnt[o.eng] = c + 1
        for o in self.ops:
            e = self.engs[o.eng]
            w = self.waited.setdefault(o.eng, {})
            need = {}
            if self.eng_bar.get(o.eng, 0) < self.bar_id:
                self.eng_bar[o.eng] = self.bar_id
                for (s, v) in self.bar_deps:
                    need[id(s)] = (s, v)
            for p in o.deps:
                s, v, _ = p.sig
                sid = id(s)
                if sid not in need or need[sid][1] < v:
                    need[sid] = (s, v)
            for sid, (s, v) in need.items():
                if w.get(sid, 0) < v:
                    e.wait_ge(s, v)
                    w[sid] = v
                    self.nwait += 1
            ins = o.fn(e)
            if o.sig is not None:
                ins.then_inc(o.sig[0], o.sig[2])
        bd = []
        for e, o in self.last_on.items():
            if o is not None and not o.is_dma and o.sig is not None:
                bd.append((o.sig[0], o.sig[1]))
        for k, s in self.dsem.items():
            bd.append((s, self.dcnt[k]))
        if not had_ops:
            bd = bd + list(self.bar_deps)
        self.bar_deps = bd
        self.bar_id += 1
        for k in list(self.dsem.keys()):
            self.free_by.setdefault(self.dcls[k], []).append((self.dsem[k], self.dcnt[k]))
        self.dcls = {}
        self.dsem = {}
        self.dcnt = {}
        self.ops = []
        self.last_on = {}
        for b in self.bufs:
            b.last_w = None
            b.readers = []
        if final:
            for en in ("sp", "act", "dve", "pool", "pe"):
                e = self.engs[en]
                w = self.waited.setdefault(en, {})
                for (s, v) in bd:
                    if w.get(id(s), 0) < v:
                        e.wait_ge(s, v)
                        w[id(s)] = v


def build_consts():
    c = np.zeros((128, 1024), np.float32)
    c[:, 0:128] = np.eye(128)
    s = np.arange(128)[:, None]
    t = np.arange(128)[None, :]
    same = (s // 64) == (t // 64)
    c[:, 128:256] = (same & (s < t))
    c[:, 256:384] = (same & (s <= t))
    c[:, 384:512] = (same & (s > t))
    c[:, 512:640] = -DS * (same & (s <= t))
    c[:, 640:768] = -DS * (same & (s < t))
    c[:, 768:896] = -DS * (same & (s > t))
    c[63, 896] = 1.0
    c[127, 897] = 1.0
    for i in range(4):
        for sl in range(4):
            c[sl * 30:(sl + 1) * 30, 898 + i * 16 + 4 * i + sl] = 1.0
    c[:, 962] = 1.0
    return c


DECL = set()
KB_S5 = int(os.environ.get('KB_S5', 15))
KB_STAGE = int(os.environ.get('KB_STAGE', 100))


def build_program(stop_after=None):
    nc = bass.Bass("TRN2", target_bir_lowering=False)

    big = ("w_gate", "w_up", "w_down", "cmk", "cmv")
    DECL.clear()

    def din(name, shape):
        if (stop_after in ("A", "B", "C") and name in big) or (stop_after == "D" and name in big[0:3]):
            return None
        DECL.add(name)
        return nc.dram_tensor(name, list(shape), F32, kind="ExternalInput").ap()

    def dout(name, shape):
        return nc.dram_tensor(name, list(shape), F32, kind="ExternalOutput").ap()

    xw = din("xw", [NW * 128, D])
    xs = din("xs", [16, D])
    mem = din("mem", [256, D])
    cconv = din("cconv", [16, 30, CW])
    sshift = din("sshift", [16, SHIFT_W])
    srwkv = din("srwkv", [16, 16, 64, 64])
    cmk = din("cmk", [16, 256, D])
    cmv = din("cmv", [16, 256, D])
    consts = din("consts", [128, 1024])
    norm_mix = din("norm_mix", [D]); w_in = din("w_in", [D, IN_W])
    conv_w = din("conv_w", [31, CW]); conv_b = din("conv_b", [CW])
    conv_ln_g = din("conv_ln_g", [CW]); conv_ln_b = din("conv_ln_b", [CW])
    shift_mu = din("shift_mu", [SHIFT_W])
    w_decay_up = din("w_decay_up", [64, 1024]); decay_bias = din("decay_bias", [1024])
    w_a_up = din("w_a_up", [64, 1024]); a_bias = din("a_bias", [1024])
    w_g_up = din("w_g_up", [160, 1024])
    k_k = din("k_k", [1024]); k_a = din("k_a", [1024]); r_k = din("r_k", [1024])
    lnx_g = din("lnx_g", [1024]); lnx_b = din("lnx_b", [1024])
    w_out = din("w_out", [D, D]); norm_x = din("norm_x", [D]); norm_mem = din("norm_mem", [D])
    w_cq = din("w_cq", [D, D]); w_ck = din("w_ck", [D, D]); w_cv = din("w_cv", [D, D]); w_co = din("w_co", [D, D])
    norm_ffn = din("norm_ffn", [D])
    w_route = din("w_route", [D, 36]); b_route = din("b_route", [36])
    w_gate = din("w_gate", [32, D, 512]); w_up = din("w_up", [32, D, 512]); w_down = din("w_down", [32, 512, D])
    norm_final = din("norm_final", [D])

    y_o = dout("y_o", [1040, D])
    convp_o = dout("convp_o", [30, CW])
    shiftp_o = dout("shiftp_o", [1, SHIFT_W])
    rwkvp_o = dout("rwkvp_o", [16, 64, 64])
    memk_o = dout("memk_o", [256, D])
    memv_o = dout("memv_o", [256, D])
    convs_o = dout("convs_o", [16, 30, CW])
    shifts_o = dout("shifts_o", [16, SHIFT_W])
    rwkvs_o = dout("rwkvs_o", [16, 16, 64, 64])

    q_scr = nc.dram_tensor("q_scr", [1 + NW * 128 + 16, SHIFT_W], F32, kind="Internal").ap()
    xres = nc.dram_tensor("xres", [1040, D], F32, kind="Internal").ap()
    qtok_scr = nc.dram_tensor("qtok_scr", [16, D], F32, kind="Internal").ap()
    bscr = nc.dram_tensor("bscr", [NW * 128 + 16, 2, 5, 512], F32, kind="Internal").ap()

    top = contextlib.ExitStack()
    with top:
        P = Prog(nc, lambda n: top.enter_context(nc.semaphore(n)))

        def MM(out, lhsT, rhs, st, sp, R, W):
            P.op("pe", lambda e: e.matmul(out, lhsT, rhs, start=st, stop=sp), R, W)

        def TR(out, in_, idn, R, W):
            P.op("pe", lambda e: e.transpose(out, in_, idn), R, W)

        def ACT(out, in_, func, R, W, **kw):
            P.op("act", lambda e: e.activation(out, in_, func, **kw), R, W)

        def TT(eng, out, a, b, op, R, W):
            P.op(eng, lambda e: e.tensor_tensor(out, a, b, op), R, W)

        def TS(eng, out, a, s1, s2, op0, op1, R, W):
            if s2 is None:
                P.op(eng, lambda e: e.tensor_scalar(out, a, s1, None, op0), R, W)
            else:
                P.op(eng, lambda e: e.tensor_scalar(out, a, s1, s2, op0, op1), R, W)

        def STT(eng, out, a, s, b, op0, op1, R, W):
            P.op(eng, lambda e: e.scalar_tensor_tensor(out, a, s, b, op0, op1), R, W)

        def CP(eng, out, in_, R, W):
            if eng == "act":
                P.op(eng, lambda e: e.copy(out, in_), R, W)
            else:
                P.op(eng, lambda e: e.tensor_copy(out, in_), R, W)

        def RED(eng, out, in_, op, R, W):
            P.op(eng, lambda e: e.tensor_reduce(out, in_, AX.X, op), R, W)

        def MEMSET(eng, ap, val, W):
            P.op(eng, lambda e: e.memset(ap, val), [], W)

        def RECIP(out, in_, R, W):
            P.op("dve", lambda e: e.reciprocal(out, in_), R, W)

        def RSQRT(ap, R, W):
            ACT(ap, ap, AF.Sqrt, R, W)
            RECIP(ap, ap, R, W)

        def sbt(es, name, shape, dt):
            return es.enter_context(nc.sbuf_tensor(name, list(shape), dt))

        def pst(es, name, shape, dt):
            return es.enter_context(nc.psum_tensor(name, list(shape), dt))

        cst = sbt(top, "cst", [128, 1024], F32); b_cst = P.buf()
        cTb = sbt(top, "cTb", [128, 16, 1040], BF16)
        identb = sbt(top, "identb", [128, 128], BF16)
        ident = cst[:, 0:128]
        mask_ur = cst[:, 128:384]
        mask_sl = cst[:, 384:512]
        tri_le = cst[:, 512:640]; tri_lt = cst[:, 640:768]; tri_gt = cst[:, 768:896]
        esel = cst[:, 896:898]
        ones_col = cst[:, 962:963]
        P.dma("sp", cst[:], consts, [], [b_cst], "cst")
        if os.environ.get("KB_SIMINIT"):
            P.op("dve", lambda e: e.memset(cTb[:], 0.0), [], [b_cst])
        CP("dve", identb[:], ident, [b_cst], [b_cst])
        P.flush()

        rr = [0]

        def rmsnorm_tile(es_bufs, xt, np_, gb, hb, ss, rstd, tmp, R, W, b_tmp):
            ACT(tmp[0:np_, :], xt[0:np_, :], AF.Square, R, [b_tmp], accum_out=ss[0:np_, :])
            TS("dve", rstd[0:np_, :], ss[0:np_, :], 1.0 / D, RMS_EPS, ALU.mult, ALU.add, [b_tmp], [b_tmp])
            RSQRT(rstd[0:np_, :], [b_tmp], [b_tmp])
            STT("dve", hb[0:np_, :], xt[0:np_, :], rstd[0:np_, 0:1], gb[0:np_, :], ALU.mult, ALU.mult,
                R + [b_tmp], W)

        NSLOT = 2
        wring = [None] * NSLOT
        b_ring = [None] * NSLOT
        ring_ctr = [0]
        ring_gen = [0]

        def alloc_ring(es_):
            ring_gen[0] += 1
            for i_ in range(NSLOT):
                wring[i_] = sbt(es_, "wring%d_%d" % (i_, ring_gen[0]), [128, 16, 512], BF16)
                b_ring[i_] = None

        def stream_w(src_ap, kc, ncols):
            i = ring_ctr[0] % NSLOT
            ring_ctr[0] += 1
            if b_ring[i] is None or b_ring[i] not in P.bufs:
                b_ring[i] = P.buf()
            P.dma("pool", wring[i][:, 0:kc, 0:ncols], src_ap.rearrange("(c p) n -> p c n", p=128),
                  [], [b_ring[i]], "ring%d" % i)
            return wring[i], b_ring[i]

        with contextlib.ExitStack() as es:
            uT = sbt(es, "uT", [128, 8, 1072], F32); b_uT = [P.buf() for _ in range(8)]
            esA = contextlib.ExitStack()
            alloc_ring(esA)
            NT = NW * 128 + 16
            hT = sbt(esA, "hT", [128, 16, NT], BF16); b_hT = P.buf()
            gb = sbt(esA, "gb", [128, D], F32); b_gb = P.buf()
            P.dma("sp", gb[:], norm_mix.partition_broadcast(128), [], [b_gb], "gb")
            xt2 = [sbt(esA, "xt0", [128, D], F32)] * 2
            b_xt = [P.buf()] * 2
            hb = sbt(esA, "hb", [128, D], BF16); b_hb = P.buf()
            ss = sbt(esA, "ss", [128, 1], F32); rstd = sbt(esA, "rstd", [128, 1], F32); b_tmp = P.buf()
            pT = [pst(esA, "pT%d" % i, [128, 8, 128], BF16) for i in range(2)]
            b_pT = [P.buf() for _ in range(2)]
            zrow = sbt(esA, "zrow", [105, 32], F32); b_z = P.buf()
            MEMSET("dve", zrow[:], 0.0, [b_z])
            P.dma("sp", q_scr[0, :].rearrange("(p n) -> p n", n=32), zrow[:], [b_z], [], "zrow")
            for i in range(NW + 1):
                np_ = 128 if i < NW else 16
                xt = xt2[i % 2]; bx = b_xt[i % 2]
                src = xw[i * 128:(i + 1) * 128, :] if i < NW else xs
                P.dma("sp" if i % 2 == 0 else "act", xt[0:np_, :], src, [], [bx], "xl0")
                rmsnorm_tile(es, xt, np_, gb, hb, ss, rstd, hb, [bx, b_gb], [b_hb], b_hb)
                for hf in range(2):
                    for j in range(8):
                        dc = hf * 8 + j
                        TR(pT[hf][0:128, j, 0:np_], hb[0:np_, dc * 128:(dc + 1) * 128], identb[0:np_, 0:np_],
                           [b_hb, b_cst], [b_pT[hf]])
                    CP("act" if hf == 0 else "dve", hT[:, hf * 8:(hf + 1) * 8, i * 128:i * 128 + np_],
                       pT[hf][:, :, 0:np_], [b_pT[hf]], [b_hT])
            pq = [pst(esA, "pq%d" % i, [128, 512], F32) for i in range(3)]
            b_pq = [P.buf() for _ in range(3)]
            qst = [sbt(esA, "qst%d" % i, [128, 512], F32) for i in range(3)]
            b_qst = [P.buf() for _ in range(3)]
            cnt = 0
            for cb in range(7):
                c0 = 2048 + cb * 512
                ncol = min(512, IN_W - c0)
                slot, bs = stream_w(w_in[:, c0:c0 + ncol], 16, ncol)
                for i in range(NW + 1):
                    np_ = 128 if i < NW else 16
                    k = cnt % 3
                    cnt += 1
                    for dc in range(16):
                        MM(pq[k][0:np_, 0:ncol], hT[:, dc, i * 128:i * 128 + np_], slot[:, dc, 0:ncol],
                           dc == 0, dc == 15, [b_hT, bs], [b_pq[k]])
                    CP("act" if cnt % 2 == 0 else "dve", qst[k][0:np_, 0:ncol], pq[k][0:np_, 0:ncol],
                       [b_pq[k]], [b_qst[k]])
                    P.dma("sp", q_scr[1 + i * 128:1 + i * 128 + np_, c0 - 2048:c0 - 2048 + ncol],
                          qst[k][0:np_, 0:ncol], [b_qst[k]], [], "qst%d" % k)
            chunks = [(992, 512), (1504, 512), (2016, 48)]
            sg = [sbt(esA, "sg%d" % i, [128, 512], F32) for i in range(2)]
            b_sg = [P.buf() for _ in range(2)]
            for cb in range(4):
                slot, bs = stream_w(w_in[:, cb * 512:(cb + 1) * 512], 16, 512)
                for fb in range(4):
                    fblk = cb * 4 + fb
                    for (n0, nn) in chunks:
                        k = cnt % 3
                        cnt += 1
                        for dc in range(16):
                            MM(pq[k][:, 0:nn], slot[:, dc, fb * 128:(fb + 1) * 128], hT[:, dc, n0:n0 + nn],
                               dc == 0, dc == 15, [b_hT, bs], [b_pq[k]])
                        if fblk < 8:
                            CP("dve", uT[:, fblk, n0 - 992:n0 - 992 + nn], pq[k][:, 0:nn], [b_pq[k]], [b_uT[fblk]])
                        else:
                            ACT(sg[k % 2][:, 0:nn], pq[k][:, 0:nn], AF.Sigmoid, [b_pq[k]], [b_sg[k % 2]])
                            TT("dve", uT[:, fblk - 8, n0 - 992:n0 - 992 + nn], uT[:, fblk - 8, n0 - 992:n0 - 992 + nn],
                               sg[k % 2][:, 0:nn], ALU.mult, [b_sg[k % 2], b_uT[fblk - 8]], [b_uT[fblk - 8]])
            P.flush()
            esA.close()
            P.dma("sp", shiftp_o, q_scr[NW * 128:NW * 128 + 1, :], [], [], "sho")
            P.dma("sp", shifts_o, q_scr[1 + NW * 128:1 + NW * 128 + 16, :], [], [], "sho")

            with contextlib.ExitStack() as es2:
                cw_tm = sbt(es2, "cw_tm", [31, CW], F32); b_cw = P.buf()
                P.dma("sp", cw_tm[:], conv_w, [], [b_cw], "cw")
                pv = sbt(es2, "pv", [24, 128], F32); b_pv = P.buf()
                P.dma("sp", pv[0:8, :], conv_b.rearrange("(b p) -> b p", p=128), [], [b_pv], "cw")
                P.dma("sp", pv[8:16, :], conv_ln_g.rearrange("(b p) -> b p", p=128), [], [b_pv], "cw")
                P.dma("sp", pv[16:24, :], conv_ln_b.rearrange("(b p) -> b p", p=128), [], [b_pv], "cw")
                cwT = sbt(es2, "cwT", [128, 8, 31], F32); b_cwT = P.buf()
                pvT = sbt(es2, "pvT", [128, 24], F32)
                pc = pst(es2, "pc", [128, 8, 32], F32); b_pc = P.buf()
                pc2 = pst(es2, "pc2", [128, 32], F32); b_pc2 = P.buf()
                for blk in range(8):
                    TR(pc[:, blk, 0:31], cw_tm[:, blk * 128:(blk + 1) * 128], ident[0:31, 0:31], [b_cw, b_cst], [b_pc])
                CP("dve", cwT[:], pc[:, :, 0:31], [b_pc], [b_cwT])
                TR(pc2[:, 0:24], pv[:], ident[0:24, 0:24], [b_pv, b_cst], [b_pc2])
                CP("dve", pvT[:], pc2[:, 0:24], [b_pc2], [b_cwT])
                cT = sbt(es2, "cT", [128, 8, 1024], F32); b_cT = [P.buf() for _ in range(8)]
                for blk in range(8):
                    eng = "dve"
                    TS(eng, cT[:, blk, :], uT[:, blk, 2:1026], cwT[:, blk, 0:1], pvT[:, blk:blk + 1], ALU.mult, ALU.add,
                       [b_uT[blk], b_cwT], [b_cT[blk]])
                    for j in range(1, 31):
                        STT(eng, cT[:, blk, :], uT[:, blk, 2 + j:1026 + j], cwT[:, blk, j:j + 1], cT[:, blk, :],
                            ALU.mult, ALU.add, [b_uT[blk], b_cwT, b_cT[blk]], [b_cT[blk]])
                urow = sbt(es2, "urow", [30, CW], F32); b_urow = P.buf()
                us = sbt(es2, "us", [16, CW], F32); b_us = P.buf()
                pu = pst(es2, "pu", [32, 1024], F32); b_pu = P.buf()
                for blk in range(8):
                    TR(pu[0:30, blk * 128:(blk + 1) * 128], uT[:, blk, 1026:1056], ident, [b_uT[blk], b_cst], [b_pu])
                CP("act", urow[:], pu[0:30, :], [b_pu], [b_urow])
                P.dma("sp", convp_o, urow[:], [b_urow], [], "cpo")
                for blk in range(8):
                    TR(pu[0:16, blk * 128:(blk + 1) * 128], uT[:, blk, 1056:1072], ident, [b_uT[blk], b_cst], [b_pu])
                CP("act", us[:], pu[0:16, :], [b_pu], [b_us])
                P.dma("sp", convs_o[:, 29, :], us[:], [b_us], [], "cso")
                P.dma("act", convs_o[:, 0:29, :], cconv[:, 1:30, :], [], [], "cso2")
                onesm = sbt(es2, "onesm", [128, 128], F32); b_ones = P.buf()
                MEMSET("dve", onesm[:], 1.0 / CW, [b_ones])
                ps1 = pst(es2, "ps1", [128, 512], F32); b_ps1 = P.buf()
                ps2 = pst(es2, "ps2", [128, 512], F32); b_ps2 = P.buf()
                sqc = [sbt(es2, "sqc%d" % i, [128, 512], F32) for i in range(2)]
                b_sqc = [P.buf() for _ in range(2)]
                mean = sbt(es2, "mean", [128, 512], F32); rs = sbt(es2, "rs", [128, 512], F32); b_st = P.buf()
                tn = [sbt(es2, "tn%d" % i, [128, 512], F32) for i in range(2)]
                b_tn = [P.buf() for _ in range(2)]
                b_cTb = P.buf()
                for ncx in range(2):
                    n0 = ncx * 512
                    for blk in range(8):
                        MM(ps1[:], onesm[:], cT[:, blk, n0:n0 + 512], blk == 0, blk == 7, [b_ones, b_cT[blk]], [b_ps1])
                        ACT(sqc[blk % 2][:], cT[:, blk, n0:n0 + 512], AF.Square, [b_cT[blk]], [b_sqc[blk % 2]])
                        MM(ps2[:], onesm[:], sqc[blk % 2][:], blk == 0, blk == 7, [b_ones, b_sqc[blk % 2]], [b_ps2])
                    CP("dve", mean[:], ps1[:], [b_ps1], [b_st])
                    TT("dve", rs[:], mean[:], mean[:], ALU.mult, [b_st], [b_st])
                    TT("dve", rs[:], ps2[:], rs[:], ALU.subtract, [b_ps2, b_st], [b_st])
                    TS("dve", rs[:], rs[:], LN_EPS, None, ALU.add, None, [b_st], [b_st])
                    RSQRT(rs[:], [b_st], [b_st])
                    for blk in range(8):
                        t = tn[blk % 2]; bt = b_tn[blk % 2]
                        TT("dve", t[:], cT[:, blk, n0:n0 + 512], mean[:], ALU.subtract, [b_cT[blk], b_st], [bt])
                        TT("dve", t[:], t[:], rs[:], ALU.mult, [bt, b_st], [bt])
                        ACT(cTb[:, blk, n0:n0 + 512], t[:], AF.Silu, [bt, b_cwT], [b_cTb],
                            bias=pvT[:, 16 + blk:17 + blk], scale=pvT[:, 8 + blk:9 + blk])
                wrep = sbt(es2, "wrep", [120, CW], F32); b_wrep = P.buf()
                for r4 in range(4):
                    P.dma("sp", wrep[r4 * 30:(r4 + 1) * 30, :], conv_w[0:30, :], [], [b_wrep], "wrep")
                bc16 = sbt(es2, "bc16", [16, 4, CW], F32); b_bc16 = P.buf()
                P.dma("sp", bc16[:, 0, :], conv_w[30, :].partition_broadcast(16), [], [b_bc16], "bc16")
                P.dma("sp", bc16[:, 1, :], conv_b.partition_broadcast(16), [], [b_bc16], "bc16")
                P.dma("sp", bc16[:, 2, :], conv_ln_g.partition_broadcast(16), [], [b_bc16], "bc16")
                P.dma("sp", bc16[:, 3, :], conv_ln_b.partition_broadcast(16), [], [b_bc16], "bc16")
                cch = [sbt(es2, "cch%d" % i, [120, CW], F32) for i in range(2)]
                b_cch = [P.buf() for _ in range(2)]
                pcs = pst(es2, "pcs", [16, 1024], F32); b_pcs = P.buf()
                for i4 in range(4):
                    t = cch[i4 % 2]; bt = b_cch[i4 % 2]
                    P.dma("sp", t[:], cconv[i4 * 4:(i4 + 1) * 4, :, :].rearrange("s j c -> (s j) c"), [], [bt], "cch%d" % (i4 % 2))
                    TT("dve", t[:], t[:], wrep[:], ALU.mult, [bt, b_wrep], [bt])
                    for hf in range(2):
                        MM(pcs[:, hf * 512:(hf + 1) * 512], cst[0:120, 898 + i4 * 16:898 + (i4 + 1) * 16],
                           t[:, hf * 512:(hf + 1) * 512], i4 == 0, i4 == 3, [bt, b_cst], [b_pcs])
                cs = sbt(es2, "cs", [16, CW], F32); b_cs = P.buf()
                cs2 = sbt(es2, "cs2", [16, CW], F32)
                st16 = sbt(es2, "st16", [16, 4], F32)
                TT("dve", cs[:], us[:], bc16[:, 0, :], ALU.mult, [b_us, b_bc16], [b_cs])
                TT("dve", cs[:], cs[:], pcs[:], ALU.add, [b_cs, b_pcs], [b_cs])
                TT("dve", cs[:], cs[:], bc16[:, 1, :], ALU.add, [b_cs, b_bc16], [b_cs])
                ACT(cs2[:], cs[:], AF.Copy, [b_cs], [b_cs], accum_out=st16[:, 0:1])
                TS("dve", st16[:, 0:1], st16[:, 0:1], 1.0 / CW, None, ALU.mult, None, [b_cs], [b_cs])
                TS("dve", cs[:], cs[:], st16[:, 0:1], None, ALU.subtract, None, [b_cs], [b_cs])
                ACT(cs2[:], cs[:], AF.Square, [b_cs], [b_cs], accum_out=st16[:, 1:2])
                TS("dve", st16[:, 1:2], st16[:, 1:2], 1.0 / CW, LN_EPS, ALU.mult, ALU.add, [b_cs], [b_cs])
                RSQRT(st16[:, 1:2], [b_cs], [b_cs])
                STT("dve", cs[:], cs[:], st16[:, 1:2], bc16[:, 2, :], ALU.mult, ALU.mult, [b_cs, b_bc16], [b_cs])
                TT("dve", cs[:], cs[:], bc16[:, 3, :], ALU.add, [b_cs, b_bc16], [b_cs])
                ACT(cs2[:], cs[:], AF.Silu, [b_cs], [b_cs])
                for blk in range(8):
                    TR(pc[:, blk, 0:16], cs2[:, blk * 128:(blk + 1) * 128], ident[0:16, 0:16], [b_cs, b_cst], [b_pc])
                CP("dve", cTb[:, 0:8, 1024:1040], pc[:, :, 0:16], [b_pc], [b_cTb])
                P.flush()
        if stop_after == "A":
            P.flush(final=True)
            return nc

        def load_bc(es_, name, src, n, np_=128):
            t = sbt(es_, name, [np_, n], F32)
            b = P.buf()
            P.dma("sp", t[:], src.partition_broadcast(np_), [], [b], "bc")
            return t, b

        with contextlib.ExitStack() as es:
            mu_b, b_mu = load_bc(es, "mu_b", shift_mu, SHIFT_W)
            kk_b, b_par = load_bc(es, "kk_b", k_k, 1024)
            ka_b, _b = load_bc(es, "ka_b", k_a, 1024); rk_b, _b2 = load_bc(es, "rk_b", r_k, 1024)
            lg_b, _b3 = load_bc(es, "lg_b", lnx_g, 1024); lb_b, _b4 = load_bc(es, "lb_b", lnx_b, 1024)
            b_pars = [b_par, _b, _b2, _b3, _b4]
            wdu = sbt(es, "wdu", [65, 1024], F32); wau = sbt(es, "wau", [65, 1024], F32)
            wgu = sbt(es, "wgu", [128, 2, 1024], F32); b_lw = P.buf()
            P.dma("sp", wdu[0:64, :], w_decay_up, [], [b_lw], "lw")
            P.dma("sp", wdu[64:65, :], decay_bias.rearrange("(o n) -> o n", o=1), [], [b_lw], "lw")
            P.dma("sp", wau[0:64, :], w_a_up, [], [b_lw], "lw")
            P.dma("sp", wau[64:65, :], a_bias.rearrange("(o n) -> o n", o=1), [], [b_lw], "lw")
            P.dma("sp", wgu[:, 0, :], w_g_up[0:128, :], [], [b_lw], "lw")
            P.dma("sp", wgu[0:32, 1, :], w_g_up[128:160, :], [], [b_lw], "lw")
            q = sbt(es, "q", [128, SHIFT_W], F32); b_q = P.buf()
            qp = sbt(es, "qp", [128, SHIFT_W], F32); b_qp = P.buf()
            Wt_full = [sbt(es, "W%d" % i, [128, 1024], F32) for i in range(3, 7)]
            Wt = [qp[:, 0:1024], qp[:, 1024:2048], qp[:, 2048:3072]] + [w_[:] for w_ in Wt_full]
            b_W = [b_qp, b_qp, b_qp] + [P.buf() for _ in range(4)]
            loT = sbt(es, "loT", [128, 4, 128], F32); b_loT = P.buf()
            MEMSET("dve", loT[:], 1.0, [b_loT])
            lo_in = sbt(es, "lo_in", [128, 288], F32); b_loin = P.buf()
            sm = sbt(es, "sm", [128, 64], F32); b_sm = P.buf()
            Ot = Wt[3]; b_Ot = b_W[3]
            W7 = sbt(es, "W7", [128, 1024], F32); b_W7 = P.buf()
            S = sbt(es, "S", [128, 8, 64], F32); b_S = P.buf()
            b_S2 = [P.buf(), P.buf()]; b_tA2 = [P.buf(), P.buf()]; b_Sw2 = [P.buf(), P.buf()]; b_sa2 = [P.buf(), P.buf()]
            b_tB2 = [P.buf(), P.buf()]; b_tC2 = [P.buf(), P.buf()]; b_tD2 = [[P.buf(), P.buf()], [P.buf(), P.buf()]]
            MEMSET("dve", S[:], 0.0, b_S2)
            tA = sbt(es, "tA", [128, 8, 64], F32); b_tA = P.buf()
            Sw = sbt(es, "Sw", [128, 8, 64], F32); b_Sw = P.buf()
            tB = sbt(es, "tB", [128, 8, 64], F32); b_tB = P.buf()
            tC = sbt(es, "tC", [128, 8, 64], F32); b_tC = P.buf()
            tD = [sbt(es, "tD%d" % i, [128, 8, 64], F32) for i in range(2)]; b_tD = [P.buf() for _ in range(2)]
            sa = sbt(es, "sa", [128, 8], F32); b_sa = P.buf()
            vT = sbt(es, "vT", [128, 8, 128], F32); b_vT = P.buf()
            oT = sbt(es, "oT", [128, 8, 128], F32); b_oT = P.buf()
            NB = 4
            bc = [sbt(es, "bc%d" % i, [128, 2560], F32) for i in range(NB)]; b_bc = [P.buf() for _ in range(NB)]
            b_scr = P.buf()
            bBp = [P.buf() for _ in range(NB)]
            bk = [pst(es, "bk%d" % i, [128, 512], F32) for i in range(8)]
            b_bk = [P.buf() for _ in range(8)]
            pL = [bk[0], bk[1]]; b_pL = [b_bk[0], b_bk[1]]
            pA = [bk[3], bk[4]]; b_pA = [b_bk[3], b_bk[4]]
            pB = [bk[2][:].rearrange("p (a b) -> p a b", b=128), bk[5][:].rearrange("p (a b) -> p a b", b=128)]
            b_pB = [b_bk[2], b_bk[5]]
            vps = [bk[6][:].rearrange("p (a b) -> p a b", b=128), bk[7][:].rearrange("p (a b) -> p a b", b=128)]

            def vec_part(np_, own):
                r_ = slice(0, np_)
                TT("dve", qp[r_, :], qp[r_, :], q[r_, :], ALU.subtract, [b_qp, b_q], [b_qp])
                TT("pool", qp[r_, :], qp[r_, :], mu_b[r_, :], ALU.mult, [b_qp, b_mu], [b_qp])
                TT("dve", q[r_, :], q[r_, :], qp[r_, :], ALU.add, [b_qp, b_q], [b_q])
                ACT(lo_in[r_, 0:64], q[r_, 3072:3136], AF.Tanh, [b_q], [b_loin])
                CP("act", lo_in[r_, 64:128], q[r_, 3136:3200], [b_q], [b_loin])
                ACT(lo_in[r_, 128:288], q[r_, 3200:3360], AF.Sigmoid, [b_q], [b_loin])
                k = 0
                TR(pB[k][0:64, 0, 0:np_], lo_in[r_, 0:64], ident[r_, r_], [b_loin, b_cst], [b_pB[k]])
                TR(pB[k][0:64, 1, 0:np_], lo_in[r_, 64:128], ident[r_, r_], [b_loin, b_cst], [b_pB[k]])
                TR(pB[k][0:128, 2, 0:np_], lo_in[r_, 128:256], ident[r_, r_], [b_loin, b_cst], [b_pB[k]])
                TR(pB[k][0:32, 3, 0:np_], lo_in[r_, 256:288], ident[r_, r_], [b_loin, b_cst], [b_pB[k]])
                CP("dve", loT[0:64, 0:2, 0:np_], pB[k][0:64, 0:2, 0:np_], [b_pB[k]], [b_loT])
                CP("dve", loT[:, 2, 0:np_], pB[k][:, 2, 0:np_], [b_pB[k]], [b_loT])
                CP("dve", loT[0:32, 3, 0:np_], pB[k][0:32, 3, 0:np_], [b_pB[k]], [b_loT])
                for hf in range(2):
                    c = slice(hf * 512, (hf + 1) * 512)
                    MM(pL[0][r_, :], loT[0:65, 0, r_], wdu[0:65, c], True, True, [b_loT, b_lw], [b_pL[0]])
                    ACT(Wt[0][r_, c], pL[0][r_, :], AF.Sigmoid, [b_pL[0]], [b_W[0]])
                    MM(pL[1][r_, :], loT[0:65, 1, r_], wau[0:65, c], True, True, [b_loT, b_lw], [b_pL[1]])
                    ACT(Wt[1][r_, c], pL[1][r_, :], AF.Sigmoid, [b_pL[1]], [b_W[1]])
                    if own:
                        MM(pA[hf][r_, :], loT[0:128, 2, r_], wgu[:, 0, c], True, False, [b_loT, b_lw], [b_pA[hf]])
                        MM(pA[hf][r_, :], loT[0:32, 3, r_], wgu[0:32, 1, c], False, True, [b_loT, b_lw], [b_pA[hf]])
                        CP("act", Wt[2][r_, c], pA[hf][r_, :], [b_pA[hf]], [b_W[2]])
                kv = q[r_, 1024:2048]; rv = q[r_, 0:1024]
                a = Wt[1][r_, :]
                TT("dve", Wt[3][r_, :], kv, kk_b[r_, :], ALU.mult, [b_q] + b_pars, [b_W[3]])
                TT("pool", Wt[5][r_, :], Wt[3][r_, :], Wt[3][r_, :], ALU.mult, [b_W[3]], [b_W[5]])
                RED("dve", sm[r_, 16:32], Wt[5][r_, :].rearrange("p (h k) -> p h k", k=64), ALU.add, [b_W[5]], [b_sm])
                ACT(sm[r_, 16:32], sm[r_, 16:32], AF.Sqrt, [b_sm], [b_sm])
                TS("dve", sm[r_, 16:32], sm[r_, 16:32], 1e-12, None, ALU.max, None, [b_sm], [b_sm])
                RECIP(sm[r_, 16:32], sm[r_, 16:32], [b_sm], [b_sm])
                TT("dve", Wt[3][r_, :].rearrange("p (h k) -> p h k", k=64), Wt[3][r_, :].rearrange("p (h k) -> p h k", k=64),
                   sm[r_, 16:32].unsqueeze(2).broadcast_to([np_, 16, 64]), ALU.mult, [b_W[3], b_sm], [b_W[3]])
                TT("pool", Wt[4][r_, :], Wt[3][r_, :], a, ALU.mult, [b_W[3], b_W[1]], [b_W[4]])
                STT("dve", Wt[5][r_, :], a, -1.0, ka_b[r_, :], ALU.add, ALU.mult, [b_W[1]] + b_pars, [b_W[5]])
                STT("dve", Wt[5][r_, :], Wt[5][r_, :], 1.0, kv, ALU.add, ALU.mult, [b_W[5], b_q], [b_W[5]])
                if own:
                    TT("pool", Wt[6][r_, :], rv, rk_b[r_, :], ALU.mult, [b_q] + b_pars, [b_W[6]])
                    TT("pool", Wt[6][r_, :], Wt[6][r_, :], Wt[5][r_, :], ALU.mult, [b_W[6], b_W[5]], [b_W[6]])
                    RED("dve", sm[r_, 0:16], Wt[6][r_, :].rearrange("p (h k) -> p h k", k=64), ALU.add, [b_W[6]], [b_sm])

            def out_part(np_, col0):
                r_ = slice(0, np_)
                O3 = Ot[r_, :].rearrange("p (h k) -> p h k", k=64)
                RED("dve", sm[r_, 32:48], O3, ALU.add, [b_Ot], [b_sm])
                TS("dve", sm[r_, 32:48], sm[r_, 32:48], 1.0 / 64, None, ALU.mult, None, [b_sm], [b_sm])
                TT("dve", O3, O3, sm[r_, 32:48].unsqueeze(2).broadcast_to([np_, 16, 64]), ALU.subtract, [b_Ot, b_sm], [b_Ot])
                TT("pool", Wt[6][r_, :], Ot[r_, :], Ot[r_, :], ALU.mult, [b_Ot], [b_W[6]])
                RED("dve", sm[r_, 48:64], Wt[6][r_, :].rearrange("p (h k) -> p h k", k=64), ALU.add, [b_W[6]], [b_sm])
                TS("dve", sm[r_, 48:64], sm[r_, 48:64], 1.0 / 64, GN_EPS, ALU.mult, ALU.add, [b_sm], [b_sm])
                RSQRT(sm[r_, 48:64], [b_sm], [b_sm])
                TT("dve", O3, O3, sm[r_, 48:64].unsqueeze(2).broadcast_to([np_, 16, 64]), ALU.mult, [b_Ot, b_sm], [b_Ot])
                TT("pool", Ot[r_, :], Ot[r_, :], lg_b[r_, :], ALU.mult, [b_Ot] + b_pars, [b_Ot])
                TT("dve", Ot[r_, :], Ot[r_, :], lb_b[r_, :], ALU.add, [b_Ot] + b_pars, [b_Ot])
                V3 = q[r_, 2048:3072].rearrange("p (h k) -> p h k", k=64)
                W63 = Wt[6][r_, :].rearrange("p (h k) -> p h k", k=64)
                TT("dve", W63, V3, sm[r_, 0:16].unsqueeze(2).broadcast_to([np_, 16, 64]), ALU.mult, [b_q, b_sm], [b_W[6]])
                TT("dve", Ot[r_, :], Ot[r_, :], Wt[6][r_, :], ALU.add, [b_Ot, b_W[6]], [b_Ot])
                TT("dve", Ot[r_, :], Ot[r_, :], Wt[2][r_, :], ALU.mult, [b_Ot, b_W[2]], [b_Ot])
                for hf in range(2):
                    for j in range(4):
                        blk = hf * 4 + j
                        TR(pB[hf][:, j, 0:np_], Ot[r_, blk * 128:(blk + 1) * 128], ident[r_, r_], [b_Ot, b_cst], [b_pB[hf]])
                    CP("act", cTb[:, 8 + hf * 4:12 + hf * 4, col0:col0 + np_], pB[hf][:, :, 0:np_], [b_pB[hf]], [b_cTbB])

            b_cTbB = P.buf()
            NTL = int(os.environ.get('KB_TILES', NW))
            for i in list(range(NTL)) + [NW]:
                samp = (i == NW)
                np_ = 16 if samp else 128
                own = samp or i >= NW - NOWN
                r_ = slice(0, np_)
                if samp:
                    P.dma("sp", q[r_, :], q_scr[1 + NW * 128:1 + NW * 128 + 16, :], [], [b_q], "ql")
                    P.dma("act", qp[r_, :], sshift, [], [b_qp], "qpl")
                else:
                    P.dma("sp", q[:], q_scr[1 + i * 128:1 + (i + 1) * 128, :], [], [b_q], "ql")
                    P.dma("act", qp[:], q_scr[i * 128:(i + 1) * 128, :], [], [b_qp], "qpl")
                vec_part(np_, own)
                if samp:
                    ACT(Wt[6][r_, :], Wt[0][r_, :], AF.Exp, [b_W[0], b_sm], [b_W[6]], scale=-DS)
                    ACT(W7[r_, :], Wt[0][r_, :], AF.Exp, [b_W[0], b_sm], [b_W7], scale=DS)
                else:
                    for hf in range(2):
                        c = slice(hf * 512, (hf + 1) * 512)
                        MM(pL[0][:], tri_le, Wt[0][:, c], True, True, [b_cst, b_W[0]], [b_pL[0]])
                        ACT(Wt[6][:, c], pL[0][:], AF.Exp, [b_pL[0], b_sm], [b_W[6]])
                        ACT(W7[:, c], pL[0][:], AF.Exp, [b_pL[0]], [b_W7], scale=-1.0)
                TT("dve", Wt[4][r_, :], Wt[4][r_, :], W7[r_, :], ALU.mult, [b_W[4], b_W7], [b_W[4]])
                TT("pool", Wt[5][r_, :], Wt[5][r_, :], W7[r_, :], ALU.mult, [b_W[5], b_W7], [b_W[5]])
                if not samp:
                    for hf in range(2):
                        c = slice(hf * 512, (hf + 1) * 512)
                        MM(pL[1][:], tri_lt, Wt[0][:, c], True, True, [b_cst, b_W[0]], [b_pL[1]])
                        ACT(W7[:, c], pL[1][:], AF.Exp, [b_pL[1], b_W[4], b_W[5]], [b_W7])
                    TT("dve", Wt[3][r_, :], Wt[3][r_, :], W7[r_, :], ALU.mult, [b_W[3], b_W7], [b_W[3]])
                if own:
                    TT("pool", q[r_, 0:1024], q[r_, 0:1024], Wt[6][r_, :], ALU.mult, [b_q, b_W[6]], [b_q])
                srcs = [(Wt[3], b_W[3]), (Wt[4], b_W[4]), (Wt[5], b_W[5]), (q[:, 0:1024], b_q), (Wt[6], b_W[6])]
                for f, (src, bs) in enumerate(srcs):
                    for hp in range(2):
                        P.dma("sp" if hp == 0 else "act",
                              bscr[i * 128:i * 128 + np_, hp, f, :].rearrange("t (j k) -> t j k", k=64),
                              src[r_, :].rearrange("p (j hp k) -> p hp j k", hp=2, k=64)[:, hp],
                              [bs], [b_scr], "scr")
                for j in range(8):
                    TR(vps[j // 4][:, j % 4, 0:np_], q[r_, 2048 + j * 128:2048 + (j + 1) * 128], ident[r_, r_],
                       [b_q, b_cst], [b_bk[6 + j // 4]])
                CP("act", vT[:, 0:4, 0:np_], vps[0][:, :, 0:np_], [b_bk[6]], [b_vT])
                CP("dve", vT[:, 4:8, 0:np_], vps[1][:, :, 0:np_], [b_bk[7]], [b_vT])
                for t in range(np_):
                    g = i * 128 + t
                    kb = g % NB
                    B_ = bc[kb]; bB = b_bc[kb]
                    for hp in range(2):
                        cend = samp or (t % 64 == 63)
                        nf = 5 if cend else (4 if own else 3)
                        if os.environ.get("KB_Q4", "0") == "1":
                            P.dma("act" if hp == 1 else "sp", B_[hp * 64:(hp + 1) * 64, 0:1024],
                                  bscr[g, hp, 0:2].rearrange("f n -> (f n)").partition_broadcast(64),
                                  [b_scr], [bB], "bc%d" % kb)
                            P.dma("pool", B_[hp * 64:(hp + 1) * 64, 1024:nf * 512],
                                  bscr[g, hp, 2:nf].rearrange("f n -> (f n)").partition_broadcast(64),
                                  [b_scr], [bBp[kb]], "bcp%d" % kb)
                        else:
                            P.dma("act" if hp == 1 else "sp", B_[hp * 64:(hp + 1) * 64, 0:nf * 512],
                                  bscr[g, hp, 0:nf].rearrange("f n -> (f n)").partition_broadcast(64),
                                  [b_scr], [bB], "bc%d" % kb)
                    if samp:
                        for hp in range(2):
                            P.dma("act", S[hp * 64:(hp + 1) * 64, :, :],
                                  srwkv[t].rearrange("(j hp) v k -> hp v j k", hp=2)[hp], [], b_S2, "Sl")
                    fv = lambda f, js: B_[:, f * 512:(f + 1) * 512].rearrange("p (j k) -> p j k", k=64)[:, js, :]
                    td = tD[g % 2]
                    JS = [slice(0, 4), slice(4, 8)]
                    G2 = (0, 1)
                    for g2 in G2:
                        js = JS[g2]
                        TT("pool", td[:, js, :], fv(2, js), vT[:, js, t].unsqueeze(2).broadcast_to([128, 4, 64]), ALU.mult,
                           [bB, b_vT], [b_tD2[g % 2][g2]])
                    for g2 in G2:
                        js = JS[g2]
                        TT("dve", tA[:, js, :], S[:, js, :], fv(0, js), ALU.mult, [b_S2[g2], bB], [b_tA2[g2]])
                    for g2 in G2:
                        js = JS[g2]
                        RED("dve", sa[:, js], tA[:, js, :], ALU.add, [b_tA2[g2]], [b_sa2[g2]])
                    for g2 in G2:
                        js = JS[g2]
                        TT("dve", tB[:, js, :], fv(1, js), sa[:, js].unsqueeze(2).broadcast_to([128, 4, 64]), ALU.mult,
                           [bB, b_sa2[g2]], [b_tB2[g2]])
                    for g2 in G2:
                        js = JS[g2]
                        TT("dve", S[:, js, :], S[:, js, :], tB[:, js, :], ALU.subtract, [b_S2[g2], b_tB2[g2]], [b_S2[g2]])
                    for g2 in G2:
                        js = JS[g2]
                        TT("dve", S[:, js, :], S[:, js, :], td[:, js, :], ALU.add, [b_S2[g2], b_tD2[g % 2][g2]], [b_S2[g2]])
                    if own:
                        for g2 in G2:
                            js = JS[g2]
                            TT("dve", tC[:, js, :], S[:, js, :], fv(3, js), ALU.mult, [b_S2[g2], bB], [b_tC2[g2]])
                        for g2 in G2:
                            js = JS[g2]
                            RED("dve", oT[:, js, t], tC[:, js, :], ALU.add, [b_tC2[g2]], [b_oT])
                    if cend:
                        for g2 in G2:
                            js = JS[g2]
                            TT("dve", S[:, js, :], S[:, js, :], fv(4, js), ALU.mult, [b_S2[g2], bB], [b_S2[g2]])
                    if samp:
                        for hp in range(2):
                            P.dma("sp", rwkvs_o[t].rearrange("(j hp) v k -> hp v j k", hp=2)[hp],
                                  S[hp * 64:(hp + 1) * 64, :, :], b_S2, [], "So")
                if i == NW - 1:
                    for hp in range(2):
                        P.dma("sp", rwkvp_o.rearrange("(j hp) v k -> hp v j k", hp=2)[hp],
                              S[hp * 64:(hp + 1) * 64, :, :], b_S2, [], "So")
                if own:
                    for j in range(8):
                        TR(vps[j // 4][0:np_, j % 4, :], oT[:, j, 0:np_], ident, [b_oT, b_cst], [b_bk[6 + j // 4]])
                    CP("act", Ot[r_, 0:512], bk[6][r_, :], [b_bk[6]], [b_Ot])
                    CP("dve", Ot[r_, 512:1024], bk[7][r_, :], [b_bk[7]], [b_Ot])
                    out_part(np_, 1024 if samp else (i - (NW - NOWN)) * 128)
                P.flush()
        if stop_after == "B":
            P.flush(final=True)
            return nc

        uid = [0]

        def un(n):
            uid[0] += 1
            return "%s_%d" % (n, uid[0])

        TILES = [(i, 128, i * 128) for i in range(8)] + [(8, 16, 1024)]
        ATT_SCALE = 512.0 ** -0.5

        def x_src(ti):
            return xw[1024 + ti * 128:1024 + (ti + 1) * 128, :] if ti < 8 else xs

        def xres_src(ti):
            (_, np_, c0) = TILES[ti]
            return xres[c0:c0 + np_, :]

        def proj_res(w, src_fn):
            with contextlib.ExitStack() as es_:
                alloc_ring(es_)
                pq = [pst(es_, un("pq"), [128, 512], F32) for _ in range(3)]; b_pq = [P.buf() for _ in range(3)]
                xin = [sbt(es_, un("xin"), [128, 512], F32) for _ in range(3)]; b_xin = [P.buf() for _ in range(3)]
                xo = [sbt(es_, un("xo"), [128, 512], F32) for _ in range(3)]; b_xo = [P.buf() for _ in range(3)]
                cnt = 0
                for cb in range(4):
                    slot, bs = stream_w(w[:, cb * 512:(cb + 1) * 512], 16, 512)
                    for (ti, np_, c0) in TILES:
                        k = cnt % 3
                        cnt += 1
                        P.dma("sp", xin[k][0:np_, :], src_fn(ti)[:, cb * 512:(cb + 1) * 512], [], [b_xin[k]], "xin")
                        for dc in range(16):
                            MM(pq[k][0:np_, :], cTb[:, dc, c0:c0 + np_], slot[:, dc, :], dc == 0, dc == 15, [bs], [b_pq[k]])
                        TT("dve", xo[k][0:np_, :], pq[k][0:np_, :], xin[k][0:np_, :], ALU.add, [b_pq[k], b_xin[k]], [b_xo[k]])
                        P.dma("act", xres[c0:c0 + np_, cb * 512:(cb + 1) * 512], xo[k][0:np_, :], [b_xo[k]], [], "xo%d" % k)
                P.flush()

        def norm_T(es_, src_list, g_dram, dst, route=None):
            gb = sbt(es_, un("gb"), [128, D], F32); b_gb = P.buf()
            P.dma("sp", gb[:], g_dram.partition_broadcast(128), [], [b_gb], "gb")
            xt2 = [sbt(es_, un("xt"), [128, D], F32) for _ in range(2)]; b_xt = [P.buf(), P.buf()]
            hb = sbt(es_, un("hb"), [128, D], BF16); b_hb = P.buf()
            ss = sbt(es_, un("ss"), [128, 1], F32); rstd = sbt(es_, un("rstd"), [128, 1], F32)
            pT = [pst(es_, un("pT"), [128, 8, 128], BF16) for _ in range(2)]; b_pT = [P.buf(), P.buf()]
            b_dst = P.buf()
            for n_, (src, np_, c0) in enumerate(src_list):
                xt = xt2[n_ % 2]; bx = b_xt[n_ % 2]
                P.dma("sp" if n_ % 2 == 0 else "act", xt[0:np_, :], src, [], [bx], "xl")
                rmsnorm_tile(None, xt, np_, gb, hb, ss, rstd, hb, [bx, b_gb], [b_hb], b_hb)
                for hf in range(2):
                    for j in range(8):
                        dc = hf * 8 + j
                        TR(pT[hf][0:128, j, 0:np_], hb[0:np_, dc * 128:(dc + 1) * 128], identb[0:np_, 0:np_],
                           [b_hb, b_cst], [b_pT[hf]])
                    CP("act" if hf == 0 else "dve", dst[:, hf * 8:(hf + 1) * 8, c0:c0 + np_], pT[hf][:, :, 0:np_],
                       [b_pT[hf]], [b_dst])
                if route is not None:
                    route(n_, np_, xt, bx, rstd, gb, b_gb, b_hb)

        proj_res(w_out, x_src)
        if stop_after == "C":
            P.flush(final=True)
            return nc

        with contextlib.ExitStack() as esD:
            qT = sbt(esD, "qT", [128, 16, 1040], BF16); b_qT = P.buf()
            kT = sbt(esD, "kT", [128, 16, 256], BF16); b_kT = P.buf()
            Vb = sbt(esD, "Vb", [128, 2, 2048], BF16); b_Vb = P.buf()
            with contextlib.ExitStack() as esa:
                norm_T(esa, [(xres_src(ti), np_, c0) for (ti, np_, c0) in TILES], norm_x, cTb)
                P.flush()
            with contextlib.ExitStack() as es1:
                mnT = sbt(es1, "mnT", [128, 16, 256], BF16)
                with contextlib.ExitStack() as esb:
                    norm_T(esb, [(mem[mt * 128:(mt + 1) * 128, :], 128, mt * 128) for mt in range(2)], norm_mem, mnT)
                    P.flush()
                alloc_ring(es1)
                pq = [pst(es1, un("pq"), [128, 512], F32) for _ in range(3)]; b_pq = [P.buf() for _ in range(3)]
                qtok = sbt(es1, "qtok", [16, D], F32); b_qtok = P.buf()
                stg = [sbt(es1, un("stg"), [128, 512], F32) for _ in range(2)]; b_stg = [P.buf(), P.buf()]
                cnt = 0
                P23 = os.environ.get("KB_P23", "qtkv")
                for cb in range(4 if "q" in P23 else 0):
                    slot, bs = stream_w(w_cq[:, cb * 512:(cb + 1) * 512], 16, 512)
                    for fb in range(4):
                        for (n0, nn) in [(0, 512), (512, 512), (1024, 16)]:
                            k = cnt % 3
                            cnt += 1
                            for dc in range(16):
                                MM(pq[k][:, 0:nn], slot[:, dc, fb * 128:(fb + 1) * 128], cTb[:, dc, n0:n0 + nn],
                                   dc == 0, dc == 15, [bs], [b_pq[k]])
                            CP("act" if cnt % 2 == 0 else "dve", qT[:, cb * 4 + fb, n0:n0 + nn], pq[k][:, 0:nn],
                               [b_pq[k]], [b_qT])
                    if "t" not in P23:
                        continue
                    k = cnt % 3
                    cnt += 1
                    for dc in range(16):
                        MM(pq[k][0:16, :], cTb[:, dc, 1024:1040], slot[:, dc, :], dc == 0, dc == 15, [bs], [b_pq[k]])
                    CP("act", qtok[:, cb * 512:(cb + 1) * 512], pq[k][0:16, :], [b_pq[k]], [b_qtok])
                if "t" in P23:
                    P.dma("sp", qtok_scr, qtok[:], [b_qtok], [], "qtk")
                scnt = 0
                for (w, is_k, o_ap) in ((w_ck, True, memk_o), (w_cv, False, memv_o)):
                    if ("k" if is_k else "v") not in P23:
                        continue
                    for cb in range(4):
                        slot, bs = stream_w(w[:, cb * 512:(cb + 1) * 512], 16, 512)
                        for mt in range(2):
                            k = cnt % 3
                            cnt += 1
                            for dc in range(16):
                                MM(pq[k][:, :], mnT[:, dc, mt * 128:(mt + 1) * 128], slot[:, dc, :], dc == 0, dc == 15,
                                   [bs], [b_pq[k]])
                            s2 = scnt % 2
                            scnt += 1
                            CP("act", stg[s2][:], pq[k][:, :], [b_pq[k]], [b_stg[s2]])
                            P.dma("sp", o_ap[mt * 128:(mt + 1) * 128, cb * 512:(cb + 1) * 512], stg[s2][:], [b_stg[s2]], [],
                                  "mo%d" % s2)
                            if not is_k:
                                CP("dve", Vb[:, mt, cb * 512:(cb + 1) * 512], stg[s2][:], [b_stg[s2]], [b_Vb])
                        if is_k:
                            for fb in range(4):
                                k = cnt % 3
                                cnt += 1
                                for dc in range(16):
                                    MM(pq[k][:, 0:256], slot[:, dc, fb * 128:(fb + 1) * 128], mnT[:, dc, 0:256],
                                       dc == 0, dc == 15, [bs], [b_pq[k]])
                                CP("dve", kT[:, cb * 4 + fb, :], pq[k][:, 0:256], [b_pq[k]], [b_kT])
                P.flush()
            with contextlib.ExitStack() as es4:
                onesb = sbt(es4, "onesb", [128, 128], BF16); b_on = P.buf()
                onesf = sbt(es4, "onesf", [128, 128], F32)
                MEMSET("dve", onesb[:], 1.0, [b_on])
                MEMSET("dve", onesf[:], 1.0, [b_on])
                psc = [pst(es4, un("psc"), [128, 512], F32) for _ in range(2)]; b_psc = [P.buf(), P.buf()]
                pdn = pst(es4, "pdn", [128, 512], F32); b_pdn = P.buf()
                pcx = [pst(es4, un("pcx"), [128, 512], F32) for _ in range(2)]; b_pcx = [P.buf(), P.buf()]
                eT = [sbt(es4, un("eT"), [128, 2, 512], BF16) for _ in range(2)]; b_eT = [P.buf(), P.buf()]
                rden = [sbt(es4, un("rden"), [128, 512], F32) for _ in range(2)]; b_rden = [P.buf(), P.buf()]
                b_ctx = P.buf()
                it = 0
                ccnt = 0
                for tb in range(0 if 'D4' in os.environ.get('KB_SKIP', '') else 2):
                    n0 = tb * 512
                    for h in range(4):
                        k2 = it % 2
                        it += 1
                        for mt in range(2):
                            for dc in range(4):
                                MM(psc[mt][:, :], kT[:, h * 4 + dc, mt * 128:(mt + 1) * 128], qT[:, h * 4 + dc, n0:n0 + 512],
                                   dc == 0, dc == 3, [], [b_psc[mt]])
                            ACT(eT[k2][:, mt, :], psc[mt][:, :], AF.Exp, [b_psc[mt]], [b_eT[k2]], scale=ATT_SCALE)
                        for mt in range(2):
                            MM(pdn[:, :], onesb[:], eT[k2][:, mt, :], mt == 0, mt == 1, [b_on, b_eT[k2]], [b_pdn])
                        RECIP(rden[k2][:], pdn[:, :], [b_pdn], [b_rden[k2]])
                        for dc in range(4):
                            c2 = ccnt % 2
                            ccnt += 1
                            for mt in range(2):
                                MM(pcx[c2][:, :], Vb[:, mt, h * 512 + dc * 128:h * 512 + (dc + 1) * 128], eT[k2][:, mt, :],
                                   mt == 0, mt == 1, [b_eT[k2]], [b_pcx[c2]])
                            TT("dve", cTb[:, h * 4 + dc, n0:n0 + 512], pcx[c2][:, :], rden[k2][:], ALU.mult,
                               [b_pcx[c2], b_rden[k2]], [b_ctx])
                P.flush()
                Ks = [sbt(es4, un("Ks"), [128, 2, D], F32) for _ in range(2)]; b_Ks = [P.buf(), P.buf()]
                Vs = [sbt(es4, un("Vs"), [128, 2, D], F32) for _ in range(2)]; b_Vs = [P.buf(), P.buf()]
                qb = [sbt(es4, un("qb"), [128, D], F32) for _ in range(2)]; b_qb = [P.buf(), P.buf()]
                sc = [sbt(es4, un("sc"), [128, 8], F32) for _ in range(2)]; b_sc = [P.buf(), P.buf()]
                rd4 = [sbt(es4, un("rd4"), [128, 4], F32) for _ in range(2)]; b_rd4 = [P.buf(), P.buf()]
                pct = [psc[0][:, 0:16], psc[1][:, 0:16]]; b_pct = b_psc
                pd4 = [pcx[0][:, 0:4], pcx[1][:, 0:4]]; b_pd4 = b_pcx
                for s_ in range(0 if 'D5' in os.environ.get('KB_SKIP', '') else 16):
                    k2 = s_ % 2
                    P.dma("sp", Ks[k2][:], cmk[s_].rearrange("(mt p) d -> p mt d", p=128), [], [b_Ks[k2]], "ks")
                    P.dma("act", Vs[k2][:], cmv[s_].rearrange("(mt p) d -> p mt d", p=128), [], [b_Vs[k2]], "vs")
                    P.dma("sp", qb[k2][:], qtok_scr[s_, :].partition_broadcast(128), [], [b_qb[k2]], "qb")
                    for mt in range(2):
                        TT("dve" if mt == 0 else "pool", Ks[k2][:, mt, :], Ks[k2][:, mt, :], qb[k2][:], ALU.mult,
                           [b_Ks[k2], b_qb[k2]], [b_Ks[k2]])
                    RED("dve", sc[k2][:], Ks[k2][:].rearrange("p mt (h d) -> p (mt h) d", d=512), ALU.add, [b_Ks[k2]], [b_sc[k2]])
                    ACT(sc[k2][:], sc[k2][:], AF.Exp, [b_sc[k2]], [b_sc[k2]], scale=ATT_SCALE)
                    for mt in range(2):
                        MM(pd4[k2], onesf[:], sc[k2][:, mt * 4:(mt + 1) * 4], mt == 0, mt == 1, [b_on, b_sc[k2]], [b_pd4[k2]])
                    RECIP(rd4[k2][:], pd4[k2], [b_pd4[k2]], [b_rd4[k2]])
                    for kc in range(16):
                        h = kc // 4
                        for mt in range(2):
                            MM(pct[k2][:, kc:kc + 1], Vs[k2][:, mt, kc * 128:(kc + 1) * 128], sc[k2][:, mt * 4 + h:mt * 4 + h + 1],
                               mt == 0, mt == 1, [b_Vs[k2], b_sc[k2]], [b_pct[k2]])
                    TT("dve", cTb[:, :, 1024 + s_].rearrange("p (h c) -> p h c", c=4),
                       pct[k2].rearrange("p (h c) -> p h c", c=4),
                       rd4[k2][:].unsqueeze(2).broadcast_to([128, 4, 4]), ALU.mult, [b_pct[k2], b_rd4[k2]], [b_ctx])
                P.flush()
        proj_res(w_co, xres_src)
        if stop_after == "D":
            P.flush(final=True)
            return nc

        with contextlib.ExitStack() as esE:
            comb = sbt(esE, "comb", [128, 9, 32], F32); b_comb = P.buf()
            with contextlib.ExitStack() as es1:
                wr_sb = sbt(es1, "wr_sb", [128, 16, 36], F32); b_wr = P.buf()
                P.dma("sp", wr_sb[:], w_route.rearrange("(c p) n -> p c n", p=128), [], [b_wr], "wr")
                br_b = sbt(es1, "br_b", [128, 36], F32)
                P.dma("sp", br_b[:], b_route.partition_broadcast(128), [], [b_wr], "wr")
                h2f = sbt(es1, "h2f", [128, D], F32); b_h2f = P.buf()
                h2Tf = sbt(es1, "h2Tf", [128, 16, 128], F32); b_h2Tf = P.buf()
                pTf = [pst(es1, un("pTf"), [128, 4, 128], F32) for _ in range(2)]; b_pTf = [P.buf(), P.buf()]
                plg = pst(es1, "plg", [128, 64], F32); b_plg = P.buf()
                lg = sbt(es1, "lg", [128, 36], F32); b_r = P.buf()
                rt = sbt(es1, "rt", [128, 96], F32)
                gmax = rt[:, 0:1]; ngmax = rt[:, 1:2]; sume = rt[:, 2:3]; gval = rt[:, 3:4]
                m1 = rt[:, 4:5]; m2 = rt[:, 5:6]; dd = rt[:, 6:7]; e1 = rt[:, 7:8]; w1 = rt[:, 8:9]; w2 = rt[:, 9:10]
                ohg = rt[:, 12:16]; junk = rt[:, 16:20]; les = rt[:, 24:32]; oh1 = rt[:, 32:40]; msk = rt[:, 40:48]
                oh2 = rt[:, 48:56]; ec = rt[:, 56:64]; t32 = rt[:, 64:96]

                def route(n_, np_, xt, bx, rstd, gb, b_gb, b_hb):
                    r_ = slice(0, np_)
                    R_ = [b_r]
                    STT("dve", h2f[r_, :], xt[r_, :], rstd[r_, 0:1], gb[r_, :], ALU.mult, ALU.mult, [bx, b_gb, b_hb], [b_h2f])
                    for grp in range(4):
                        pt = pTf[grp % 2]; bpt = b_pTf[grp % 2]
                        for j in range(4):
                            dc = grp * 4 + j
                            TR(pt[:, j, 0:np_], h2f[r_, dc * 128:(dc + 1) * 128], ident[r_, r_], [b_h2f, b_cst], [bpt])
                        CP("act" if grp % 2 == 0 else "dve", h2Tf[:, grp * 4:(grp + 1) * 4, 0:np_], pt[:, :, 0:np_], [bpt], [b_h2Tf])
                    for dc in range(16):
                        MM(plg[r_, 0:36], h2Tf[:, dc, 0:np_], wr_sb[:, dc, :], dc == 0, dc == 15, [b_h2Tf, b_wr], [b_plg])
                    TT("dve", lg[r_, :], plg[r_, 0:36], br_b[r_, :], ALU.add, [b_plg, b_wr], R_)
                    RED("dve", gmax[r_, :], lg[r_, 0:4], ALU.max, R_, R_)
                    TS("dve", ohg[r_, :], lg[r_, 0:4], gmax[r_, :], None, ALU.is_ge, None, R_, R_)
                    TS("dve", ngmax[r_, :], gmax[r_, :], -1.0, None, ALU.mult, None, R_, R_)
                    ACT(junk[r_, :], lg[r_, 0:4], AF.Exp, R_, R_, bias=ngmax[r_, :], scale=1.0, accum_out=sume[r_, :])
                    RECIP(gval[r_, :], sume[r_, :], R_, R_)
                    TT("dve", t32[r_, :].rearrange("p (g e) -> p g e", e=8), lg[r_, 4:36].rearrange("p (g e) -> p g e", e=8),
                       ohg[r_, :].unsqueeze(2).broadcast_to([np_, 4, 8]), ALU.mult, R_, R_)
                    RED("dve", les[r_, :], t32[r_, :].rearrange("p (g e) -> p e g", e=8), ALU.add, R_, R_)
                    RED("dve", m1[r_, :], les[r_, :], ALU.max, R_, R_)
                    TS("dve", oh1[r_, :], les[r_, :], m1[r_, :], None, ALU.is_ge, None, R_, R_)
                    STT("dve", msk[r_, :], oh1[r_, :], -1e30, les[r_, :], ALU.mult, ALU.add, R_, R_)
                    RED("dve", m2[r_, :], msk[r_, :], ALU.max, R_, R_)
                    TS("dve", oh2[r_, :], msk[r_, :], m2[r_, :], None, ALU.is_ge, None, R_, R_)
                    TT("dve", dd[r_, :], m2[r_, :], m1[r_, :], ALU.subtract, R_, R_)
                    ACT(e1[r_, :], dd[r_, :], AF.Exp, R_, R_)
                    TS("dve", w1[r_, :], e1[r_, :], 1.0, None, ALU.add, None, R_, R_)
                    RECIP(w1[r_, :], w1[r_, :], R_, R_)
                    TT("dve", w2[r_, :], e1[r_, :], w1[r_, :], ALU.mult, R_, R_)
                    TT("dve", w1[r_, :], w1[r_, :], gval[r_, :], ALU.mult, R_, R_)
                    TT("dve", w2[r_, :], w2[r_, :], gval[r_, :], ALU.mult, R_, R_)
                    TS("dve", ec[r_, :], oh1[r_, :], w1[r_, :], None, ALU.mult, None, R_, R_)
                    STT("dve", ec[r_, :], oh2[r_, :], w2[r_, :], ec[r_, :], ALU.mult, ALU.add, R_, R_)
                    TT("dve", comb[r_, n_, :].rearrange("p (g e) -> p g e", e=8),
                       ohg[r_, :].unsqueeze(2).broadcast_to([np_, 4, 8]),
                       ec[r_, :].unsqueeze(1).broadcast_to([np_, 4, 8]), ALU.mult, R_, [b_comb])

                norm_T(es1, [(xres_src(ti), np_, c0) for (ti, np_, c0) in TILES], norm_ffn, cTb, route=route)
                P.flush()
            acc = sbt(esE, "acc", [128, 9, D], F32); b_acc = [P.buf() for _ in range(9)]
            for (ti, np_, c0) in TILES:
                P.dma("sp" if ti % 2 == 0 else "act", acc[0:np_, ti, :], xres_src(ti), [], [b_acc[ti]], "acc")
            esM = contextlib.ExitStack()
            alloc_ring(esM)
            wd = sbt(esM, "wd", [128, 4, D], BF16); b_wd = P.buf()
            pg = [pst(esM, un("pg"), [128, 512], F32) for _ in range(2)]; b_pg = [P.buf(), P.buf()]
            pu = [pst(esM, un("pu"), [128, 512], F32) for _ in range(2)]; b_pu = [P.buf(), P.buf()]
            po = [pst(esM, un("po"), [128, 512], F32) for _ in range(3)]; b_po = [P.buf() for _ in range(3)]
            sg = [sbt(esM, un("sg"), [128, 512], F32) for _ in range(2)]; b_sg = [P.buf(), P.buf()]
            aT = [sbt(esM, un("aT"), [128, 4, 512], BF16) for _ in range(2)]; b_aT = [P.buf(), P.buf()]
            gcnt = 0
            ocnt = 0
            bcnt = 0
            NEXP = int(os.environ.get('KB_NEXP', 32))
            for e_ in range(NEXP):
                wg_, b_wg = stream_w(w_gate[e_], 16, 512)
                wu_, b_wu = stream_w(w_up[e_], 16, 512)
                P.dma("pool", wd[:], w_down[e_].rearrange("(c p) n -> p c n", p=128), [], [b_wd], "wd")
                for (n0, nn, tls) in [(0, 512, [0, 1, 2, 3]), (512, 512, [4, 5, 6, 7]), (1024, 16, [8])]:
                    a_ = aT[bcnt % 2]; ba = b_aT[bcnt % 2]
                    bcnt += 1
                    for fc in range(4):
                        k = gcnt % 2
                        gcnt += 1
                        for dc in range(16):
                            MM(pg[k][:, 0:nn], wg_[:, dc, fc * 128:(fc + 1) * 128], cTb[:, dc, n0:n0 + nn], dc == 0, dc == 15,
                               [b_wg], [b_pg[k]])
                        ACT(sg[k][:, 0:nn], pg[k][:, 0:nn], AF.Silu, [b_pg[k]], [b_sg[k]])
                        for dc in range(16):
                            MM(pu[k][:, 0:nn], wu_[:, dc, fc * 128:(fc + 1) * 128], cTb[:, dc, n0:n0 + nn], dc == 0, dc == 15,
                               [b_wu], [b_pu[k]])
                        TT("dve", a_[:, fc, 0:nn], pu[k][:, 0:nn], sg[k][:, 0:nn], ALU.mult, [b_pu[k], b_sg[k]], [ba])
                    for ti in tls:
                        (_, np_, c0) = TILES[ti]
                        for cb in range(4):
                            k = ocnt % 3
                            ocnt += 1
                            for fc in range(4):
                                MM(po[k][0:np_, :], a_[:, fc, c0 - n0:c0 - n0 + np_], wd[:, fc, cb * 512:(cb + 1) * 512],
                                   fc == 0, fc == 3, [ba, b_wd], [b_po[k]])
                            STT("dve", acc[0:np_, ti, cb * 512:(cb + 1) * 512], po[k][0:np_, :], comb[0:np_, ti, e_:e_ + 1],
                                acc[0:np_, ti, cb * 512:(cb + 1) * 512], ALU.mult, ALU.add, [b_po[k], b_acc[ti], b_comb], [b_acc[ti]])
            P.flush()
            esM.close()
            gbf = sbt(esE, "gbf", [128, D], F32); b_gbf = P.buf()
            P.dma("sp", gbf[:], norm_final.partition_broadcast(128), [], [b_gbf], "gbf")
            yt = [sbt(esE, un("yt"), [128, D], F32) for _ in range(2)]; b_yt = [P.buf(), P.buf()]
            ssf = sbt(esE, "ssf", [128, 1], F32); rsf = sbt(esE, "rsf", [128, 1], F32)
            for (ti, np_, c0) in TILES:
                y_ = yt[ti % 2]; by = b_yt[ti % 2]
                rmsnorm_tile(None, acc[:, ti, :], np_, gbf, y_, ssf, rsf, y_, [b_gbf, b_acc[ti]], [by], by)
                P.dma("sp" if ti % 2 == 0 else "act", y_o[c0:c0 + np_, :], y_[0:np_, :], [by], [], "yo%d" % (ti % 2))
            P.flush()
        P.flush(final=True)
    return nc


_CONSTS = None


def make_in_maps(inputs):
    global _CONSTS
    if _CONSTS is None:
        _CONSTS = build_consts()
    g = lambda k: np.ascontiguousarray(np.asarray(inputs[k], dtype=np.float32))
    xp = g("x_prompt"); xs = g("x_sample"); memp = g("mem_prompt")
    shared = {
        "consts": _CONSTS,
        "norm_mix": g("norm_mix")[0], "w_in": g("w_in")[0], "conv_w": g("conv_w")[0], "conv_b": g("conv_b")[0],
        "conv_ln_g": g("conv_ln_g")[0], "conv_ln_b": g("conv_ln_b")[0], "shift_mu": g("shift_mu")[0],
        "w_decay_up": g("w_decay_up")[0], "decay_bias": g("decay_bias")[0], "w_a_up": g("w_a_up")[0],
        "a_bias": g("a_bias")[0], "w_g_up": g("w_g_up")[0], "k_k": g("k_k")[0], "k_a": g("k_a")[0],
        "r_k": g("r_k")[0].reshape(1024), "lnx_g": g("lnx_g")[0], "lnx_b": g("lnx_b")[0],
        "w_out": g("w_out")[0], "norm_x": g("norm_x")[0], "norm_mem": g("norm_mem")[0],
        "w_cq": g("w_cq")[0], "w_ck": g("w_ck")[0], "w_cv": g("w_cv")[0], "w_co": g("w_co")[0],
        "norm_ffn": g("norm_ffn")[0],
        "w_route": np.ascontiguousarray(np.concatenate([g("w_route_group")[0], g("w_route_expert")[0].reshape(D, 32)], axis=1)),
        "b_route": np.ascontiguousarray(np.concatenate([g("b_route_group")[0], g("b_route_expert")[0].reshape(32)])),
        "w_gate": g("w_gate")[0].reshape(32, D, 512), "w_up": g("w_up")[0].reshape(32, D, 512),
        "w_down": g("w_down")[0].reshape(32, 512, D), "norm_final": g("norm_final"),
    }
    cc = g("cache_conv")[0]; ssh = g("state_shift")[0]; srw = g("state_rwkv")[0]
    cmk = g("cache_mem_k")[0].reshape(128, 256, D); cmv = g("cache_mem_v")[0].reshape(128, 256, D)
    maps = []
    for c in range(8):
        b, half = c // 2, c % 2
        if half == 0:
            xw = np.concatenate([np.zeros((1024, D), np.float32), xp[b, 0:1024]], axis=0)
        else:
            xw = xp[b]
        m = dict(shared)
        m.update({"xw": np.ascontiguousarray(xw), "xs": np.ascontiguousarray(xs[16 * c:16 * c + 16, 0]),
                  "mem": memp[b], "cconv": cc[16 * c:16 * c + 16], "sshift": ssh[16 * c:16 * c + 16],
                  "srwkv": srw[16 * c:16 * c + 16], "cmk": cmk[16 * c:16 * c + 16], "cmv": cmv[16 * c:16 * c + 16]})
        maps.append({k: v for k, v in m.items() if k in DECL})
    return maps


def gather(res):
    R = res.results
    y_p = np.zeros((4, 2048, D), np.float32); y_s = np.zeros((128, 1, D), np.float32)
    conv_p = np.zeros((1, 4, 30, CW), np.float32); shift_p = np.zeros((1, 4, SHIFT_W), np.float32)
    rwkv_p = np.zeros((1, 4, 16, 64, 64), np.float32)
    memk = np.zeros((1, 4, 256, 4, 512), np.float32); memv = np.zeros((1, 4, 256, 4, 512), np.float32)
    conv_s = np.zeros((1, 128, 30, CW), np.float32); shift_s = np.zeros((1, 128, SHIFT_W), np.float32)
    rwkv_s = np.zeros((1, 128, 16, 64, 64), np.float32)
    for c in range(8):
        b, half = c // 2, c % 2
        r = R[c]
        y_p[b, half * 1024:(half + 1) * 1024] = r["y_o"][0:1024]
        y_s[16 * c:16 * c + 16, 0] = r["y_o"][1024:1040]
        conv_s[0, 16 * c:16 * c + 16] = r["convs_o"]; shift_s[0, 16 * c:16 * c + 16] = r["shifts_o"]
        rwkv_s[0, 16 * c:16 * c + 16] = r["rwkvs_o"]
        if half == 1:
            conv_p[0, b] = r["convp_o"]; shift_p[0, b] = r["shiftp_o"][0]; rwkv_p[0, b] = r["rwkvp_o"]
            memk[0, b] = r["memk_o"].reshape(256, 4, 512); memv[0, b] = r["memv_o"].reshape(256, 4, 512)
    return (y_p, y_s, conv_p, shift_p, rwkv_p, memk, memv, conv_s, shift_s, rwkv_s)


_NC = None


def kernel(**inputs):
    global _NC
    if _NC is None:
        _NC = build_program()
    maps = make_in_maps(inputs)
    res = run_bass_kernel_spmd(_NC, maps, core_ids=list(range(8)))
    return gather(res)
```

```python
import contextlib
import math
import os
import numpy as np
import concourse.bass as bass
import concourse.mybir as mybir
from concourse.bass_utils import run_bass_kernel_spmd

F32 = mybir.dt.float32
BF16 = mybir.dt.bfloat16
AF = mybir.ActivationFunctionType
ALU = mybir.AluOpType
AX = mybir.AxisListType

D = 2048
NW = 16
NOWN = 8
SHIFT_W = 3360
IN_W = 5408
DS = math.exp(-0.5)
RMS_EPS = 1e-6
LN_EPS = 1e-5
GN_EPS = 64e-5
CW = 1024


class Buf:
    __slots__ = ("last_w", "readers")

    def __init__(self):
        self.last_w = None
        self.readers = []


class Op:
    __slots__ = ("eng", "fn", "deps", "idx", "eidx", "is_dma", "sig", "need", "key")


class Prog:
    EPOCH = 16000

    def __init__(self, nc, semfn):
        self.nc = nc
        self.semfn = semfn
        self.ops = []
        self.nops = 0
        self.ecount = {}
        self.engs = {"pe": nc.tensor, "act": nc.scalar, "dve": nc.vector,
                     "pool": nc.gpsimd, "sp": nc.sync}
        self.bufs = []
        self.esem = {}
        self.ecnt = {}
        self.dsem = {}
        self.dcnt = {}
        self.waited = {}
        self.last_on = {}
        self.bar_deps = []
        self.bar_id = 0
        self.eng_bar = {}
        self.nwait = 0
        self.free_dsems = []
        self.free_by = {}
        self.dcls = {}
        self.nds = 0

    def buf(self):
        b = Buf()
        self.bufs.append(b)
        return b

    def op(self, eng, fn, reads=(), writes=(), dma=False, key=None):
        o = Op()
        o.eng, o.fn, o.is_dma, o.key = eng, fn, dma, key
        o.sig = None
        o.need = dma
        o.idx = self.nops
        self.nops += 1
        o.eidx = self.ecount.get(eng, 0)
        self.ecount[eng] = o.eidx + 1
        deps = {}
        for b in reads:
            p = b.last_w
            if p is None:
                continue
            if (not p.is_dma) and p.eng == eng:
                if dma or (eng != "pe" and o.eidx - p.eidx <= 2):
                    deps[p.idx] = p
            else:
                deps[p.idx] = p
        for b in writes:
            p = b.last_w
            if p is not None and (p.is_dma or p.eng != eng or dma):
                deps[p.idx] = p
            for rd in b.readers:
                if rd.is_dma or rd.eng != eng or dma:
                    deps[rd.idx] = rd
        for b in reads:
            b.readers.append(o)
        for b in writes:
            b.last_w = o
            b.readers = []
        deps.pop(o.idx, None)
        o.deps = list(deps.values())
        for p in o.deps:
            p.need = True
        self.ops.append(o)
        self.last_on[eng] = o
        return o

    def dma(self, q, out, in_, reads, writes, key):
        if writes:
            key = ("w", id(writes[0]))
        return self.op(q, lambda e: e.dma_start(out=out, in_=in_), reads, writes, dma=True, key=key)

    def flush(self, final=False):
        self.nflush = getattr(self, "nflush", -1) + 1
        if self.nflush in [int(x) for x in os.environ.get("KB_DROP", "").split(",") if x]:
            self.ops = []
            self.last_on = {}
            for b in self.bufs:
                b.last_w = None
                b.readers = []
            return
        had_ops = len(self.ops) > 0
        for e, o in self.last_on.items():
            if o is not None and not o.is_dma:
                o.need = True
        nkey = {}
        for o in self.ops:
            if o.need and o.is_dma:
                nkey[o.key] = nkey.get(o.key, 0) + 1
        for o in self.ops:
            if not o.need:
                continue
            if o.is_dma:
                k = o.key
                cls = "sw" if o.eng == "pool" else "hw"
                if k not in self.dsem:
                    fl = self.free_by.setdefault(cls, [])
                    fl.sort(key=lambda sc: -sc[1])
                    self.dcls[k] = cls
                    if fl and fl[-1][1] + 16 * nkey[k] <= 24000:
                        self.dsem[k], self.dcnt[k] = fl.pop()
                    else:
                        self.nds += 1
                        self.dsem[k] = self.semfn("dsem%d" % self.nds)
                        self.dcnt[k] = 0
                assert self.dcls[k] == cls, (k, cls)
                self.dcnt[k] += 16
                o.sig = (self.dsem[k], self.dcnt[k], 16)
            else:
                c = self.ecnt.get(o.eng, 0)
                ep = c // self.EPOCH
                lst = self.esem.setdefault(o.eng, [])
                while len(lst) <= ep:
                    lst.append(self.semfn("e_%s_%d" % (o.eng, len(lst))))
                o.sig = (lst[ep], c % self.EPOCH + 1, 1)
                self.ecnt[o.eng] = c + 1
        for o in self.ops:
            e = self.engs[o.eng]
            w = self.waited.setdefault(o.eng, {})
            need = {}
            if self.eng_bar.get(o.eng, 0) < self.bar_id:
                self.eng_bar[o.eng] = self.bar_id
                for (s, v) in self.bar_deps:
                    need[id(s)] = (s, v)
            for p in o.deps:
                s, v, _ = p.sig
                sid = id(s)
                if sid not in need or need[sid][1] < v:
                    need[sid] = (s, v)
            for sid, (s, v) in need.items():
                if w.get(sid, 0) < v:
                    e.wait_ge(s, v)
                    w[sid] = v
                    self.nwait += 1
            ins = o.fn(e)
            if o.sig is not None:
                ins.then_inc(o.sig[0], o.sig[2])
        bd = []
        for e, o in self.last_on.items():
            if o is not None and not o.is_dma and o.sig is not None:
                bd.append((o.sig[0], o.sig[1]))
        for k, s in self.dsem.items():
            bd.append((s, self.dcnt[k]))
        if not had_ops:
            bd = bd + list(self.bar_deps)
        self.bar_deps = bd
        self.bar_id += 1
        for k in list(self.dsem.keys()):
            self.free_by.setdefault(self.dcls[k], []).append((self.dsem[k], self.dcnt[k]))
        self.dcls = {}
        self.dsem = {}
        self.dcnt = {}
        self.ops = []
        self.last_on = {}
        for b in self.bufs:
            b.last_w = None
            b.readers = []
        if final:
            for en in ("sp", "act", "dve", "pool", "pe"):
                e = self.engs[en]
                w = self.waited.setdefault(en, {})
                for (s, v) in bd:
                    if w.get(id(s), 0) < v:
                        e.wait_ge(s, v)
                        w[id(s)] = v


def build_consts():
    c = np.zeros((128, 1024), np.float32)
    c[:, 0:128] = np.eye(128)
    s = np.arange(128)[:, None]
    t = np.arange(128)[None, :]
    same = (s // 64) == (t // 64)
    c[:, 128:256] = (same & (s < t))
    c[:, 256:384] = (same & (s <= t))
    c[:, 384:512] = (same & (s > t))
    c[:, 512:640] = -DS * (same & (s <= t))
    c[:, 640:768] = -DS * (same & (s < t))
    c[:, 768:896] = -DS * (same & (s > t))
    c[63, 896] = 1.0
    c[127, 897] = 1.0
    for i in range(4):
        for sl in range(4):
            c[sl * 30:(sl + 1) * 30, 898 + i * 16 + 4 * i + sl] = 1.0
    c[:, 962] = 1.0
    return c


DECL = set()
KB_S5 = int(os.environ.get('KB_S5', 15))
KB_STAGE = int(os.environ.get('KB_STAGE', 100))


def build_program(stop_after=None):
    nc = bass.Bass("TRN2", target_bir_lowering=False)

    big = ("w_gate", "w_up", "w_down", "cmk", "cmv")
    DECL.clear()

    def din(name, shape):
        if (stop_after in ("A", "B", "C") and name in big) or (stop_after == "D" and name in big[0:3]):
            return None
        DECL.add(name)
        return nc.dram_tensor(name, list(shape), F32, kind="ExternalInput").ap()

    def dout(name, shape):
        return nc.dram_tensor(name, list(shape), F32, kind="ExternalOutput").ap()

    xw = din("xw", [NW * 128, D])
    xs = din("xs", [16, D])
    mem = din("mem", [256, D])
    cconv = din("cconv", [16, 30, CW])
    sshift = din("sshift", [16, SHIFT_W])
    srwkv = din("srwkv", [16, 16, 64, 64])
    cmk = din("cmk", [16, 256, D])
    cmv = din("cmv", [16, 256, D])
    consts = din("consts", [128, 1024])
    norm_mix = din("norm_mix", [D]); w_in = din("w_in", [D, IN_W])
    conv_w = din("conv_w", [31, CW]); conv_b = din("conv_b", [CW])
    conv_ln_g = din("conv_ln_g", [CW]); conv_ln_b = din("conv_ln_b", [CW])
    shift_mu = din("shift_mu", [SHIFT_W])
    w_decay_up = din("w_decay_up", [64, 1024]); decay_bias = din("decay_bias", [1024])
    w_a_up = din("w_a_up", [64, 1024]); a_bias = din("a_bias", [1024])
    w_g_up = din("w_g_up", [160, 1024])
    k_k = din("k_k", [1024]); k_a = din("k_a", [1024]); r_k = din("r_k", [1024])
    lnx_g = din("lnx_g", [1024]); lnx_b = din("lnx_b", [1024])
    w_out = din("w_out", [D, D]); norm_x = din("norm_x", [D]); norm_mem = din("norm_mem", [D])
    w_cq = din("w_cq", [D, D]); w_ck = din("w_ck", [D, D]); w_cv = din("w_cv", [D, D]); w_co = din("w_co", [D, D])
    norm_ffn = din("norm_ffn", [D])
    w_route = din("w_route", [D, 36]); b_route = din("b_route", [36])
    w_gate = din("w_gate", [32, D, 512]); w_up = din("w_up", [32, D, 512]); w_down = din("w_down", [32, 512, D])
    norm_final = din("norm_final", [D])

    y_o = dout("y_o", [1040, D])
    convp_o = dout("convp_o", [30, CW])
    shiftp_o = dout("shiftp_o", [1, SHIFT_W])
    rwkvp_o = dout("rwkvp_o", [16, 64, 64])
    memk_o = dout("memk_o", [256, D])
    memv_o = dout("memv_o", [256, D])
    convs_o = dout("convs_o", [16, 30, CW])
    shifts_o = dout("shifts_o", [16, SHIFT_W])
    rwkvs_o = dout("rwkvs_o", [16, 16, 64, 64])

    q_scr = nc.dram_tensor("q_scr", [1 + NW * 128 + 16, SHIFT_W], F32, kind="Internal").ap()
    xres = nc.dram_tensor("xres", [1040, D], F32, kind="Internal").ap()
    qtok_scr = nc.dram_tensor("qtok_scr", [16, D], F32, kind="Internal").ap()
    bscr = nc.dram_tensor("bscr", [NW * 128 + 16, 2, 5, 512], F32, kind="Internal").ap()

    top = contextlib.ExitStack()
    with top:
        P = Prog(nc, lambda n: top.enter_context(nc.semaphore(n)))

        def MM(out, lhsT, rhs, st, sp, R, W):
            P.op("pe", lambda e: e.matmul(out, lhsT, rhs, start=st, stop=sp), R, W)

        def TR(out, in_, idn, R, W):
            P.op("pe", lambda e: e.transpose(out, in_, idn), R, W)

        def ACT(out, in_, func, R, W, **kw):
            P.op("act", lambda e: e.activation(out, in_, func, **kw), R, W)

        def TT(eng, out, a, b, op, R, W):
            P.op(eng, lambda e: e.tensor_tensor(out, a, b, op), R, W)

        def TS(eng, out, a, s1, s2, op0, op1, R, W):
            if s2 is None:
                P.op(eng, lambda e: e.tensor_scalar(out, a, s1, None, op0), R, W)
            else:
                P.op(eng, lambda e: e.tensor_scalar(out, a, s1, s2, op0, op1), R, W)

        def STT(eng, out, a, s, b, op0, op1, R, W):
            P.op(eng, lambda e: e.scalar_tensor_tensor(out, a, s, b, op0, op1), R, W)

        def CP(eng, out, in_, R, W):
            if eng == "act":
                P.op(eng, lambda e: e.copy(out, in_), R, W)
            else:
                P.op(eng, lambda e: e.tensor_copy(out, in_), R, W)

        def RED(eng, out, in_, op, R, W):
            P.op(eng, lambda e: e.tensor_reduce(out, in_, AX.X, op), R, W)

        def MEMSET(eng, ap, val, W):
            P.op(eng, lambda e: e.memset(ap, val), [], W)

        def RECIP(out, in_, R, W):
            P.op("dve", lambda e: e.reciprocal(out, in_), R, W)

        def RSQRT(ap, R, W):
            ACT(ap, ap, AF.Sqrt, R, W)
            RECIP(ap, ap, R, W)

        def sbt(es, name, shape, dt):
            return es.enter_context(nc.sbuf_tensor(name, list(shape), dt))

        def pst(es, name, shape, dt):
            return es.enter_context(nc.psum_tensor(name, list(shape), dt))

        cst = sbt(top, "cst", [128, 1024], F32); b_cst = P.buf()
        cTb = sbt(top, "cTb", [128, 16, 1040], BF16)
        identb = sbt(top, "identb", [128, 128], BF16)
        ident = cst[:, 0:128]
        mask_ur = cst[:, 128:384]
        mask_sl = cst[:, 384:512]
        tri_le = cst[:, 512:640]; tri_lt = cst[:, 640:768]; tri_gt = cst[:, 768:896]
        esel = cst[:, 896:898]
        ones_col = cst[:, 962:963]
        P.dma("sp", cst[:], consts, [], [b_cst], "cst")
        if os.environ.get("KB_SIMINIT"):
            P.op("dve", lambda e: e.memset(cTb[:], 0.0), [], [b_cst])
        CP("dve", identb[:], ident, [b_cst], [b_cst])
        P.flush()

        rr = [0]

        def rmsnorm_tile(es_bufs, xt, np_, gb, hb, ss, rstd, tmp, R, W, b_tmp):
            ACT(tmp[0:np_, :], xt[0:np_, :], AF.Square, R, [b_tmp], accum_out=ss[0:np_, :])
            TS("dve", rstd[0:np_, :], ss[0:np_, :], 1.0 / D, RMS_EPS, ALU.mult, ALU.add, [b_tmp], [b_tmp])
            RSQRT(rstd[0:np_, :], [b_tmp], [b_tmp])
            STT("dve", hb[0:np_, :], xt[0:np_, :], rstd[0:np_, 0:1], gb[0:np_, :], ALU.mult, ALU.mult,
                R + [b_tmp], W)

        NSLOTV = [2]
        wring = [None] * 4
        b_ring = [None] * 4
        ring_ctr = [0]
        ring_gen = [0]

        def alloc_ring(es_, n=2):
            ring_gen[0] += 1
            NSLOTV[0] = n
            ring_ctr[0] = 0
            for i_ in range(n):
                wring[i_] = sbt(es_, "wring%d_%d" % (i_, ring_gen[0]), [128, 16, 512], BF16)
                b_ring[i_] = None

        def stream_w(src_ap, kc, ncols):
            i = ring_ctr[0] % NSLOTV[0]
            ring_ctr[0] += 1
            if b_ring[i] is None or b_ring[i] not in P.bufs:
                b_ring[i] = P.buf()
            P.dma("pool", wring[i][:, 0:kc, 0:ncols], src_ap.rearrange("(c p) n -> p c n", p=128),
                  [], [b_ring[i]], "ring%d" % i)
            return wring[i], b_ring[i]

        with contextlib.ExitStack() as es:
            uT = sbt(es, "uT", [128, 8, 1072], F32); b_uT = [P.buf() for _ in range(8)]
            esA = contextlib.ExitStack()
            alloc_ring(esA)
            NT = NW * 128 + 16
            hT = sbt(esA, "hT", [128, 16, NT], BF16); b_hT = P.buf()
            gb = sbt(esA, "gb", [128, D], F32); b_gb = P.buf()
            P.dma("sp", gb[:], norm_mix.partition_broadcast(128), [], [b_gb], "gb")
            xt2 = [sbt(esA, "xt0", [128, D], F32)] * 2
            b_xt = [P.buf()] * 2
            hb = sbt(esA, "hb", [128, D], BF16); b_hb = P.buf()
            ss = sbt(esA, "ss", [128, 1], F32); rstd = sbt(esA, "rstd", [128, 1], F32); b_tmp = P.buf()
            pT = [pst(esA, "pT%d" % i, [128, 8, 128], BF16) for i in range(2)]
            b_pT = [P.buf() for _ in range(2)]
            zrow = sbt(esA, "zrow", [105, 32], F32); b_z = P.buf()
            MEMSET("dve", zrow[:], 0.0, [b_z])
            P.dma("sp", q_scr[0, :].rearrange("(p n) -> p n", n=32), zrow[:], [b_z], [], "zrow")
            for i in range(NW + 1):
                np_ = 128 if i < NW else 16
                xt = xt2[i % 2]; bx = b_xt[i % 2]
                src = xw[i * 128:(i + 1) * 128, :] if i < NW else xs
                P.dma("sp" if i % 2 == 0 else "act", xt[0:np_, :], src, [], [bx], "xl0")
                rmsnorm_tile(es, xt, np_, gb, hb, ss, rstd, hb, [bx, b_gb], [b_hb], b_hb)
                for hf in range(2):
                    for j in range(8):
                        dc = hf * 8 + j
                        TR(pT[hf][0:128, j, 0:np_], hb[0:np_, dc * 128:(dc + 1) * 128], identb[0:np_, 0:np_],
                           [b_hb, b_cst], [b_pT[hf]])
                    CP("act" if hf == 0 else "dve", hT[:, hf * 8:(hf + 1) * 8, i * 128:i * 128 + np_],
                       pT[hf][:, :, 0:np_], [b_pT[hf]], [b_hT])
            pq = [pst(esA, "pq%d" % i, [128, 512], F32) for i in range(3)]
            b_pq = [P.buf() for _ in range(3)]
            qst = [sbt(esA, "qst%d" % i, [128, 512], F32) for i in range(3)]
            b_qst = [P.buf() for _ in range(3)]
            cnt = 0
            for cb in range(7):
                c0 = 2048 + cb * 512
                ncol = min(512, IN_W - c0)
                slot, bs = stream_w(w_in[:, c0:c0 + ncol], 16, ncol)
                for i in range(NW + 1):
                    np_ = 128 if i < NW else 16
                    k = cnt % 3
                    cnt += 1
                    for dc in range(16):
                        MM(pq[k][0:np_, 0:ncol], hT[:, dc, i * 128:i * 128 + np_], slot[:, dc, 0:ncol],
                           dc == 0, dc == 15, [b_hT, bs], [b_pq[k]])
                    CP("act" if cnt % 2 == 0 else "dve", qst[k][0:np_, 0:ncol], pq[k][0:np_, 0:ncol],
                       [b_pq[k]], [b_qst[k]])
                    P.dma("sp", q_scr[1 + i * 128:1 + i * 128 + np_, c0 - 2048:c0 - 2048 + ncol],
                          qst[k][0:np_, 0:ncol], [b_qst[k]], [], "qst%d" % k)
            chunks = [(992, 512), (1504, 512), (2016, 48)]
            sg = [sbt(esA, "sg%d" % i, [128, 512], F32) for i in range(2)]
            b_sg = [P.buf() for _ in range(2)]
            for cb in range(4):
                slot, bs = stream_w(w_in[:, cb * 512:(cb + 1) * 512], 16, 512)
                for fb in range(4):
                    fblk = cb * 4 + fb
                    for (n0, nn) in chunks:
                        k = cnt % 3
                        cnt += 1
                        for dc in range(16):
                            MM(pq[k][:, 0:nn], slot[:, dc, fb * 128:(fb + 1) * 128], hT[:, dc, n0:n0 + nn],
                               dc == 0, dc == 15, [b_hT, bs], [b_pq[k]])
                        if fblk < 8:
                            CP("dve", uT[:, fblk, n0 - 992:n0 - 992 + nn], pq[k][:, 0:nn], [b_pq[k]], [b_uT[fblk]])
                        else:
                            ACT(sg[k % 2][:, 0:nn], pq[k][:, 0:nn], AF.Sigmoid, [b_pq[k]], [b_sg[k % 2]])
                            TT("dve", uT[:, fblk - 8, n0 - 992:n0 - 992 + nn], uT[:, fblk - 8, n0 - 992:n0 - 992 + nn],
                               sg[k % 2][:, 0:nn], ALU.mult, [b_sg[k % 2], b_uT[fblk - 8]], [b_uT[fblk - 8]])
            P.flush()
            esA.close()
            P.dma("sp", shiftp_o, q_scr[NW * 128:NW * 128 + 1, :], [], [], "sho")
            P.dma("sp", shifts_o, q_scr[1 + NW * 128:1 + NW * 128 + 16, :], [], [], "sho")

            with contextlib.ExitStack() as es2:
                cw_tm = sbt(es2, "cw_tm", [31, CW], F32); b_cw = P.buf()
                P.dma("sp", cw_tm[:], conv_w, [], [b_cw], "cw")
                pv = sbt(es2, "pv", [24, 128], F32); b_pv = P.buf()
                P.dma("sp", pv[0:8, :], conv_b.rearrange("(b p) -> b p", p=128), [], [b_pv], "cw")
                P.dma("sp", pv[8:16, :], conv_ln_g.rearrange("(b p) -> b p", p=128), [], [b_pv], "cw")
                P.dma("sp", pv[16:24, :], conv_ln_b.rearrange("(b p) -> b p", p=128), [], [b_pv], "cw")
                cwT = sbt(es2, "cwT", [128, 8, 31], F32); b_cwT = P.buf()
                pvT = sbt(es2, "pvT", [128, 24], F32)
                pc = pst(es2, "pc", [128, 8, 32], F32); b_pc = P.buf()
                pc2 = pst(es2, "pc2", [128, 32], F32); b_pc2 = P.buf()
                for blk in range(8):
                    TR(pc[:, blk, 0:31], cw_tm[:, blk * 128:(blk + 1) * 128], ident[0:31, 0:31], [b_cw, b_cst], [b_pc])
                CP("dve", cwT[:], pc[:, :, 0:31], [b_pc], [b_cwT])
                TR(pc2[:, 0:24], pv[:], ident[0:24, 0:24], [b_pv, b_cst], [b_pc2])
                CP("dve", pvT[:], pc2[:, 0:24], [b_pc2], [b_cwT])
                cT = sbt(es2, "cT", [128, 8, 1024], F32); b_cT = [P.buf() for _ in range(8)]
                for blk in range(8):
                    eng = "dve"
                    TS(eng, cT[:, blk, :], uT[:, blk, 2:1026], cwT[:, blk, 0:1], pvT[:, blk:blk + 1], ALU.mult, ALU.add,
                       [b_uT[blk], b_cwT], [b_cT[blk]])
                    for j in range(1, 31):
                        STT(eng, cT[:, blk, :], uT[:, blk, 2 + j:1026 + j], cwT[:, blk, j:j + 1], cT[:, blk, :],
                            ALU.mult, ALU.add, [b_uT[blk], b_cwT, b_cT[blk]], [b_cT[blk]])
                urow = sbt(es2, "urow", [30, CW], F32); b_urow = P.buf()
                us = sbt(es2, "us", [16, CW], F32); b_us = P.buf()
                pu = pst(es2, "pu", [32, 1024], F32); b_pu = P.buf()
                for blk in range(8):
                    TR(pu[0:30, blk * 128:(blk + 1) * 128], uT[:, blk, 1026:1056], ident, [b_uT[blk], b_cst], [b_pu])
                CP("act", urow[:], pu[0:30, :], [b_pu], [b_urow])
                P.dma("sp", convp_o, urow[:], [b_urow], [], "cpo")
                for blk in range(8):
                    TR(pu[0:16, blk * 128:(blk + 1) * 128], uT[:, blk, 1056:1072], ident, [b_uT[blk], b_cst], [b_pu])
                CP("act", us[:], pu[0:16, :], [b_pu], [b_us])
                P.dma("sp", convs_o[:, 29, :], us[:], [b_us], [], "cso")
                P.dma("act", convs_o[:, 0:29, :], cconv[:, 1:30, :], [], [], "cso2")
                onesm = sbt(es2, "onesm", [128, 128], F32); b_ones = P.buf()
                MEMSET("dve", onesm[:], 1.0 / CW, [b_ones])
                ps1 = pst(es2, "ps1", [128, 512], F32); b_ps1 = P.buf()
                ps2 = pst(es2, "ps2", [128, 512], F32); b_ps2 = P.buf()
                sqc = [sbt(es2, "sqc%d" % i, [128, 512], F32) for i in range(2)]
                b_sqc = [P.buf() for _ in range(2)]
                mean = sbt(es2, "mean", [128, 512], F32); rs = sbt(es2, "rs", [128, 512], F32); b_st = P.buf()
                tn = [sbt(es2, "tn%d" % i, [128, 512], F32) for i in range(2)]
                b_tn = [P.buf() for _ in range(2)]
                b_cTb = P.buf()
                for ncx in range(2):
                    n0 = ncx * 512
                    for blk in range(8):
                        MM(ps1[:], onesm[:], cT[:, blk, n0:n0 + 512], blk == 0, blk == 7, [b_ones, b_cT[blk]], [b_ps1])
                        ACT(sqc[blk % 2][:], cT[:, blk, n0:n0 + 512], AF.Square, [b_cT[blk]], [b_sqc[blk % 2]])
                        MM(ps2[:], onesm[:], sqc[blk % 2][:], blk == 0, blk == 7, [b_ones, b_sqc[blk % 2]], [b_ps2])
                    CP("dve", mean[:], ps1[:], [b_ps1], [b_st])
                    TT("dve", rs[:], mean[:], mean[:], ALU.mult, [b_st], [b_st])
                    TT("dve", rs[:], ps2[:], rs[:], ALU.subtract, [b_ps2, b_st], [b_st])
                    TS("dve", rs[:], rs[:], LN_EPS, None, ALU.add, None, [b_st], [b_st])
                    RSQRT(rs[:], [b_st], [b_st])
                    for blk in range(8):
                        t = tn[blk % 2]; bt = b_tn[blk % 2]
                        TT("dve", t[:], cT[:, blk, n0:n0 + 512], mean[:], ALU.subtract, [b_cT[blk], b_st], [bt])
                        TT("dve", t[:], t[:], rs[:], ALU.mult, [bt, b_st], [bt])
                        ACT(cTb[:, blk, n0:n0 + 512], t[:], AF.Silu, [bt, b_cwT], [b_cTb],
                            bias=pvT[:, 16 + blk:17 + blk], scale=pvT[:, 8 + blk:9 + blk])
                wrep = sbt(es2, "wrep", [120, CW], F32); b_wrep = P.buf()
                for r4 in range(4):
                    P.dma("sp", wrep[r4 * 30:(r4 + 1) * 30, :], conv_w[0:30, :], [], [b_wrep], "wrep")
                bc16 = sbt(es2, "bc16", [16, 4, CW], F32); b_bc16 = P.buf()
                P.dma("sp", bc16[:, 0, :], conv_w[30, :].partition_broadcast(16), [], [b_bc16], "bc16")
                P.dma("sp", bc16[:, 1, :], conv_b.partition_broadcast(16), [], [b_bc16], "bc16")
                P.dma("sp", bc16[:, 2, :], conv_ln_g.partition_broadcast(16), [], [b_bc16], "bc16")
                P.dma("sp", bc16[:, 3, :], conv_ln_b.partition_broadcast(16), [], [b_bc16], "bc16")
                cch = [sbt(es2, "cch%d" % i, [120, CW], F32) for i in range(2)]
                b_cch = [P.buf() for _ in range(2)]
                pcs = pst(es2, "pcs", [16, 1024], F32); b_pcs = P.buf()
                for i4 in range(4):
                    t = cch[i4 % 2]; bt = b_cch[i4 % 2]
                    P.dma("sp", t[:], cconv[i4 * 4:(i4 + 1) * 4, :, :].rearrange("s j c -> (s j) c"), [], [bt], "cch%d" % (i4 % 2))
                    TT("dve", t[:], t[:], wrep[:], ALU.mult, [bt, b_wrep], [bt])
                    for hf in range(2):
                        MM(pcs[:, hf * 512:(hf + 1) * 512], cst[0:120, 898 + i4 * 16:898 + (i4 + 1) * 16],
                           t[:, hf * 512:(hf + 1) * 512], i4 == 0, i4 == 3, [bt, b_cst], [b_pcs])
                cs = sbt(es2, "cs", [16, CW], F32); b_cs = P.buf()
                cs2 = sbt(es2, "cs2", [16, CW], F32)
                st16 = sbt(es2, "st16", [16, 4], F32)
                TT("dve", cs[:], us[:], bc16[:, 0, :], ALU.mult, [b_us, b_bc16], [b_cs])
                TT("dve", cs[:], cs[:], pcs[:], ALU.add, [b_cs, b_pcs], [b_cs])
                TT("dve", cs[:], cs[:], bc16[:, 1, :], ALU.add, [b_cs, b_bc16], [b_cs])
                ACT(cs2[:], cs[:], AF.Copy, [b_cs], [b_cs], accum_out=st16[:, 0:1])
                TS("dve", st16[:, 0:1], st16[:, 0:1], 1.0 / CW, None, ALU.mult, None, [b_cs], [b_cs])
                TS("dve", cs[:], cs[:], st16[:, 0:1], None, ALU.subtract, None, [b_cs], [b_cs])
                ACT(cs2[:], cs[:], AF.Square, [b_cs], [b_cs], accum_out=st16[:, 1:2])
                TS("dve", st16[:, 1:2], st16[:, 1:2], 1.0 / CW, LN_EPS, ALU.mult, ALU.add, [b_cs], [b_cs])
                RSQRT(st16[:, 1:2], [b_cs], [b_cs])
                STT("dve", cs[:], cs[:], st16[:, 1:2], bc16[:, 2, :], ALU.mult, ALU.mult, [b_cs, b_bc16], [b_cs])
                TT("dve", cs[:], cs[:], bc16[:, 3, :], ALU.add, [b_cs, b_bc16], [b_cs])
                ACT(cs2[:], cs[:], AF.Silu, [b_cs], [b_cs])
                for blk in range(8):
                    TR(pc[:, blk, 0:16], cs2[:, blk * 128:(blk + 1) * 128], ident[0:16, 0:16], [b_cs, b_cst], [b_pc])
                CP("dve", cTb[:, 0:8, 1024:1040], pc[:, :, 0:16], [b_pc], [b_cTb])
                P.flush()
        if stop_after == "A":
            P.flush(final=True)
            return nc

        def load_bc(es_, name, src, n, np_=128):
            t = sbt(es_, name, [np_, n], F32)
            b = P.buf()
            P.dma("sp", t[:], src.partition_broadcast(np_), [], [b], "bc")
            return t, b

        with contextlib.ExitStack() as es:
            mu_b, b_mu = load_bc(es, "mu_b", shift_mu, SHIFT_W)
            kk_b, b_par = load_bc(es, "kk_b", k_k, 1024)
            ka_b, _b = load_bc(es, "ka_b", k_a, 1024); rk_b, _b2 = load_bc(es, "rk_b", r_k, 1024)
            lg_b, _b3 = load_bc(es, "lg_b", lnx_g, 1024); lb_b, _b4 = load_bc(es, "lb_b", lnx_b, 1024)
            b_pars = [b_par, _b, _b2, _b3, _b4]
            wdu = sbt(es, "wdu", [65, 1024], F32); wau = sbt(es, "wau", [65, 1024], F32)
            wgu = sbt(es, "wgu", [128, 2, 1024], F32); b_lw = P.buf()
            P.dma("sp", wdu[0:64, :], w_decay_up, [], [b_lw], "lw")
            P.dma("sp", wdu[64:65, :], decay_bias.rearrange("(o n) -> o n", o=1), [], [b_lw], "lw")
            P.dma("sp", wau[0:64, :], w_a_up, [], [b_lw], "lw")
            P.dma("sp", wau[64:65, :], a_bias.rearrange("(o n) -> o n", o=1), [], [b_lw], "lw")
            P.dma("sp", wgu[:, 0, :], w_g_up[0:128, :], [], [b_lw], "lw")
            P.dma("sp", wgu[0:32, 1, :], w_g_up[128:160, :], [], [b_lw], "lw")
            q = sbt(es, "q", [128, SHIFT_W], F32); b_q = P.buf()
            qp = sbt(es, "qp", [128, SHIFT_W], F32); b_qp = P.buf()
            Wt_full = [sbt(es, "W%d" % i, [128, 1024], F32) for i in range(3, 7)]
            Wt = [qp[:, 0:1024], qp[:, 1024:2048], qp[:, 2048:3072]] + [w_[:] for w_ in Wt_full]
            b_W = [b_qp, b_qp, b_qp] + [P.buf() for _ in range(4)]
            loT = sbt(es, "loT", [128, 4, 128], F32); b_loT = P.buf()
            MEMSET("dve", loT[:], 1.0, [b_loT])
            lo_in = sbt(es, "lo_in", [128, 288], F32); b_loin = P.buf()
            sm = sbt(es, "sm", [128, 64], F32); b_sm = P.buf()
            Ot = Wt[3]; b_Ot = b_W[3]
            W7 = sbt(es, "W7", [128, 1024], F32); b_W7 = P.buf()
            S = sbt(es, "S", [128, 8, 64], F32); b_S = P.buf()
            b_S2 = [P.buf(), P.buf()]; b_tA2 = [P.buf(), P.buf()]; b_Sw2 = [P.buf(), P.buf()]; b_sa2 = [P.buf(), P.buf()]
            b_tB2 = [P.buf(), P.buf()]; b_tC2 = [P.buf(), P.buf()]; b_tD2 = [[P.buf(), P.buf()], [P.buf(), P.buf()]]
            MEMSET("dve", S[:], 0.0, b_S2)
            tA = sbt(es, "tA", [128, 8, 64], F32); b_tA = P.buf()
            Sw = sbt(es, "Sw", [128, 8, 64], F32); b_Sw = P.buf()
            tB = sbt(es, "tB", [128, 8, 64], F32); b_tB = P.buf()
            tC = sbt(es, "tC", [128, 8, 64], F32); b_tC = P.buf()
            tD = [sbt(es, "tD%d" % i, [128, 8, 64], F32) for i in range(2)]; b_tD = [P.buf() for _ in range(2)]
            sa = sbt(es, "sa", [128, 8], F32); b_sa = P.buf()
            vT = sbt(es, "vT", [128, 8, 128], F32); b_vT = P.buf()
            oT = sbt(es, "oT", [128, 8, 128], F32); b_oT = P.buf()
            NB = 4
            bc = [sbt(es, "bc%d" % i, [128, 2560], F32) for i in range(NB)]; b_bc = [P.buf() for _ in range(NB)]
            b_scr = P.buf()
            bBp = [P.buf() for _ in range(NB)]
            bk = [pst(es, "bk%d" % i, [128, 512], F32) for i in range(8)]
            b_bk = [P.buf() for _ in range(8)]
            pL = [bk[0], bk[1]]; b_pL = [b_bk[0], b_bk[1]]
            pA = [bk[3], bk[4]]; b_pA = [b_bk[3], b_bk[4]]
            pB = [bk[2][:].rearrange("p (a b) -> p a b", b=128), bk[5][:].rearrange("p (a b) -> p a b", b=128)]
            b_pB = [b_bk[2], b_bk[5]]
            vps = [bk[6][:].rearrange("p (a b) -> p a b", b=128), bk[7][:].rearrange("p (a b) -> p a b", b=128)]

            def vec_part(np_, own):
                r_ = slice(0, np_)
                TT("dve", qp[r_, :], qp[r_, :], q[r_, :], ALU.subtract, [b_qp, b_q], [b_qp])
                TT("pool", qp[r_, :], qp[r_, :], mu_b[r_, :], ALU.mult, [b_qp, b_mu], [b_qp])
                TT("dve", q[r_, :], q[r_, :], qp[r_, :], ALU.add, [b_qp, b_q], [b_q])
                ACT(lo_in[r_, 0:64], q[r_, 3072:3136], AF.Tanh, [b_q], [b_loin])
                CP("act", lo_in[r_, 64:128], q[r_, 3136:3200], [b_q], [b_loin])
                ACT(lo_in[r_, 128:288], q[r_, 3200:3360], AF.Sigmoid, [b_q], [b_loin])
                k = 0
                TR(pB[k][0:64, 0, 0:np_], lo_in[r_, 0:64], ident[r_, r_], [b_loin, b_cst], [b_pB[k]])
                TR(pB[k][0:64, 1, 0:np_], lo_in[r_, 64:128], ident[r_, r_], [b_loin, b_cst], [b_pB[k]])
                TR(pB[k][0:128, 2, 0:np_], lo_in[r_, 128:256], ident[r_, r_], [b_loin, b_cst], [b_pB[k]])
                TR(pB[k][0:32, 3, 0:np_], lo_in[r_, 256:288], ident[r_, r_], [b_loin, b_cst], [b_pB[k]])
                CP("dve", loT[0:64, 0:2, 0:np_], pB[k][0:64, 0:2, 0:np_], [b_pB[k]], [b_loT])
                CP("dve", loT[:, 2, 0:np_], pB[k][:, 2, 0:np_], [b_pB[k]], [b_loT])
                CP("dve", loT[0:32, 3, 0:np_], pB[k][0:32, 3, 0:np_], [b_pB[k]], [b_loT])
                for hf in range(2):
                    c = slice(hf * 512, (hf + 1) * 512)
                    MM(pL[0][r_, :], loT[0:65, 0, r_], wdu[0:65, c], True, True, [b_loT, b_lw], [b_pL[0]])
                    ACT(Wt[0][r_, c], pL[0][r_, :], AF.Sigmoid, [b_pL[0]], [b_W[0]])
                    MM(pL[1][r_, :], loT[0:65, 1, r_], wau[0:65, c], True, True, [b_loT, b_lw], [b_pL[1]])
                    ACT(Wt[1][r_, c], pL[1][r_, :], AF.Sigmoid, [b_pL[1]], [b_W[1]])
                    if own:
                        MM(pA[hf][r_, :], loT[0:128, 2, r_], wgu[:, 0, c], True, False, [b_loT, b_lw], [b_pA[hf]])
                        MM(pA[hf][r_, :], loT[0:32, 3, r_], wgu[0:32, 1, c], False, True, [b_loT, b_lw], [b_pA[hf]])
                        CP("act", Wt[2][r_, c], pA[hf][r_, :], [b_pA[hf]], [b_W[2]])
                kv = q[r_, 1024:2048]; rv = q[r_, 0:1024]
                a = Wt[1][r_, :]
                TT("dve", Wt[3][r_, :], kv, kk_b[r_, :], ALU.mult, [b_q] + b_pars, [b_W[3]])
                TT("pool", Wt[5][r_, :], Wt[3][r_, :], Wt[3][r_, :], ALU.mult, [b_W[3]], [b_W[5]])
                RED("dve", sm[r_, 16:32], Wt[5][r_, :].rearrange("p (h k) -> p h k", k=64), ALU.add, [b_W[5]], [b_sm])
                ACT(sm[r_, 16:32], sm[r_, 16:32], AF.Sqrt, [b_sm], [b_sm])
                TS("dve", sm[r_, 16:32], sm[r_, 16:32], 1e-12, None, ALU.max, None, [b_sm], [b_sm])
                RECIP(sm[r_, 16:32], sm[r_, 16:32], [b_sm], [b_sm])
                TT("dve", Wt[3][r_, :].rearrange("p (h k) -> p h k", k=64), Wt[3][r_, :].rearrange("p (h k) -> p h k", k=64),
                   sm[r_, 16:32].unsqueeze(2).broadcast_to([np_, 16, 64]), ALU.mult, [b_W[3], b_sm], [b_W[3]])
                TT("pool", Wt[4][r_, :], Wt[3][r_, :], a, ALU.mult, [b_W[3], b_W[1]], [b_W[4]])
                STT("dve", Wt[5][r_, :], a, -1.0, ka_b[r_, :], ALU.add, ALU.mult, [b_W[1]] + b_pars, [b_W[5]])
                STT("dve", Wt[5][r_, :], Wt[5][r_, :], 1.0, kv, ALU.add, ALU.mult, [b_W[5], b_q], [b_W[5]])
                if own:
                    TT("pool", Wt[6][r_, :], rv, rk_b[r_, :], ALU.mult, [b_q] + b_pars, [b_W[6]])
                    TT("pool", Wt[6][r_, :], Wt[6][r_, :], Wt[5][r_, :], ALU.mult, [b_W[6], b_W[5]], [b_W[6]])
                    RED("dve", sm[r_, 0:16], Wt[6][r_, :].rearrange("p (h k) -> p h k", k=64), ALU.add, [b_W[6]], [b_sm])

            def out_part(np_, col0):
                r_ = slice(0, np_)
                O3 = Ot[r_, :].rearrange("p (h k) -> p h k", k=64)
                RED("dve", sm[r_, 32:48], O3, ALU.add, [b_Ot], [b_sm])
                TS("dve", sm[r_, 32:48], sm[r_, 32:48], 1.0 / 64, None, ALU.mult, None, [b_sm], [b_sm])
                TT("dve", O3, O3, sm[r_, 32:48].unsqueeze(2).broadcast_to([np_, 16, 64]), ALU.subtract, [b_Ot, b_sm], [b_Ot])
                TT("pool", Wt[6][r_, :], Ot[r_, :], Ot[r_, :], ALU.mult, [b_Ot], [b_W[6]])
                RED("dve", sm[r_, 48:64], Wt[6][r_, :].rearrange("p (h k) -> p h k", k=64), ALU.add, [b_W[6]], [b_sm])
                TS("dve", sm[r_, 48:64], sm[r_, 48:64], 1.0 / 64, GN_EPS, ALU.mult, ALU.add, [b_sm], [b_sm])
                RSQRT(sm[r_, 48:64], [b_sm], [b_sm])
                TT("dve", O3, O3, sm[r_, 48:64].unsqueeze(2).broadcast_to([np_, 16, 64]), ALU.mult, [b_Ot, b_sm], [b_Ot])
                TT("pool", Ot[r_, :], Ot[r_, :], lg_b[r_, :], ALU.mult, [b_Ot] + b_pars, [b_Ot])
                TT("dve", Ot[r_, :], Ot[r_, :], lb_b[r_, :], ALU.add, [b_Ot] + b_pars, [b_Ot])
                V3 = q[r_, 2048:3072].rearrange("p (h k) -> p h k", k=64)
                W63 = Wt[6][r_, :].rearrange("p (h k) -> p h k", k=64)
                TT("dve", W63, V3, sm[r_, 0:16].unsqueeze(2).broadcast_to([np_, 16, 64]), ALU.mult, [b_q, b_sm], [b_W[6]])
                TT("dve", Ot[r_, :], Ot[r_, :], Wt[6][r_, :], ALU.add, [b_Ot, b_W[6]], [b_Ot])
                TT("dve", Ot[r_, :], Ot[r_, :], Wt[2][r_, :], ALU.mult, [b_Ot, b_W[2]], [b_Ot])
                for hf in range(2):
                    for j in range(4):
                        blk = hf * 4 + j
                        TR(pB[hf][:, j, 0:np_], Ot[r_, blk * 128:(blk + 1) * 128], ident[r_, r_], [b_Ot, b_cst], [b_pB[hf]])
                    CP("act", cTb[:, 8 + hf * 4:12 + hf * 4, col0:col0 + np_], pB[hf][:, :, 0:np_], [b_pB[hf]], [b_cTbB])

            b_cTbB = P.buf()
            NTL = int(os.environ.get('KB_TILES', NW))
            for i in list(range(NTL)) + [NW]:
                samp = (i == NW)
                np_ = 16 if samp else 128
                own = samp or i >= NW - NOWN
                r_ = slice(0, np_)
                if samp:
                    P.dma("sp", q[r_, :], q_scr[1 + NW * 128:1 + NW * 128 + 16, :], [], [b_q], "ql")
                    P.dma("act", qp[r_, :], sshift, [], [b_qp], "qpl")
                else:
                    P.dma("sp", q[:], q_scr[1 + i * 128:1 + (i + 1) * 128, :], [], [b_q], "ql")
                    P.dma("act", qp[:], q_scr[i * 128:(i + 1) * 128, :], [], [b_qp], "qpl")
                vec_part(np_, own)
                if samp:
                    ACT(Wt[6][r_, :], Wt[0][r_, :], AF.Exp, [b_W[0], b_sm], [b_W[6]], scale=-DS)
                    ACT(W7[r_, :], Wt[0][r_, :], AF.Exp, [b_W[0], b_sm], [b_W7], scale=DS)
                else:
                    for hf in range(2):
                        c = slice(hf * 512, (hf + 1) * 512)
                        MM(pL[0][:], tri_le, Wt[0][:, c], True, True, [b_cst, b_W[0]], [b_pL[0]])
                        ACT(Wt[6][:, c], pL[0][:], AF.Exp, [b_pL[0], b_sm], [b_W[6]])
                        ACT(W7[:, c], pL[0][:], AF.Exp, [b_pL[0]], [b_W7], scale=-1.0)
                TT("dve", Wt[4][r_, :], Wt[4][r_, :], W7[r_, :], ALU.mult, [b_W[4], b_W7], [b_W[4]])
                TT("pool", Wt[5][r_, :], Wt[5][r_, :], W7[r_, :], ALU.mult, [b_W[5], b_W7], [b_W[5]])
                if not samp:
                    for hf in range(2):
                        c = slice(hf * 512, (hf + 1) * 512)
                        MM(pL[1][:], tri_lt, Wt[0][:, c], True, True, [b_cst, b_W[0]], [b_pL[1]])
                        ACT(W7[:, c], pL[1][:], AF.Exp, [b_pL[1], b_W[4], b_W[5]], [b_W7])
                    TT("dve", Wt[3][r_, :], Wt[3][r_, :], W7[r_, :], ALU.mult, [b_W[3], b_W7], [b_W[3]])
                if own:
                    TT("pool", q[r_, 0:1024], q[r_, 0:1024], Wt[6][r_, :], ALU.mult, [b_q, b_W[6]], [b_q])
                srcs = [(Wt[3], b_W[3]), (Wt[4], b_W[4]), (Wt[5], b_W[5]), (q[:, 0:1024], b_q), (Wt[6], b_W[6])]
                for f, (src, bs) in enumerate(srcs):
                    for hp in range(2):
                        P.dma("sp" if hp == 0 else "act",
                              bscr[i * 128:i * 128 + np_, hp, f, :].rearrange("t (j k) -> t j k", k=64),
                              src[r_, :].rearrange("p (j hp k) -> p hp j k", hp=2, k=64)[:, hp],
                              [bs], [b_scr], "scr")
                for j in range(8):
                    TR(vps[j // 4][:, j % 4, 0:np_], q[r_, 2048 + j * 128:2048 + (j + 1) * 128], ident[r_, r_],
                       [b_q, b_cst], [b_bk[6 + j // 4]])
                CP("act", vT[:, 0:4, 0:np_], vps[0][:, :, 0:np_], [b_bk[6]], [b_vT])
                CP("dve", vT[:, 4:8, 0:np_], vps[1][:, :, 0:np_], [b_bk[7]], [b_vT])
                for t in range(np_):
                    g = i * 128 + t
                    kb = g % NB
                    B_ = bc[kb]; bB = b_bc[kb]
                    for hp in range(2):
                        cend = samp or (t % 64 == 63)
                        nf = 5 if cend else (4 if own else 3)
                        if os.environ.get("KB_Q4", "0") == "1":
                            P.dma("act" if hp == 1 else "sp", B_[hp * 64:(hp + 1) * 64, 0:1024],
                                  bscr[g, hp, 0:2].rearrange("f n -> (f n)").partition_broadcast(64),
                                  [b_scr], [bB], "bc%d" % kb)
                            P.dma("pool", B_[hp * 64:(hp + 1) * 64, 1024:nf * 512],
                                  bscr[g, hp, 2:nf].rearrange("f n -> (f n)").partition_broadcast(64),
                                  [b_scr], [bBp[kb]], "bcp%d" % kb)
                        else:
                            P.dma("act" if hp == 1 else "sp", B_[hp * 64:(hp + 1) * 64, 0:nf * 512],
                                  bscr[g, hp, 0:nf].rearrange("f n -> (f n)").partition_broadcast(64),
                                  [b_scr], [bB], "bc%d" % kb)
                    if samp:
                        for hp in range(2):
                            P.dma("act", S[hp * 64:(hp + 1) * 64, :, :],
                                  srwkv[t].rearrange("(j hp) v k -> hp v j k", hp=2)[hp], [], b_S2, "Sl")
                    fv = lambda f, js: B_[:, f * 512:(f + 1) * 512].rearrange("p (j k) -> p j k", k=64)[:, js, :]
                    td = tD[g % 2]
                    JS = [slice(0, 4), slice(4, 8)]
                    G2 = (0, 1)
                    for g2 in G2:
                        js = JS[g2]
                        TT("pool", td[:, js, :], fv(2, js), vT[:, js, t].unsqueeze(2).broadcast_to([128, 4, 64]), ALU.mult,
                           [bB, b_vT], [b_tD2[g % 2][g2]])
                    for g2 in G2:
                        js = JS[g2]
                        TT("dve", tA[:, js, :], S[:, js, :], fv(0, js), ALU.mult, [b_S2[g2], bB], [b_tA2[g2]])
                    for g2 in G2:
                        js = JS[g2]
                        RED("dve", sa[:, js], tA[:, js, :], ALU.add, [b_tA2[g2]], [b_sa2[g2]])
                    for g2 in G2:
                        js = JS[g2]
                        TT("dve", tB[:, js, :], fv(1, js), sa[:, js].unsqueeze(2).broadcast_to([128, 4, 64]), ALU.mult,
                           [bB, b_sa2[g2]], [b_tB2[g2]])
                    for g2 in G2:
                        js = JS[g2]
                        TT("dve", S[:, js, :], S[:, js, :], tB[:, js, :], ALU.subtract, [b_S2[g2], b_tB2[g2]], [b_S2[g2]])
                    for g2 in G2:
                        js = JS[g2]
                        TT("dve", S[:, js, :], S[:, js, :], td[:, js, :], ALU.add, [b_S2[g2], b_tD2[g % 2][g2]], [b_S2[g2]])
                    if own:
                        for g2 in G2:
                            js = JS[g2]
                            TT("dve", tC[:, js, :], S[:, js, :], fv(3, js), ALU.mult, [b_S2[g2], bB], [b_tC2[g2]])
                        for g2 in G2:
                            js = JS[g2]
                            RED("dve", oT[:, js, t], tC[:, js, :], ALU.add, [b_tC2[g2]], [b_oT])
                    if cend:
                        for g2 in G2:
                            js = JS[g2]
                            TT("dve", S[:, js, :], S[:, js, :], fv(4, js), ALU.mult, [b_S2[g2], bB], [b_S2[g2]])
                    if samp:
                        for hp in range(2):
                            P.dma("sp", rwkvs_o[t].rearrange("(j hp) v k -> hp v j k", hp=2)[hp],
                                  S[hp * 64:(hp + 1) * 64, :, :], b_S2, [], "So")
                if i == NW - 1:
                    for hp in range(2):
                        P.dma("sp", rwkvp_o.rearrange("(j hp) v k -> hp v j k", hp=2)[hp],
                              S[hp * 64:(hp + 1) * 64, :, :], b_S2, [], "So")
                if own:
                    for j in range(8):
                        TR(vps[j // 4][0:np_, j % 4, :], oT[:, j, 0:np_], ident, [b_oT, b_cst], [b_bk[6 + j // 4]])
                    CP("act", Ot[r_, 0:512], bk[6][r_, :], [b_bk[6]], [b_Ot])
                    CP("dve", Ot[r_, 512:1024], bk[7][r_, :], [b_bk[7]], [b_Ot])
                    out_part(np_, 1024 if samp else (i - (NW - NOWN)) * 128)
                P.flush()
        if stop_after == "B":
            P.flush(final=True)
            return nc

        uid = [0]

        def un(n):
            uid[0] += 1
            return "%s_%d" % (n, uid[0])

        TILES = [(i, 128, i * 128) for i in range(8)] + [(8, 16, 1024)]
        ATT_SCALE = 512.0 ** -0.5

        def x_src(ti):
            return xw[1024 + ti * 128:1024 + (ti + 1) * 128, :] if ti < 8 else xs

        def xres_src(ti):
            (_, np_, c0) = TILES[ti]
            return xres[c0:c0 + np_, :]

        def proj_res(w, src_fn):
            with contextlib.ExitStack() as es_:
                alloc_ring(es_)
                pq = [pst(es_, un("pq"), [128, 512], F32) for _ in range(3)]; b_pq = [P.buf() for _ in range(3)]
                xin = [sbt(es_, un("xin"), [128, 512], F32) for _ in range(3)]; b_xin = [P.buf() for _ in range(3)]
                xo = [sbt(es_, un("xo"), [128, 512], F32) for _ in range(3)]; b_xo = [P.buf() for _ in range(3)]
                cnt = 0
                for cb in range(4):
                    slot, bs = stream_w(w[:, cb * 512:(cb + 1) * 512], 16, 512)
                    for (ti, np_, c0) in TILES:
                        k = cnt % 3
                        cnt += 1
                        P.dma("sp", xin[k][0:np_, :], src_fn(ti)[:, cb * 512:(cb + 1) * 512], [], [b_xin[k]], "xin")
                        for dc in range(16):
                            MM(pq[k][0:np_, :], cTb[:, dc, c0:c0 + np_], slot[:, dc, :], dc == 0, dc == 15, [bs], [b_pq[k]])
                        TT("dve", xo[k][0:np_, :], pq[k][0:np_, :], xin[k][0:np_, :], ALU.add, [b_pq[k], b_xin[k]], [b_xo[k]])
                        P.dma("act", xres[c0:c0 + np_, cb * 512:(cb + 1) * 512], xo[k][0:np_, :], [b_xo[k]], [], "xo%d" % k)
                P.flush()

        def norm_T(es_, src_list, g_dram, dst, route=None):
            gb = sbt(es_, un("gb"), [128, D], F32); b_gb = P.buf()
            P.dma("sp", gb[:], g_dram.partition_broadcast(128), [], [b_gb], "gb")
            xt2 = [sbt(es_, un("xt"), [128, D], F32) for _ in range(2)]; b_xt = [P.buf(), P.buf()]
            hb = sbt(es_, un("hb"), [128, D], BF16); b_hb = P.buf()
            ss = sbt(es_, un("ss"), [128, 1], F32); rstd = sbt(es_, un("rstd"), [128, 1], F32)
            pT = [pst(es_, un("pT"), [128, 8, 128], BF16) for _ in range(2)]; b_pT = [P.buf(), P.buf()]
            b_dst = P.buf()
            for n_, (src, np_, c0) in enumerate(src_list):
                xt = xt2[n_ % 2]; bx = b_xt[n_ % 2]
                P.dma("sp" if n_ % 2 == 0 else "act", xt[0:np_, :], src, [], [bx], "xl")
                rmsnorm_tile(None, xt, np_, gb, hb, ss, rstd, hb, [bx, b_gb], [b_hb], b_hb)
                for hf in range(2):
                    for j in range(8):
                        dc = hf * 8 + j
                        TR(pT[hf][0:128, j, 0:np_], hb[0:np_, dc * 128:(dc + 1) * 128], identb[0:np_, 0:np_],
                           [b_hb, b_cst], [b_pT[hf]])
                    CP("act" if hf == 0 else "dve", dst[:, hf * 8:(hf + 1) * 8, c0:c0 + np_], pT[hf][:, :, 0:np_],
                       [b_pT[hf]], [b_dst])
                if route is not None:
                    route(n_, np_, xt, bx, rstd, gb, b_gb, b_hb)

        proj_res(w_out, x_src)
        if stop_after == "C":
            P.flush(final=True)
            return nc

        with contextlib.ExitStack() as esD:
            qT = sbt(esD, "qT", [128, 16, 1040], BF16); b_qT = P.buf()
            kT = sbt(esD, "kT", [128, 16, 256], BF16); b_kT = P.buf()
            Vb = sbt(esD, "Vb", [128, 2, 2048], BF16); b_Vb = P.buf()
            with contextlib.ExitStack() as esa:
                norm_T(esa, [(xres_src(ti), np_, c0) for (ti, np_, c0) in TILES], norm_x, cTb)
                P.flush()
            with contextlib.ExitStack() as es1:
                mnT = sbt(es1, "mnT", [128, 16, 256], BF16)
                with contextlib.ExitStack() as esb:
                    norm_T(esb, [(mem[mt * 128:(mt + 1) * 128, :], 128, mt * 128) for mt in range(2)], norm_mem, mnT)
                    P.flush()
                alloc_ring(es1)
                pq = [pst(es1, un("pq"), [128, 512], F32) for _ in range(3)]; b_pq = [P.buf() for _ in range(3)]
                qtok = sbt(es1, "qtok", [16, D], F32); b_qtok = P.buf()
                stg = [sbt(es1, un("stg"), [128, 512], F32) for _ in range(2)]; b_stg = [P.buf(), P.buf()]
                cnt = 0
                P23 = os.environ.get("KB_P23", "qtkv")
                for cb in range(4 if "q" in P23 else 0):
                    slot, bs = stream_w(w_cq[:, cb * 512:(cb + 1) * 512], 16, 512)
                    for fb in range(4):
                        for (n0, nn) in [(0, 512), (512, 512), (1024, 16)]:
                            k = cnt % 3
                            cnt += 1
                            for dc in range(16):
                                MM(pq[k][:, 0:nn], slot[:, dc, fb * 128:(fb + 1) * 128], cTb[:, dc, n0:n0 + nn],
                                   dc == 0, dc == 15, [bs], [b_pq[k]])
                            CP("act" if cnt % 2 == 0 else "dve", qT[:, cb * 4 + fb, n0:n0 + nn], pq[k][:, 0:nn],
                               [b_pq[k]], [b_qT])
                    if "t" not in P23:
                        continue
                    k = cnt % 3
                    cnt += 1
                    for dc in range(16):
                        MM(pq[k][0:16, :], cTb[:, dc, 1024:1040], slot[:, dc, :], dc == 0, dc == 15, [bs], [b_pq[k]])
                    CP("act", qtok[:, cb * 512:(cb + 1) * 512], pq[k][0:16, :], [b_pq[k]], [b_qtok])
                if "t" in P23:
                    P.dma("sp", qtok_scr, qtok[:], [b_qtok], [], "qtk")
                scnt = 0
                for (w, is_k, o_ap) in ((w_ck, True, memk_o), (w_cv, False, memv_o)):
                    if ("k" if is_k else "v") not in P23:
                        continue
                    for cb in range(4):
                        slot, bs = stream_w(w[:, cb * 512:(cb + 1) * 512], 16, 512)
                        for mt in range(2):
                            k = cnt % 3
                            cnt += 1
                            for dc in range(16):
                                MM(pq[k][:, :], mnT[:, dc, mt * 128:(mt + 1) * 128], slot[:, dc, :], dc == 0, dc == 15,
                                   [bs], [b_pq[k]])
                            s2 = scnt % 2
                            scnt += 1
                            CP("act", stg[s2][:], pq[k][:, :], [b_pq[k]], [b_stg[s2]])
                            P.dma("sp", o_ap[mt * 128:(mt + 1) * 128, cb * 512:(cb + 1) * 512], stg[s2][:], [b_stg[s2]], [],
                                  "mo%d" % s2)
                            if not is_k:
                                CP("dve", Vb[:, mt, cb * 512:(cb + 1) * 512], stg[s2][:], [b_stg[s2]], [b_Vb])
                        if is_k:
                            for fb in range(4):
                                k = cnt % 3
                                cnt += 1
                                for dc in range(16):
                                    MM(pq[k][:, 0:256], slot[:, dc, fb * 128:(fb + 1) * 128], mnT[:, dc, 0:256],
                                       dc == 0, dc == 15, [bs], [b_pq[k]])
                                CP("dve", kT[:, cb * 4 + fb, :], pq[k][:, 0:256], [b_pq[k]], [b_kT])
                P.flush()
            with contextlib.ExitStack() as es4:
                onesb = sbt(es4, "onesb", [128, 128], BF16); b_on = P.buf()
                onesf = sbt(es4, "onesf", [128, 128], F32)
                MEMSET("dve", onesb[:], 1.0, [b_on])
                MEMSET("dve", onesf[:], 1.0, [b_on])
                psc = [pst(es4, un("psc"), [128, 512], F32) for _ in range(2)]; b_psc = [P.buf(), P.buf()]
                pdn = pst(es4, "pdn", [128, 512], F32); b_pdn = P.buf()
                pcx = [pst(es4, un("pcx"), [128, 512], F32) for _ in range(2)]; b_pcx = [P.buf(), P.buf()]
                eT = [sbt(es4, un("eT"), [128, 2, 512], BF16) for _ in range(2)]; b_eT = [P.buf(), P.buf()]
                rden = [sbt(es4, un("rden"), [128, 512], F32) for _ in range(2)]; b_rden = [P.buf(), P.buf()]
                b_ctx = P.buf()
                it = 0
                ccnt = 0
                for tb in range(0 if 'D4' in os.environ.get('KB_SKIP', '') else 2):
                    n0 = tb * 512
                    for h in range(4):
                        k2 = it % 2
                        it += 1
                        for mt in range(2):
                            for dc in range(4):
                                MM(psc[mt][:, :], kT[:, h * 4 + dc, mt * 128:(mt + 1) * 128], qT[:, h * 4 + dc, n0:n0 + 512],
                                   dc == 0, dc == 3, [], [b_psc[mt]])
                            ACT(eT[k2][:, mt, :], psc[mt][:, :], AF.Exp, [b_psc[mt]], [b_eT[k2]], scale=ATT_SCALE)
                        for mt in range(2):
                            MM(pdn[:, :], onesb[:], eT[k2][:, mt, :], mt == 0, mt == 1, [b_on, b_eT[k2]], [b_pdn])
                        RECIP(rden[k2][:], pdn[:, :], [b_pdn], [b_rden[k2]])
                        for dc in range(4):
                            c2 = ccnt % 2
                            ccnt += 1
                            for mt in range(2):
                                MM(pcx[c2][:, :], Vb[:, mt, h * 512 + dc * 128:h * 512 + (dc + 1) * 128], eT[k2][:, mt, :],
                                   mt == 0, mt == 1, [b_eT[k2]], [b_pcx[c2]])
                            TT("dve", cTb[:, h * 4 + dc, n0:n0 + 512], pcx[c2][:, :], rden[k2][:], ALU.mult,
                               [b_pcx[c2], b_rden[k2]], [b_ctx])
                P.flush()
                Ks = [sbt(es4, un("Ks"), [128, 2, D], F32) for _ in range(2)]; b_Ks = [P.buf(), P.buf()]
                Vs = [sbt(es4, un("Vs"), [128, 2, D], F32) for _ in range(2)]; b_Vs = [P.buf(), P.buf()]
                qb = [sbt(es4, un("qb"), [128, D], F32) for _ in range(2)]; b_qb = [P.buf(), P.buf()]
                sc = [sbt(es4, un("sc"), [128, 8], F32) for _ in range(2)]; b_sc = [P.buf(), P.buf()]
                rd4 = [sbt(es4, un("rd4"), [128, 4], F32) for _ in range(2)]; b_rd4 = [P.buf(), P.buf()]
                pct = [psc[0][:, 0:16], psc[1][:, 0:16]]; b_pct = b_psc
                pd4 = [pcx[0][:, 0:4], pcx[1][:, 0:4]]; b_pd4 = b_pcx
                for s_ in range(0 if 'D5' in os.environ.get('KB_SKIP', '') else 16):
                    k2 = s_ % 2
                    P.dma("sp", Ks[k2][:], cmk[s_].rearrange("(mt p) d -> p mt d", p=128), [], [b_Ks[k2]], "ks")
                    P.dma("act", Vs[k2][:], cmv[s_].rearrange("(mt p) d -> p mt d", p=128), [], [b_Vs[k2]], "vs")
                    P.dma("sp", qb[k2][:], qtok_scr[s_, :].partition_broadcast(128), [], [b_qb[k2]], "qb")
                    for mt in range(2):
                        TT("dve" if mt == 0 else "pool", Ks[k2][:, mt, :], Ks[k2][:, mt, :], qb[k2][:], ALU.mult,
                           [b_Ks[k2], b_qb[k2]], [b_Ks[k2]])
                    RED("dve", sc[k2][:], Ks[k2][:].rearrange("p mt (h d) -> p (mt h) d", d=512), ALU.add, [b_Ks[k2]], [b_sc[k2]])
                    ACT(sc[k2][:], sc[k2][:], AF.Exp, [b_sc[k2]], [b_sc[k2]], scale=ATT_SCALE)
                    for mt in range(2):
                        MM(pd4[k2], onesf[:], sc[k2][:, mt * 4:(mt + 1) * 4], mt == 0, mt == 1, [b_on, b_sc[k2]], [b_pd4[k2]])
                    RECIP(rd4[k2][:], pd4[k2], [b_pd4[k2]], [b_rd4[k2]])
                    for kc in range(16):
                        h = kc // 4
                        for mt in range(2):
                            MM(pct[k2][:, kc:kc + 1], Vs[k2][:, mt, kc * 128:(kc + 1) * 128], sc[k2][:, mt * 4 + h:mt * 4 + h + 1],
                               mt == 0, mt == 1, [b_Vs[k2], b_sc[k2]], [b_pct[k2]])
                    TT("dve", cTb[:, :, 1024 + s_].rearrange("p (h c) -> p h c", c=4),
                       pct[k2].rearrange("p (h c) -> p h c", c=4),
                       rd4[k2][:].unsqueeze(2).broadcast_to([128, 4, 4]), ALU.mult, [b_pct[k2], b_rd4[k2]], [b_ctx])
                P.flush()
        proj_res(w_co, xres_src)
        if stop_after == "D":
            P.flush(final=True)
            return nc

        with contextlib.ExitStack() as esE:
            comb = sbt(esE, "comb", [128, 9, 32], F32); b_comb = P.buf()
            with contextlib.ExitStack() as es1:
                wr_sb = sbt(es1, "wr_sb", [128, 16, 36], F32); b_wr = P.buf()
                P.dma("sp", wr_sb[:], w_route.rearrange("(c p) n -> p c n", p=128), [], [b_wr], "wr")
                br_b = sbt(es1, "br_b", [128, 36], F32)
                P.dma("sp", br_b[:], b_route.partition_broadcast(128), [], [b_wr], "wr")
                h2f = sbt(es1, "h2f", [128, D], F32); b_h2f = P.buf()
                h2Tf = sbt(es1, "h2Tf", [128, 16, 128], F32); b_h2Tf = P.buf()
                pTf = [pst(es1, un("pTf"), [128, 4, 128], F32) for _ in range(2)]; b_pTf = [P.buf(), P.buf()]
                plg = pst(es1, "plg", [128, 64], F32); b_plg = P.buf()
                lg = sbt(es1, "lg", [128, 36], F32); b_r = P.buf()
                rt = sbt(es1, "rt", [128, 96], F32)
                gmax = rt[:, 0:1]; ngmax = rt[:, 1:2]; sume = rt[:, 2:3]; gval = rt[:, 3:4]
                m1 = rt[:, 4:5]; m2 = rt[:, 5:6]; dd = rt[:, 6:7]; e1 = rt[:, 7:8]; w1 = rt[:, 8:9]; w2 = rt[:, 9:10]
                ohg = rt[:, 12:16]; junk = rt[:, 16:20]; les = rt[:, 24:32]; oh1 = rt[:, 32:40]; msk = rt[:, 40:48]
                oh2 = rt[:, 48:56]; ec = rt[:, 56:64]; t32 = rt[:, 64:96]

                def route(n_, np_, xt, bx, rstd, gb, b_gb, b_hb):
                    r_ = slice(0, np_)
                    R_ = [b_r]
                    STT("dve", h2f[r_, :], xt[r_, :], rstd[r_, 0:1], gb[r_, :], ALU.mult, ALU.mult, [bx, b_gb, b_hb], [b_h2f])
                    for grp in range(4):
                        pt = pTf[grp % 2]; bpt = b_pTf[grp % 2]
                        for j in range(4):
                            dc = grp * 4 + j
                            TR(pt[:, j, 0:np_], h2f[r_, dc * 128:(dc + 1) * 128], ident[r_, r_], [b_h2f, b_cst], [bpt])
                        CP("act" if grp % 2 == 0 else "dve", h2Tf[:, grp * 4:(grp + 1) * 4, 0:np_], pt[:, :, 0:np_], [bpt], [b_h2Tf])
                    for dc in range(16):
                        MM(plg[r_, 0:36], h2Tf[:, dc, 0:np_], wr_sb[:, dc, :], dc == 0, dc == 15, [b_h2Tf, b_wr], [b_plg])
                    TT("dve", lg[r_, :], plg[r_, 0:36], br_b[r_, :], ALU.add, [b_plg, b_wr], R_)
                    RED("dve", gmax[r_, :], lg[r_, 0:4], ALU.max, R_, R_)
                    TS("dve", ohg[r_, :], lg[r_, 0:4], gmax[r_, :], None, ALU.is_ge, None, R_, R_)
                    TS("dve", ngmax[r_, :], gmax[r_, :], -1.0, None, ALU.mult, None, R_, R_)
                    ACT(junk[r_, :], lg[r_, 0:4], AF.Exp, R_, R_, bias=ngmax[r_, :], scale=1.0, accum_out=sume[r_, :])
                    RECIP(gval[r_, :], sume[r_, :], R_, R_)
                    TT("dve", t32[r_, :].rearrange("p (g e) -> p g e", e=8), lg[r_, 4:36].rearrange("p (g e) -> p g e", e=8),
                       ohg[r_, :].unsqueeze(2).broadcast_to([np_, 4, 8]), ALU.mult, R_, R_)
                    RED("dve", les[r_, :], t32[r_, :].rearrange("p (g e) -> p e g", e=8), ALU.add, R_, R_)
                    RED("dve", m1[r_, :], les[r_, :], ALU.max, R_, R_)
                    TS("dve", oh1[r_, :], les[r_, :], m1[r_, :], None, ALU.is_ge, None, R_, R_)
                    STT("dve", msk[r_, :], oh1[r_, :], -1e30, les[r_, :], ALU.mult, ALU.add, R_, R_)
                    RED("dve", m2[r_, :], msk[r_, :], ALU.max, R_, R_)
                    TS("dve", oh2[r_, :], msk[r_, :], m2[r_, :], None, ALU.is_ge, None, R_, R_)
                    TT("dve", dd[r_, :], m2[r_, :], m1[r_, :], ALU.subtract, R_, R_)
                    ACT(e1[r_, :], dd[r_, :], AF.Exp, R_, R_)
                    TS("dve", w1[r_, :], e1[r_, :], 1.0, None, ALU.add, None, R_, R_)
                    RECIP(w1[r_, :], w1[r_, :], R_, R_)
                    TT("dve", w2[r_, :], e1[r_, :], w1[r_, :], ALU.mult, R_, R_)
                    TT("dve", w1[r_, :], w1[r_, :], gval[r_, :], ALU.mult, R_, R_)
                    TT("dve", w2[r_, :], w2[r_, :], gval[r_, :], ALU.mult, R_, R_)
                    TS("dve", ec[r_, :], oh1[r_, :], w1[r_, :], None, ALU.mult, None, R_, R_)
                    STT("dve", ec[r_, :], oh2[r_, :], w2[r_, :], ec[r_, :], ALU.mult, ALU.add, R_, R_)
                    TT("dve", comb[r_, n_, :].rearrange("p (g e) -> p g e", e=8),
                       ohg[r_, :].unsqueeze(2).broadcast_to([np_, 4, 8]),
                       ec[r_, :].unsqueeze(1).broadcast_to([np_, 4, 8]), ALU.mult, R_, [b_comb])

                norm_T(es1, [(xres_src(ti), np_, c0) for (ti, np_, c0) in TILES], norm_ffn, cTb, route=route)
                P.flush()
            acc = sbt(esE, "acc", [128, 9, D], F32); b_acc = [P.buf() for _ in range(9)]
            for (ti, np_, c0) in TILES:
                P.dma("sp" if ti % 2 == 0 else "act", acc[0:np_, ti, :], xres_src(ti), [], [b_acc[ti]], "acc")
            esM = contextlib.ExitStack()
            alloc_ring(esM, 4)
            wd = sbt(esM, "wd", [128, 4, D], BF16); b_wd = P.buf()
            pg = [pst(esM, un("pg"), [128, 512], F32) for _ in range(2)]; b_pg = [P.buf(), P.buf()]
            pu = [pst(esM, un("pu"), [128, 512], F32) for _ in range(2)]; b_pu = [P.buf(), P.buf()]
            po = [pst(esM, un("po"), [128, 512], F32) for _ in range(3)]; b_po = [P.buf() for _ in range(3)]
            sg = [sbt(esM, un("sg"), [128, 512], F32) for _ in range(2)]; b_sg = [P.buf(), P.buf()]
            aT = [sbt(esM, un("aT"), [128, 4, 512], BF16) for _ in range(2)]; b_aT = [P.buf(), P.buf()]
            gcnt = 0
            ocnt = 0
            bcnt = 0
            NEXP = int(os.environ.get('KB_NEXP', 32))
            for e_ in range(NEXP):
                wg_, b_wg = stream_w(w_gate[e_], 16, 512)
                wu_, b_wu = stream_w(w_up[e_], 16, 512)
                P.dma("pool", wd[:], w_down[e_].rearrange("(c p) n -> p c n", p=128), [], [b_wd], "wd")
                for (n0, nn, tls) in [(0, 512, [0, 1, 2, 3]), (512, 512, [4, 5, 6, 7]), (1024, 16, [8])]:
                    a_ = aT[bcnt % 2]; ba = b_aT[bcnt % 2]
                    bcnt += 1
                    for fc in range(4):
                        k = gcnt % 2
                        gcnt += 1
                        for dc in range(16):
                            MM(pg[k][:, 0:nn], wg_[:, dc, fc * 128:(fc + 1) * 128], cTb[:, dc, n0:n0 + nn], dc == 0, dc == 15,
                               [b_wg], [b_pg[k]])
                        ACT(sg[k][:, 0:nn], pg[k][:, 0:nn], AF.Silu, [b_pg[k]], [b_sg[k]])
                        for dc in range(16):
                            MM(pu[k][:, 0:nn], wu_[:, dc, fc * 128:(fc + 1) * 128], cTb[:, dc, n0:n0 + nn], dc == 0, dc == 15,
                               [b_wu], [b_pu[k]])
                        TT("dve", a_[:, fc, 0:nn], pu[k][:, 0:nn], sg[k][:, 0:nn], ALU.mult, [b_pu[k], b_sg[k]], [ba])
                    for ti in tls:
                        (_, np_, c0) = TILES[ti]
                        for cb in range(4):
                            k = ocnt % 3
                            ocnt += 1
                            for fc in range(4):
                                MM(po[k][0:np_, :], a_[:, fc, c0 - n0:c0 - n0 + np_], wd[:, fc, cb * 512:(cb + 1) * 512],
                                   fc == 0, fc == 3, [ba, b_wd], [b_po[k]])
                            STT("dve", acc[0:np_, ti, cb * 512:(cb + 1) * 512], po[k][0:np_, :], comb[0:np_, ti, e_:e_ + 1],
                                acc[0:np_, ti, cb * 512:(cb + 1) * 512], ALU.mult, ALU.add, [b_po[k], b_acc[ti], b_comb], [b_acc[ti]])
            P.flush()
            esM.close()
            gbf = sbt(esE, "gbf", [128, D], F32); b_gbf = P.buf()
            P.dma("sp", gbf[:], norm_final.partition_broadcast(128), [], [b_gbf], "gbf")
            yt = [sbt(esE, un("yt"), [128, D], F32) for _ in range(2)]; b_yt = [P.buf(), P.buf()]
            ssf = sbt(esE, "ssf", [128, 1], F32); rsf = sbt(esE, "rsf", [128, 1], F32)
            for (ti, np_, c0) in TILES:
                y_ = yt[ti % 2]; by = b_yt[ti % 2]
                rmsnorm_tile(None, acc[:, ti, :], np_, gbf, y_, ssf, rsf, y_, [b_gbf, b_acc[ti]], [by], by)
                P.dma("sp" if ti % 2 == 0 else "act", y_o[c0:c0 + np_, :], y_[0:np_, :], [by], [], "yo%d" % (ti % 2))
            P.flush()
        P.flush(final=True)
    return nc


_CONSTS = None


def make_in_maps(inputs):
    global _CONSTS
    if _CONSTS is None:
        _CONSTS = build_consts()
    g = lambda k: np.ascontiguousarray(np.asarray(inputs[k], dtype=np.float32))
    xp = g("x_prompt"); xs = g("x_sample"); memp = g("mem_prompt")
    shared = {
        "consts": _CONSTS,
        "norm_mix": g("norm_mix")[0], "w_in": g("w_in")[0], "conv_w": g("conv_w")[0], "conv_b": g("conv_b")[0],
        "conv_ln_g": g("conv_ln_g")[0], "conv_ln_b": g("conv_ln_b")[0], "shift_mu": g("shift_mu")[0],
        "w_decay_up": g("w_decay_up")[0], "decay_bias": g("decay_bias")[0], "w_a_up": g("w_a_up")[0],
        "a_bias": g("a_bias")[0], "w_g_up": g("w_g_up")[0], "k_k": g("k_k")[0], "k_a": g("k_a")[0],
        "r_k": g("r_k")[0].reshape(1024), "lnx_g": g("lnx_g")[0], "lnx_b": g("lnx_b")[0],
        "w_out": g("w_out")[0], "norm_x": g("norm_x")[0], "norm_mem": g("norm_mem")[0],
        "w_cq": g("w_cq")[0], "w_ck": g("w_ck")[0], "w_cv": g("w_cv")[0], "w_co": g("w_co")[0],
        "norm_ffn": g("norm_ffn")[0],
        "w_route": np.ascontiguousarray(np.concatenate([g("w_route_group")[0], g("w_route_expert")[0].reshape(D, 32)], axis=1)),
        "b_route": np.ascontiguousarray(np.concatenate([g("b_route_group")[0], g("b_route_expert")[0].reshape(32)])),
        "w_gate": g("w_gate")[0].reshape(32, D, 512), "w_up": g("w_up")[0].reshape(32, D, 512),
        "w_down": g("w_down")[0].reshape(32, 512, D), "norm_final": g("norm_final"),
    }
    cc = g("cache_conv")[0]; ssh = g("state_shift")[0]; srw = g("state_rwkv")[0]
    cmk = g("cache_mem_k")[0].reshape(128, 256, D); cmv = g("cache_mem_v")[0].reshape(128, 256, D)
    maps = []
    for c in range(8):
        b, half = c // 2, c % 2
        if half == 0:
            xw = np.concatenate([np.zeros((1024, D), np.float32), xp[b, 0:1024]], axis=0)
        else:
            xw = xp[b]
        m = dict(shared)
        m.update({"xw": np.ascontiguousarray(xw), "xs": np.ascontiguousarray(xs[16 * c:16 * c + 16, 0]),
                  "mem": memp[b], "cconv": cc[16 * c:16 * c + 16], "sshift": ssh[16 * c:16 * c + 16],
                  "srwkv": srw[16 * c:16 * c + 16], "cmk": cmk[16 * c:16 * c + 16], "cmv": cmv[16 * c:16 * c + 16]})
        maps.append({k: v for k, v in m.items() if k in DECL})
    return maps


def gather(res):
    R = res.results
    y_p = np.zeros((4, 2048, D), np.float32); y_s = np.zeros((128, 1, D), np.float32)
    conv_p = np.zeros((1, 4, 30, CW), np.float32); shift_p = np.zeros((1, 4, SHIFT_W), np.float32)
    rwkv_p = np.zeros((1, 4, 16, 64, 64), np.float32)
    memk = np.zeros((1, 4, 256, 4, 512), np.float32); memv = np.zeros((1, 4, 256, 4, 512), np.float32)
    conv_s = np.zeros((1, 128, 30, CW), np.float32); shift_s = np.zeros((1, 128, SHIFT_W), np.float32)
    rwkv_s = np.zeros((1, 128, 16, 64, 64), np.float32)
    for c in range(8):
        b, half = c // 2, c % 2
        r = R[c]
        y_p[b, half * 1024:(half + 1) * 1024] = r["y_o"][0:1024]
        y_s[16 * c:16 * c + 16, 0] = r["y_o"][1024:1040]
        conv_s[0, 16 * c:16 * c + 16] = r["convs_o"]; shift_s[0, 16 * c:16 * c + 16] = r["shifts_o"]
        rwkv_s[0, 16 * c:16 * c + 16] = r["rwkvs_o"]
        if half == 1:
            conv_p[0, b] = r["convp_o"]; shift_p[0, b] = r["shiftp_o"][0]; rwkv_p[0, b] = r["rwkvp_o"]
            memk[0, b] = r["memk_o"].reshape(256, 4, 512); memv[0, b] = r["memv_o"].reshape(256, 4, 512)
    return (y_p, y_s, conv_p, shift_p, rwkv_p, memk, memv, conv_s, shift_s, rwkv_s)


_NC = None


def kernel(**inputs):
    global _NC
    if _NC is None:
        _NC = build_program()
    maps = make_in_maps(inputs)
    res = run_bass_kernel_spmd(_NC, maps, core_ids=list(range(8)))
    return gather(res)
```
